# Optimizing a Trainium2 kernel written in Bass

```python
import math
import jax
import jax.numpy as jnp
from jax import lax
import numpy as np

D_MODEL = 2048
BATCH = 2
SEQ = 8192
DEPTH = 1

D_ATTN = D_MODEL // 2
HEAD_DIM_A = 64
N_HEADS_A = D_ATTN // HEAD_DIM_A
DILATED_CONFIGS = ((128, 1), (512, 4), (2048, 16))
ATTN_BLOCK = 128
NUM_BUCKETS = 32
MAX_DISTANCE = 2048
D_MLSTM = D_MODEL - D_ATTN
N_HEADS_M = 4
HEAD_DIM_M = D_MLSTM // N_HEADS_M
CONV_K = 4
MLSTM_CHUNK = 128
IN_COLS = 3 * D_ATTN + 4 * D_MLSTM + 2 * N_HEADS_M
SPLITS = (D_ATTN, 2 * D_ATTN, 3 * D_ATTN, 3 * D_ATTN + 2 * D_MLSTM, 3 * D_ATTN + 3 * D_MLSTM,
          3 * D_ATTN + 4 * D_MLSTM, 3 * D_ATTN + 4 * D_MLSTM + N_HEADS_M)
N_EXPERTS = 32
TOP_K = 4
D_FF = D_MODEL
SWIGLU_LIMIT = 7.0
SWIGLU_ALPHA = 1.702
EXPERT_BLOCK = 128
DN_ALPHA = (2 * DEPTH) ** 0.25
DN_BETA = (8 * DEPTH) ** -0.25
LN_EPS = 1e-5
HEAD_NORM_EPS = 1e-6
NEG_INF = -1e30

kernel_name = "hymba_dilated_mlstm_moe_deepnorm"


def layer_norm(x, g, b):
    xf = x.astype(jnp.float32)
    mu = jnp.mean(xf, axis=-1, keepdims=True)
    var = jnp.mean(jnp.square(xf - mu), axis=-1, keepdims=True)
    return ((xf - mu) * lax.rsqrt(var + LN_EPS) * g + b).astype(x.dtype)


def head_rms(t):
    tf = t.astype(jnp.float32)
    tf = tf * lax.rsqrt(jnp.mean(jnp.square(tf), axis=-1, keepdims=True) + HEAD_NORM_EPS)
    return tf.reshape(t.shape[0], t.shape[1], -1)


def t5_bucket(dist):
    max_exact = NUM_BUCKETS // 2
    d_f = jnp.maximum(dist, 1).astype(jnp.float32)
    large = max_exact + (jnp.log(d_f / max_exact) / math.log(MAX_DISTANCE / max_exact)
                         * (NUM_BUCKETS - max_exact)).astype(jnp.int32)
    large = jnp.minimum(large, NUM_BUCKETS - 1)
    return jnp.where(dist < max_exact, dist, large)


def dilated_branch(q, k, v, rel_bias, window, dil):
    B, H, S, dh = q.shape
    n_back = window // dil
    L = S // dil
    Lp = -(-L // ATTN_BLOCK) * ATTN_BLOCK
    nb = Lp // ATTN_BLOCK

    def to_blocks(t):
        t = t.reshape(B, H, L, dil, dh).transpose(0, 1, 3, 2, 4)
        t = jnp.pad(t, ((0, 0), (0, 0), (0, 0), (0, Lp - L), (0, 0)))
        return t.reshape(B, H, dil, nb, ATTN_BLOCK, dh)

    def with_prev(t):
        prev = jnp.concatenate([jnp.zeros_like(t[:, :, :, :1]), t[:, :, :, :-1]], axis=3)
        return jnp.concatenate([prev, t], axis=4)

    qb = to_blocks(q)
    kc = with_prev(to_blocks(k))
    vc = with_prev(to_blocks(v)).astype(jnp.float32)
    s = jnp.einsum('bhrnqc,bhrnkc->bhrnqk', qb, kc).astype(jnp.float32)

    j = (jnp.arange(ATTN_BLOCK)[:, None] + ATTN_BLOCK) - jnp.arange(2 * ATTN_BLOCK)[None, :]
    band = (j >= 0) & (j <= n_back)
    has_prev = (jnp.arange(nb)[:, None, None] > 0) | (jnp.arange(2 * ATTN_BLOCK)[None, None, :] >= ATTN_BLOCK)
    mask = band[None] & has_prev
    bias = rel_bias[t5_bucket(jnp.maximum(j, 0) * dil)].transpose(2, 0, 1)
    s = s + bias[None, :, None, None].astype(jnp.float32)
    s = jnp.where(mask, s, NEG_INF)

    m = jnp.max(s, axis=-1, keepdims=True)
    p = jnp.exp(s - m)
    den = jnp.sum(p, axis=-1, keepdims=True)
    o = jnp.einsum('bhrnqk,bhrnkc->bhrnqc', p, vc) / den
    lse = (m + jnp.log(den))[..., 0]

    o = o.reshape(B, H, dil, Lp, dh)[:, :, :, :L].transpose(0, 1, 3, 2, 4).reshape(B, H, S, dh)
    lse = lse.reshape(B, H, dil, Lp)[:, :, :, :L].transpose(0, 1, 3, 2).reshape(B, H, S)
    return o, lse


def causal_depthwise_conv(t, w, b):
    C = t.shape[-1]
    out = lax.conv_general_dilated(t, w[:, None, :].astype(t.dtype), window_strides=(1,),
                                   padding=[(CONV_K - 1, 0)], dimension_numbers=('NWC', 'WIO', 'NWC'),
                                   feature_group_count=C)
    return out + b


def mlstm(q, k, v, i_pre, f_pre):
    B, S, H, dh = q.shape
    N = S // MLSTM_CHUNK
    f32 = jnp.float32

    def chunks(t):
        return t.astype(f32).reshape(B, N, MLSTM_CHUNK, H, dh).transpose(1, 0, 3, 2, 4)

    def gchunks(t):
        return t.astype(f32).reshape(B, N, MLSTM_CHUNK, H).transpose(1, 0, 3, 2)

    qc, kc, vc = chunks(q), chunks(k) * (dh ** -0.5), chunks(v)
    ic = gchunks(i_pre)
    lfc = jax.nn.log_sigmoid(gchunks(f_pre))
    causal = jnp.tril(jnp.ones((MLSTM_CHUNK, MLSTM_CHUNK), dtype=bool))

    def step(carry, inp):
        C, n, m = carry
        qt, kt, vt, it, lf = inp
        b = jnp.cumsum(lf, axis=-1)
        D = jnp.where(causal, b[..., :, None] - b[..., None, :] + it[..., None, :], -jnp.inf)
        m_inter = b + m[..., None]
        m_t = jnp.maximum(m_inter, jnp.max(D, axis=-1))
        W = jnp.exp(D - m_t[..., None]) * jnp.einsum('bhtd,bhsd->bhts', qt, kt)
        decay = jnp.exp(m_inter - m_t)
        num = jnp.einsum('bhts,bhsd->bhtd', W, vt) + decay[..., None] * jnp.einsum('bhvk,bhtk->bhtv', C, qt)
        den = jnp.sum(W, axis=-1) + decay * jnp.einsum('bhk,bhtk->bht', n, qt)
        h = num / jnp.maximum(jnp.abs(den), jnp.exp(-m_t))[..., None]
        g = b[..., -1]
        a = g[..., None] - b + it
        m_new = jnp.maximum(g + m, jnp.max(a, axis=-1))
        carry_decay = jnp.exp(g + m - m_new)
        wa = jnp.exp(a - m_new[..., None])
        C = carry_decay[..., None, None] * C + jnp.einsum('bhsv,bhsk->bhvk', wa[..., None] * vt, kt)
        n = carry_decay[..., None] * n + jnp.einsum('bhs,bhsk->bhk', wa, kt)
        return (C, n, m_new), h

    init = (jnp.zeros((B, H, dh, dh), f32), jnp.zeros((B, H, dh), f32), jnp.zeros((B, H), f32))
    _, hs = lax.scan(step, init, (qc, kc, vc, ic, lfc))
    return hs.transpose(1, 0, 3, 2, 4).reshape(B, S, H, dh)


def hybrid_mixer(h, w_in, b_igate, b_fgate, conv_w, conv_b, rel_bias, beta_attn, beta_mlstm, w_out):
    B, S, _ = h.shape
    proj = h @ w_in
    qa, ka, va, qk_m, vm, om, ig, fg = jnp.split(proj, SPLITS, axis=-1)

    def heads_a(t):
        return t.reshape(B, S, N_HEADS_A, HEAD_DIM_A).transpose(0, 2, 1, 3)
    qa = heads_a(qa) * (HEAD_DIM_A ** -0.5)
    ka, va = heads_a(ka), heads_a(va)
    outs, lses = [], []
    for window, dil in DILATED_CONFIGS:
        o, l = dilated_branch(qa, ka, va, rel_bias, window, dil)
        outs.append(o)
        lses.append(l)
    wts = jax.nn.softmax(jnp.stack(lses), axis=0)
    o_attn = jnp.einsum('gbhs,gbhsd->bshd', wts, jnp.stack(outs))

    qk_m = jax.nn.silu(causal_depthwise_conv(qk_m, conv_w, conv_b))
    qm, km = jnp.split(qk_m, 2, axis=-1)
    def heads_m(t):
        return t.reshape(B, S, N_HEADS_M, HEAD_DIM_M)
    hm = mlstm(heads_m(qm), heads_m(km), heads_m(vm), ig + b_igate, fg + b_fgate)
    hm = jax.nn.sigmoid(heads_m(om).astype(jnp.float32)) * hm

    y = jnp.concatenate([head_rms(o_attn) * beta_attn, head_rms(hm) * beta_mlstm], axis=-1)
    return y.astype(h.dtype) @ w_out


def moe_ffn(xf, w_router, b_router, w_up, b_up, w_down, b_down):
    N, D = xf.shape
    logits = (xf @ w_router + b_router).astype(jnp.float32)
    top_vals, top_idx = lax.top_k(logits, TOP_K)
    gates = jax.nn.softmax(top_vals, axis=-1)
    A = N * TOP_K
    e_flat = top_idx.reshape(-1).astype(jnp.int32)
    tok_flat = jnp.arange(A, dtype=jnp.int32) // TOP_K
    g_flat = gates.reshape(-1)

    order = jnp.argsort(e_flat, stable=True)
    e_sorted = e_flat[order]
    counts = jnp.zeros((N_EXPERTS,), jnp.int32).at[e_flat].add(1)
    padded = (counts + EXPERT_BLOCK - 1) // EXPERT_BLOCK * EXPERT_BLOCK
    pad_end = jnp.cumsum(padded)
    pad_start = pad_end - padded
    start = jnp.cumsum(counts) - counts
    dest = pad_start[e_sorted] + (jnp.arange(A, dtype=jnp.int32) - start[e_sorted])
    n_blocks = -(-A // EXPERT_BLOCK) + N_EXPERTS
    P = n_blocks * EXPERT_BLOCK
    slot_tok = jnp.zeros((P,), jnp.int32).at[dest].set(tok_flat[order])
    slot_gate = jnp.zeros((P,), jnp.float32).at[dest].set(g_flat[order])
    blk_exp = jnp.minimum(jnp.searchsorted(pad_end, jnp.arange(n_blocks, dtype=jnp.int32) * EXPERT_BLOCK,
                                           side='right'), N_EXPERTS - 1).astype(jnp.int32)
    xs = xf[slot_tok].reshape(n_blocks, EXPERT_BLOCK, D)

    def expert_block(args):
        e, xb = args
        hg = xb @ w_up[e] + b_up[e]
        gate = jnp.minimum(hg[:, :D_FF], SWIGLU_LIMIT)
        up = jnp.clip(hg[:, D_FF:], -SWIGLU_LIMIT, SWIGLU_LIMIT)
        act = (up + 1.0) * (gate * jax.nn.sigmoid(SWIGLU_ALPHA * gate))
        return act @ w_down[e] + b_down[e]

    ys = lax.map(expert_block, (blk_exp, xs)).reshape(P, D)
    return jnp.zeros_like(xf).at[slot_tok].add(ys * slot_gate[:, None].astype(ys.dtype))


def setup_inputs(seed: int = 0) -> dict:
    key = jax.random.key(seed)
    ks = jax.random.split(key, 20)
    f32 = jnp.float32
    nrm = lambda k, shape: jax.random.normal(k, shape, f32)
    return {
        "x": nrm(ks[0], (BATCH, SEQ, D_MODEL)),
        "w_in": nrm(ks[1], (DEPTH, D_MODEL, IN_COLS)) * D_MODEL ** -0.5,
        "b_igate": 0.1 * nrm(ks[2], (DEPTH, N_HEADS_M)),
        "b_fgate": jnp.linspace(3.0, 6.0, N_HEADS_M, dtype=f32)[None] + 0.1 * nrm(ks[3], (DEPTH, N_HEADS_M)),
        "conv_w": nrm(ks[4], (DEPTH, CONV_K, 2 * D_MLSTM)) * CONV_K ** -0.5,
        "conv_b": 0.01 * nrm(ks[5], (DEPTH, 2 * D_MLSTM)),
        "rel_bias": 0.5 * nrm(ks[6], (NUM_BUCKETS, N_HEADS_A)),
        "beta_attn": 1.0 + 0.02 * nrm(ks[7], (DEPTH, D_ATTN)),
        "beta_mlstm": 1.0 + 0.02 * nrm(ks[8], (DEPTH, D_MLSTM)),
        "w_out": nrm(ks[9], (DEPTH, D_MODEL, D_MODEL)) * (D_MODEL ** -0.5 * DN_BETA),
        "ln1_g": 1.0 + 0.02 * nrm(ks[10], (DEPTH, D_MODEL)),
        "ln1_b": 0.02 * nrm(ks[11], (DEPTH, D_MODEL)),
        "w_router": nrm(ks[12], (DEPTH, D_MODEL, N_EXPERTS)) * D_MODEL ** -0.5,
        "b_router": 0.01 * nrm(ks[13], (DEPTH, N_EXPERTS)),
        "w_up": nrm(ks[14], (DEPTH, N_EXPERTS, D_MODEL, 2 * D_FF)) * D_MODEL ** -0.5,
        "b_up": 0.01 * nrm(ks[15], (DEPTH, N_EXPERTS, 2 * D_FF)),
        "w_down": nrm(ks[16], (DEPTH, N_EXPERTS, D_FF, D_MODEL)) * (D_FF ** -0.5 * DN_BETA),
        "b_down": 0.01 * nrm(ks[17], (DEPTH, N_EXPERTS, D_MODEL)),
        "ln2_g": 1.0 + 0.02 * nrm(ks[18], (DEPTH, D_MODEL)),
        "ln2_b": 0.02 * nrm(ks[19], (DEPTH, D_MODEL)),
    }


def reference(x, w_in, b_igate, b_fgate, conv_w, conv_b, rel_bias, beta_attn, beta_mlstm, w_out,
              ln1_g, ln1_b, w_router, b_router, w_up, b_up, w_down, b_down, ln2_g, ln2_b):
    h = x
    B, S, D = h.shape
    for l in range(DEPTH):
        mix = hybrid_mixer(h, w_in[l], b_igate[l], b_fgate[l], conv_w[l], conv_b[l], rel_bias,
                           beta_attn[l], beta_mlstm[l], w_out[l])
        h = layer_norm(DN_ALPHA * h + mix, ln1_g[l], ln1_b[l])
        ffn = moe_ffn(h.reshape(B * S, D), w_router[l], b_router[l], w_up[l], b_up[l],
                      w_down[l], b_down[l]).reshape(B, S, D)
        h = layer_norm(DN_ALPHA * h + ffn, ln2_g[l], ln2_b[l])
    return h
```

```python
import math
from contextlib import ExitStack

import numpy as np
import concourse.bass as bass
import concourse.mybir as mybir
from concourse.bass_utils import run_bass_kernel_spmd

F32 = mybir.dt.float32
BF16 = mybir.dt.bfloat16
I32 = mybir.dt.int32
AF = mybir.ActivationFunctionType
ALU = mybir.AluOpType
AX = mybir.AxisListType

NCORES = 8
D = 2048
NSL = 8192
OWN0 = 6144
HAL0 = 4096
CAP = 512
NTC = CAP // 128
NEXP = 32
NSLOT = NEXP * CAP
ALPHA = 2.0 ** 0.25
NEGB = -80.0
LN_EPS = 1e-5
HN_EPS = 1e-6
IN_COLS = 7176
DILS = (1, 4, 16)


class Buf:
    __slots__ = ("name", "lw", "rd", "excl")

    def __init__(self, name):
        self.name = name
        self.lw = None
        self.rd = []
        self.excl = False


class DBuf(Buf):
    pass


class Op:
    __slots__ = ("eng", "fn", "deps", "dma", "sig", "idx", "key", "dsem", "dval", "xw", "done")


class Sched:
    ENGS = ["pe", "act", "dve", "pool", "sp"]
    EP = 20000

    def __init__(self, nc, stack):
        self.nc = nc
        self.stack = stack
        self.ops = []
        self.cnt = {e: 0 for e in self.ENGS}
        self.esems = {e: [] for e in self.ENGS}
        self.dsem_pool = []
        self.dsem_free = []
        self.keymap = {}
        self.waited = {e: {} for e in self.ENGS}

    def _dsem(self, key):
        if key not in self.keymap:
            if not self.dsem_free:
                sem = self.stack.enter_context(self.nc.semaphore("dq%d" % len(self.dsem_pool)))
                self.dsem_pool.append([sem, 0])
                self.dsem_free.append(len(self.dsem_pool) - 1)
            self.keymap[key] = self.dsem_free.pop()
        return self.keymap[key]

    def add(self, eng, fn, reads=(), writes=(), dma=False, key=None, par=False):
        op = Op()
        op.eng, op.fn, op.dma, op.sig, op.deps, op.xw, op.done = eng, fn, dma, False, [], None, False
        for b in reads:
            if b.lw is not None and not b.lw.done:
                op.deps.append((b.lw, "raw"))
            if b.excl:
                for r in b.rd:
                    if r.eng != eng and not r.done:
                        op.deps.append((r, "rar"))
        for b in writes:
            if b.lw is not None and not b.lw.done and not (par and b.lw.dma and dma):
                op.deps.append((b.lw, "waw"))
            for r in b.rd:
                if r is not op and not r.done:
                    op.deps.append((r, "war"))
        for b in reads:
            b.rd.append(op)
        for b in writes:
            b.lw = op
            b.rd = []
        if dma:
            if key is None:
                key = writes[0] if (writes and not isinstance(writes[0], DBuf)) else reads[0]
            si = self._dsem(key)
            self.dsem_pool[si][1] += 16
            op.dsem = self.dsem_pool[si][0]
            op.dval = self.dsem_pool[si][1]
        self.ops.append(op)
        return op

    def flush(self):
        nc = self.nc
        last = {}
        for op in self.ops:
            if not op.dma and op.fn is not None:
                last[op.eng] = op
        dm = [(p[0], p[1]) for p in self.dsem_pool if p[1] > 0]
        for e in self.ENGS:
            op = Op()
            op.eng, op.fn, op.dma, op.sig, op.done = e, None, False, False, False
            op.deps = [(o, "bar") for o in last.values()]
            op.xw = dm
            self.ops.append(op)
        for op in self.ops:
            nd = []
            for d, kind in op.deps:
                if d.dma:
                    nd.append(d)
                    continue
                if d.eng == op.eng and not op.dma:
                    if kind == "bar" or op.eng == "pe" or kind == "war":
                        continue
                d.sig = True
                nd.append(d)
            op.deps = nd
        EP = self.EP
        for op in self.ops:
            if op.sig:
                self.cnt[op.eng] += 1
                op.idx = self.cnt[op.eng]
        for e in self.ENGS:
            need = (self.cnt[e] + EP - 1) // EP
            while len(self.esems[e]) < max(need, 1):
                self.esems[e].append(self.stack.enter_context(nc.semaphore("e_%s%d" % (e, len(self.esems[e])))))
        ops = self.ops

        def gen(ename):
            def body(e):
                waited = self.waited[ename]
                for op in ops:
                    if op.eng != ename:
                        continue
                    need = {}
                    for d in op.deps:
                        if d.dma:
                            sem, val = d.dsem, d.dval
                        else:
                            sem = self.esems[d.eng][(d.idx - 1) // EP]
                            val = (d.idx - 1) % EP + 1
                        if need.get(sem, (None, 0))[1] < val:
                            need[sem] = (sem, val)
                    if op.xw:
                        for sem, c in op.xw:
                            if need.get(sem, (None, 0))[1] < c:
                                need[sem] = (sem, c)
                    for sem, val in need.values():
                        if waited.get(sem, 0) >= val:
                            continue
                        e.wait_ge(sem, val)
                        waited[sem] = val
                    if op.fn is None:
                        continue
                    ins = op.fn(e)
                    if op.dma:
                        ins.then_inc(op.dsem, 16)
                    elif op.sig:
                        ins.then_inc(self.esems[ename][(op.idx - 1) // EP], 1)
            return body

        with nc.Block() as block:
            block.tensor(gen("pe"))
            block.scalar(gen("act"))
            block.vector(gen("dve"))
            block.gpsimd(gen("pool"))
            block.sync(gen("sp"))
        for op in self.ops:
            op.done = True
        self.ops = []
        self.keymap = {}
        self.dsem_free = list(range(len(self.dsem_pool)))


class TB:
    def __init__(self, t, name):
        self.t = t
        self.b = Buf(name)


class Ctx:
    pass


def _bs(xs):
    return [x.b if isinstance(x, TB) else x for x in xs]


def build(ph_stop=5, debug=False, cut=99):
    nc = bass.Bass("TRN2", target_bir_lowering=False)
    gst = ExitStack()
    S = Sched(nc, gst)

    def din(name, shape, dt=F32):
        return nc.dram_tensor(name, shape, dt, kind="ExternalInput")

    def dscr(name, shape, dt):
        return nc.dram_tensor(name, shape, dt, kind="ExternalOutput" if debug else "Internal")

    def op(eng, fn, r=(), w=(), **kw):
        return S.add(eng, fn, _bs(r), _bs(w), **kw)

    st_holder = [gst]

    def sb(name, shape, dt):
        return TB(st_holder[0].enter_context(nc.sbuf_tensor(name, shape, dt)), name)

    xT = din("xT", [D, NSL])
    valid = din("valid", [128, 64])
    hv = din("hv", [128, 1])
    w_in = din("w_in", [D, IN_COLS])
    bgate = din("bgate", [1, 8])
    cw = din("cw", [128, 16, 4])
    cbias = din("cb", [128, 16])
    battn = din("battn", [128, 3 * 16 * 256])
    beta = din("beta", [1, D])
    ident = din("ident", [128, 128])
    triu = din("triu", [128, 128])
    trist = din("trist", [128, 128])
    ynT_d = dscr("ynT_d", [16, 128, 2048], BF16)
    v_d = nc.dram_tensor("v_d", [4096, 260], BF16, kind="Internal")
    ynT_db = DBuf("ynT_d")
    v_db = DBuf("v_d")
    if ph_stop >= 3:
        x_own = din("x_own", [2048, D])
        w_out = din("w_out", [D, D])
        ln1_g = din("ln1_g", [1, D])
        ln1_b = din("ln1_b", [1, D])
        w_router = din("w_router", [D, NEXP])
        b_router = din("b_router", [1, NEXP])
        ecap = din("ecap", [1, NEXP])
        h1_d = dscr("h1_d", [2048, D], F32)
        xs_d = nc.dram_tensor("xs_d", [NSLOT + 1, D], BF16, kind="Internal")
        h1_db = DBuf("h1_d")
        xs_db = DBuf("xs_d")
        if debug:
            dbg_r = nc.dram_tensor("dbg_r", [128, 16, 40], F32, kind="ExternalOutput")
            dbg_rb = DBuf("dbg_r")
    if ph_stop >= 4:
        w_up = din("w_up", [NEXP, D, 2 * D])
        bupT = din("bupT", [128, NEXP, 32])
        w_down = din("w_down", [NEXP, D, D])
        b_down = din("b_down", [NEXP, D])
        ys_d = nc.dram_tensor("ys_d", [NSLOT + 1, D], F32, kind="Internal")
        ys_db = DBuf("ys_d")
    if ph_stop >= 5:
        ln2_g = din("ln2_g", [1, D])
        ln2_b = din("ln2_b", [1, D])
        out_d = nc.dram_tensor("out", [2048, D], F32, kind="ExternalOutput")
        out_db = [DBuf("out%d" % i) for i in range(16)]

    xT_r = xT.ap().rearrange("(kc p) s -> p kc s", p=128)
    w_in_r = w_in.ap().rearrange("(kc p) c -> p kc c", p=128)

    def bcast(dt_, n):
        return bass.AP(dt_, 0, [[0, 128], [1, n]])

    ps = []
    for i in range(8):
        t = gst.enter_context(nc.psum_tensor("psb%d" % i, [128, 512], F32))
        ps.append(TB(t, "psb%d" % i))
        ps[-1].b.excl = True
    psb16 = [p.t.bitcast(BF16) for p in ps]

    ident_f = sb("ident_f", [128, 128], F32)
    ident_b = sb("ident_b", [128, 128], BF16)
    triu_f = sb("triu_f", [128, 128], F32)
    trist_f = sb("trist_f", [128, 128], F32)
    ones_f = sb("ones_f", [128, 128], F32)
    ones_b = sb("ones_b", [1, 128], BF16)
    hv_s = sb("hv_s", [128, 1], F32)
    valid_s = sb("valid_s", [128, 64], F32)
    beta_bc = sb("beta_bc", [128, D], F32)
    cw_s = sb("cw_s", [128, 16, 4], F32)
    cb_s = sb("cb_s", [128, 16], F32)
    bg_bc = sb("bg_bc", [128, 8], F32)
    offs_i = sb("offs_i", [128, 16, 4], I32)
    gk = sb("gk", [128, 16, 4], F32)

    op("sp", lambda e: e.dma_start(out=ident_f.t[:], in_=ident.ap()), w=[ident_f], dma=True)
    op("sp", lambda e: e.dma_start(out=triu_f.t[:], in_=triu.ap()), w=[triu_f], dma=True)
    op("sp", lambda e: e.dma_start(out=trist_f.t[:], in_=trist.ap()), w=[trist_f], dma=True)
    op("sp", lambda e: e.dma_start(out=hv_s.t[:], in_=hv.ap()), w=[hv_s], dma=True)
    op("sp", lambda e: e.dma_start(out=valid_s.t[:], in_=valid.ap()), w=[valid_s], dma=True)
    op("sp", lambda e: e.dma_start(out=beta_bc.t[:], in_=bcast(beta, D)), w=[beta_bc], dma=True)
    op("sp", lambda e: e.dma_start(out=cw_s.t[:], in_=cw.ap()), w=[cw_s], dma=True)
    op("sp", lambda e: e.dma_start(out=cb_s.t[:], in_=cbias.ap()), w=[cb_s], dma=True)
    op("sp", lambda e: e.dma_start(out=bg_bc.t[:], in_=bcast(bgate, 8)), w=[bg_bc], dma=True)
    op("dve", lambda e: e.tensor_copy(out=ident_b.t[:], in_=ident_f.t[:]), r=[ident_f], w=[ident_b])
    op("pool", lambda e: e.memset(ones_f.t[:], 1.0), w=[ones_f])
    op("pool", lambda e: e.memset(ones_b.t[:], 1.0), w=[ones_b])

    def phase1():
        with ExitStack() as st:
            st_holder[0] = st
            expB = sb("expB", [128, 3 * 16 * 256], BF16)
            tmpBs = [sb("tmpB%d" % i, [128, 1024], F32) for i in range(2)]
            for g in range(12):
                tmpB = tmpBs[g % 2]
                op("sp", lambda e, g=g, tmpB=tmpB: e.dma_start(out=tmpB.t[:], in_=battn.ap()[:, g * 1024:(g + 1) * 1024]),
                   w=[tmpB], dma=True)
                op("act", lambda e, g=g, tmpB=tmpB: e.activation(out=expB.t[:, g * 1024:(g + 1) * 1024], in_=tmpB.t[:],
                                                      func=AF.Exp), r=[tmpB], w=[expB])
            Wq = sb("Wq", [128, 16, 256], BF16)
            Wk = sb("Wk", [128, 16, 256], BF16)
            Wv = sb("Wv", [128, 16, 256], BF16)
            kT = sb("kT", [128, 2, 4096], BF16)
            qT = sb("qT", [128, 2, 2048], BF16)
            xg = [sb("xg%d" % i, [128, 16, 512], BF16) for i in range(2)]
            vt = [sb("vt%d" % i, [128, 4, 4, 65], BF16) for i in range(2)]
            NBLK = 17 + 20 + 32
            vaug = sb("vaug", [128, NBLK, 4, 65], BF16)
            acc = sb("acc", [128, 4, 2048], F32)
            E = [sb("E%d" % i, [128, 256], BF16) for i in range(3)]
            PT = [sb("PT%d" % i, [128, 256], BF16) for i in range(3)]
            dn = sb("dn", [128, 4], F32)
            d2 = sb("d2", [128, 4], F32)
            sq = sb("sq", [128, 4, 65], F32)
            ssq = sb("ssq", [128, 4], F32)
            sc = sb("sc", [128, 4], F32)
            yn_tm = [sb("yn_tm%d" % i, [128, 256], BF16) for i in range(2)]
            ynTs = sb("ynTs", [128, 2, 1024], BF16)

            for v_ in vt:
                op("pool", lambda e, v_=v_: e.memset(v_.t[:], 1.0), w=[v_])
            blk = {}
            nb = 0
            for g, d in enumerate(DILS):
                first = 16 // d
                for r in range(d):
                    for mb in range(first - 1, 2 * first):
                        blk[(g, r, mb)] = nb
                        nb += 1
            assert nb == NBLK

            for hg in range(4):
                if cut <= 0:
                    break
                cq, ck, cv = 256 * hg, 1024 + 256 * hg, 2048 + 256 * hg
                op("pool", lambda e, c0=cq: e.dma_start(out=Wq.t[:], in_=w_in_r[:, :, c0:c0 + 256]), w=[Wq], dma=True)
                op("pool", lambda e, c0=ck: e.dma_start(out=Wk.t[:], in_=w_in_r[:, :, c0:c0 + 256]), w=[Wk], dma=True)
                op("pool", lambda e, c0=cv: e.dma_start(out=Wv.t[:], in_=w_in_r[:, :, c0:c0 + 256]), w=[Wv], dma=True)
                op("pool", lambda e: e.memset(acc.t[:], 0.0), w=[acc])
                for sg in range(8):
                    s0 = HAL0 + 512 * sg
                    x_ = xg[sg % 2]
                    op("pool", lambda e, x_=x_, s0=s0: e.dma_start(out=x_.t[:], in_=xT_r[:, :, s0:s0 + 512]),
                       w=[x_], dma=True)
                    for pi in range(2):
                        pk = ps[pi]

                        def mmk(e, pk=pk, x_=x_, pi=pi):
                            for kc in range(16):
                                ins = e.matmul(pk.t[:, :], Wk.t[:, kc, pi * 128:(pi + 1) * 128], x_.t[:, kc, :],
                                               start=(kc == 0), stop=(kc == 15))
                            return ins
                        op("pe", mmk, r=[Wk, x_], w=[pk])
                        op("act", lambda e, pk=pk, pi=pi, sg=sg: e.copy(out=kT.t[:, pi, sg * 512:(sg + 1) * 512],
                                                                       in_=pk.t[:, :]), r=[pk], w=[kT])
                    if sg >= 4:
                        for pi in range(2):
                            pq = ps[2 + pi]

                            def mmq(e, pq=pq, x_=x_, pi=pi):
                                for kc in range(16):
                                    ins = e.matmul(pq.t[:, :], Wq.t[:, kc, pi * 128:(pi + 1) * 128], x_.t[:, kc, :],
                                                   start=(kc == 0), stop=(kc == 15))
                                return ins
                            op("pe", mmq, r=[Wq, x_], w=[pq])
                            op("act", lambda e, pq=pq, pi=pi, sg=sg: e.activation(
                                out=qT.t[:, pi, (sg - 4) * 512:(sg - 3) * 512], in_=pq.t[:, :], func=AF.Copy,
                                scale=0.125), r=[pq], w=[qT])
                    v_ = vt[sg % 2]
                    for tl in range(4):
                        pv = ps[4 + tl % 2]

                        def mmv(e, pv=pv, x_=x_, tl=tl):
                            for kc in range(16):
                                ins = e.matmul(pv.t[:, 0:256], x_.t[:, kc, tl * 128:(tl + 1) * 128], Wv.t[:, kc, :],
                                               start=(kc == 0), stop=(kc == 15))
                            return ins
                        op("pe", mmv, r=[Wv, x_], w=[pv])
                        op("dve", lambda e, pv=pv, v_=v_, tl=tl: e.tensor_copy(
                            out=v_.t[:, tl, :, 0:64], in_=pv.t[:, 0:256].rearrange("p (h c) -> p h c", h=4)),
                           r=[pv], w=[v_])
                    u0 = 512 * sg
                    op("sp", lambda e, v_=v_, u0=u0: e.dma_start(
                        out=v_d.ap()[u0:u0 + 512, :].rearrange("(t p) c -> p t c", p=128),
                        in_=v_.t[:].rearrange("p t h c -> p t (h c)")),
                       r=[v_], w=[v_db], dma=True)
                if cut <= 1:
                    continue
                for g, d in enumerate(DILS):
                    first = 16 // d
                    nblk = first + 1
                    src = v_d.ap().rearrange("(mb p r) c -> r p mb c", p=128, r=d)
                    for r in range(d):
                        b0 = blk[(g, r, first - 1)]
                        op("sp", lambda e, src=src, r=r, b0=b0, nblk=nblk, first=first: e.dma_start(
                            out=vaug.t[:, b0:b0 + nblk].rearrange("p b h c -> p b (h c)"), in_=src[r][:, first - 1:2 * first]),
                           r=[v_db], w=[vaug], dma=True, par=True)
                if cut <= 2:
                    continue
                it = 0
                for hl in range(4):
                    pi, p0 = hl // 2, 64 * (hl % 2)
                    h = 4 * hg + hl
                    for g, d in enumerate(DILS):
                        first = 16 // d
                        for r in range(d):
                            for mb in range(first - 1, 2 * first):
                                halo = (mb == first - 1)
                                last = (mb == 2 * first - 1)
                                if halo:
                                    qb0, nq, bc0 = mb + 1, 1, 128
                                elif last:
                                    qb0, nq, bc0 = mb, 1, 0
                                else:
                                    qb0, nq, bc0 = mb, 2, 0
                                N = 128 * nq
                                ks = r + d * 128 * mb
                                ksl = slice(ks, ks + d * 127 + 1, d)
                                qs = r + d * 128 * qb0 - 2048
                                qsl = slice(qs, qs + d * (N - 1) + 1, d)
                                bcol = (g * 16 + h) * 256 + bc0
                                bi = blk[(g, r, mb)]
                                pS = ps[it % 3]
                                pO = ps[3 + it % 3]
                                E_ = E[it % 3]
                                P_ = PT[it % 3]
                                it += 1
                                op("pe", lambda e, pS=pS, ksl=ksl, qsl=qsl, pi=pi, p0=p0, N=N: e.matmul(
                                    pS.t[:, 0:N], kT.t[p0:p0 + 64, pi, ksl], qT.t[p0:p0 + 64, pi, qsl],
                                    start=True, stop=True), r=[kT, qT], w=[pS])
                                op("act", lambda e, pS=pS, E_=E_, N=N: e.activation(
                                    out=E_.t[:, 0:N], in_=pS.t[:, 0:N], func=AF.Exp), r=[pS], w=[E_])
                                if halo:
                                    op("dve", lambda e, E_=E_, P_=P_, N=N, bcol=bcol: e.scalar_tensor_tensor(
                                        out=P_.t[:, 0:N], in0=E_.t[:, 0:N], scalar=hv_s.t[:, 0:1],
                                        in1=expB.t[:, bcol:bcol + N], op0=ALU.mult, op1=ALU.mult),
                                       r=[E_, expB, hv_s], w=[P_])
                                else:
                                    op("dve", lambda e, E_=E_, P_=P_, N=N, bcol=bcol: e.tensor_tensor(
                                        out=P_.t[:, 0:N], in0=E_.t[:, 0:N], in1=expB.t[:, bcol:bcol + N],
                                        op=ALU.mult), r=[E_, expB], w=[P_])
                                op("pe", lambda e, pO=pO, P_=P_, bi=bi, hl=hl, N=N: e.matmul(
                                    pO.t[0:65, 0:N], vaug.t[:, bi, hl, 0:65], P_.t[:, 0:N], start=True, stop=True),
                                   r=[vaug, P_], w=[pO])
                                op("dve", lambda e, pO=pO, hl=hl, qsl=qsl, N=N: e.tensor_tensor(
                                    out=acc.t[0:65, hl, qsl], in0=acc.t[0:65, hl, qsl], in1=pO.t[0:65, 0:N],
                                    op=ALU.add), r=[pO, acc], w=[acc])
                if cut <= 3:
                    continue
                pF = ps[6]
                pFv = pF.t[:, 0:512].rearrange("p (h c) -> p h c", h=4)
                pY = ps[7]
                pYb = psb16[7]
                for t in range(16):
                    def trF(e, t=t):
                        for hl in range(4):
                            ins = e.transpose(pF.t[:, hl * 128:(hl + 1) * 128], acc.t[:, hl, t * 128:(t + 1) * 128],
                                              ident_f.t[:, :])
                        return ins
                    op("pe", trF, r=[acc, ident_f], w=[pF])
                    if cut <= 4:
                        continue
                    op("act", lambda e: e.activation(out=sq.t[:], in_=pFv[:, :, 0:65],
                                                     func=AF.Square), r=[pF], w=[sq])
                    op("dve", lambda e: e.tensor_copy(out=dn.t[:], in_=pFv[:, :, 64]), r=[pF], w=[dn])
                    op("dve", lambda e: e.tensor_tensor(out=d2.t[:], in0=dn.t[:], in1=dn.t[:], op=ALU.mult),
                       r=[dn], w=[d2])
                    op("dve", lambda e: e.tensor_reduce(out=ssq.t[:], in_=sq.t[:, :, 0:64], axis=AX.X, op=ALU.add),
                       r=[sq], w=[ssq])
                    op("dve", lambda e: e.scalar_tensor_tensor(out=sc.t[:], in0=d2.t[:], scalar=64.0 * HN_EPS,
                                                               in1=ssq.t[:], op0=ALU.mult, op1=ALU.add),
                       r=[d2, ssq], w=[sc])
                    op("act", lambda e: e.activation(out=sc.t[:], in_=sc.t[:], func=AF.Sqrt, scale=1.0 / 64.0),
                       r=[sc], w=[sc])
                    op("dve", lambda e: e.reciprocal(out=sc.t[:], in_=sc.t[:]), r=[sc], w=[sc])
                    if cut <= 5:
                        continue
                    y_ = yn_tm[t % 2]
                    for hl in range(4):
                        hh = 4 * hg + hl
                        op("dve", lambda e, y_=y_, hl=hl, hh=hh: e.scalar_tensor_tensor(
                            out=y_.t[:, hl * 64:(hl + 1) * 64], in0=pFv[:, hl, 0:64], scalar=sc.t[:, hl:hl + 1],
                            in1=beta_bc.t[:, hh * 64:(hh + 1) * 64], op0=ALU.mult, op1=ALU.mult),
                           r=[pF, sc, beta_bc], w=[y_])

                    if cut <= 6:
                        continue

                    def trY(e, y_=y_):
                        for cc in range(2):
                            ins = e.transpose(pYb[:, cc * 128:(cc + 1) * 128], y_.t[:, cc * 128:(cc + 1) * 128],
                                              ident_b.t[:, :])
                        return ins
                    op("pe", trY, r=[y_, ident_b], w=[pY])
                    op("act", lambda e, t=t: e.copy(out=ynTs.t[:, :, (t % 8) * 128:(t % 8 + 1) * 128],
                                                    in_=pYb[:, 0:256].rearrange("p (c t) -> p c t", c=2)),
                       r=[pY], w=[ynTs])
                    if t % 8 == 7:
                        th = (t // 8) * 1024
                        op("sp", lambda e, hg=hg, th=th: e.dma_start(
                            out=ynT_d.ap()[2 * hg:2 * hg + 2, :, th:th + 1024].rearrange("c p t -> p c t"),
                            in_=ynTs.t[:]), r=[ynTs], w=[ynT_db], dma=True, par=True)
            S.flush()
        st_holder[0] = gst

    def phase2():
        with ExitStack() as st:
            st_holder[0] = st
            xg = [sb("m_xg%d" % i, [128, 16, 512], BF16) for i in range(2)]
            wp = [sb("m_wp%d" % i, [128, 16, 256], BF16) for i in range(3)]
            wg = sb("m_wg", [128, 16, 8], BF16)
            kpre = sb("kpre", [128, 8, 515], BF16)
            qpre = sb("qpre", [128, 8, 515], BF16)
            kTm = sb("kTm", [128, 8, 512], BF16)
            qTm = sb("qTm", [128, 8, 512], BF16)
            ctmp = [sb("ctmp%d" % i, [128, 512], F32) for i in range(2)]
            v_tm = sb("v_tm", [128, 4, 4, 257], BF16)
            og = sb("og", [128, 4, 1024], BF16)
            gz = sb("gz", [128, 4, 8], F32)
            ex = sb("ex", [128, 4, 4], F32)
            lfn = sb("lfn", [128, 4, 4], F32)
            gt = [sb("gt%d" % i, [128, 4], F32) for i in range(2)]
            a_s = [sb("a_s%d" % i, [128, 4], F32) for i in range(2)]
            eb = [sb("eb%d" % i, [128, 4], F32) for i in range(2)]
            eG = [sb("eG%d" % i, [128, 4], F32) for i in range(2)]
            Cst = sb("Cst", [128, 4, 2, 257], F32)
            Cb = sb("Cb", [128, 4, 2, 257], BF16)
            ak = [sb("ak%d" % i, [128, 256], BF16) for i in range(2)]
            WT = [sb("WT%d" % i, [128, 128], BF16) for i in range(2)]
            dd = [sb("dd%d" % i, [128, 1], F32) for i in range(2)]
            hm = [sb("hm%d" % i, [128, 256], F32) for i in range(2)]
            sqm = sb("sqm", [128, 256], F32)
            ssm = [sb("ssm%d" % i, [128, 1], F32) for i in range(2)]
            ynm = [sb("ynm%d" % i, [128, 256], BF16) for i in range(2)]
            ynTs = sb("m_ynTs", [128, 8, 512], BF16)

            op("pool", lambda e: e.memset(kpre.t[:], 0.0), w=[kpre])
            op("pool", lambda e: e.memset(qpre.t[:], 0.0), w=[qpre])
            op("pool", lambda e: e.memset(v_tm.t[:], 1.0), w=[v_tm])
            op("pool", lambda e: e.memset(Cst.t[:], 0.0), w=[Cst])
            op("pool", lambda e: e.memset(Cb.t[:], 0.0), w=[Cb])
            op("pool", lambda e: e.dma_start(out=wg.t[:], in_=w_in_r[:, :, 7168:7176]), w=[wg], dma=True)

            pK = ps[4]
            pKb = psb16[4]
            pS = ps[5]
            pG = ps[2]
            pN = ps[3]
            pH = ps[6]
            pU = ps[7]
            wpi = 0
            fmi = 0
            tmi = 0
            ci_ = 0
            for gi in range(16):
                own = gi >= 12
                qcomp = gi >= 11
                s0 = 512 * gi
                x_ = xg[gi % 2]
                op("pool", lambda e, x_=x_, s0=s0: e.dma_start(out=x_.t[:], in_=xT_r[:, :, s0:s0 + 512]),
                   w=[x_], dma=True)
                pieces = [("k", i, 4096 + 256 * i) for i in range(4)] + [("v", i, 5120 + 256 * i) for i in range(4)]
                if qcomp:
                    pieces += [("q", i, 3072 + 256 * i) for i in range(4)]
                if own:
                    pieces += [("o", i, 6144 + 256 * i) for i in range(4)]
                for kind, i, c0 in pieces:
                    w_ = wp[wpi % 3]
                    wpi += 1
                    op("pool", lambda e, w_=w_, c0=c0: e.dma_start(out=w_.t[:], in_=w_in_r[:, :, c0:c0 + 256]),
                       w=[w_], dma=True)
                    if kind in ("k", "q"):
                        pre = kpre if kind == "k" else qpre
                        for cc in range(2):
                            pp = ps[fmi % 2]
                            fmi += 1

                            def mmf(e, pp=pp, w_=w_, x_=x_, cc=cc):
                                for kc in range(16):
                                    ins = e.matmul(pp.t[:, :], w_.t[:, kc, cc * 128:(cc + 1) * 128], x_.t[:, kc, :],
                                                   start=(kc == 0), stop=(kc == 15))
                                return ins
                            op("pe", mmf, r=[w_, x_], w=[pp])
                            op("act", lambda e, pp=pp, pre=pre, ch=2 * i + cc: e.copy(
                                out=pre.t[:, ch, 3:515], in_=pp.t[:, :]), r=[pp], w=[pre])
                    else:
                        for tl in range(4):
                            pp = ps[tmi % 2]
                            tmi += 1

                            def mmt(e, pp=pp, w_=w_, x_=x_, tl=tl):
                                for kc in range(16):
                                    ins = e.matmul(pp.t[:, 0:256], x_.t[:, kc, tl * 128:(tl + 1) * 128], w_.t[:, kc, :],
                                                   start=(kc == 0), stop=(kc == 15))
                                return ins
                            op("pe", mmt, r=[w_, x_], w=[pp])
                            if kind == "v":
                                op("dve", lambda e, pp=pp, tl=tl, i=i: e.tensor_copy(
                                    out=v_tm.t[:, tl, i, 0:256], in_=pp.t[:, 0:256]), r=[pp], w=[v_tm])
                            else:
                                op("act", lambda e, pp=pp, tl=tl, i=i: e.activation(
                                    out=og.t[:, tl, i * 256:(i + 1) * 256], in_=pp.t[:, 0:256], func=AF.Sigmoid),
                                   r=[pp], w=[og])
                for tl in range(4):
                    pp = ps[tmi % 2]
                    tmi += 1

                    def mmg(e, pp=pp, x_=x_, tl=tl):
                        for kc in range(16):
                            ins = e.matmul(pp.t[:, 0:8], x_.t[:, kc, tl * 128:(tl + 1) * 128], wg.t[:, kc, :],
                                           start=(kc == 0), stop=(kc == 15))
                        return ins
                    op("pe", mmg, r=[wg, x_], w=[pp])
                    op("dve", lambda e, pp=pp, tl=tl: e.tensor_tensor(out=gz.t[:, tl, :], in0=pp.t[:, 0:8],
                                                                     in1=bg_bc.t[:, :], op=ALU.add),
                       r=[pp, bg_bc], w=[gz])
                op("act", lambda e: e.activation(out=ex.t[:], in_=gz.t[:, :, 4:8], func=AF.Exp, scale=-1.0),
                   r=[gz], w=[ex])
                op("act", lambda e: e.activation(out=lfn.t[:], in_=ex.t[:], func=AF.Ln, bias=1.0, scale=1.0),
                   r=[ex], w=[lfn])
                convs = [("k", kpre, kTm, 8)]
                if own:
                    convs.append(("q", qpre, qTm, 0))
                for kind, pre, dst, cof in convs:
                    for ch in range(8):
                        ct = ctmp[ch % 2]
                        wch = cof + ch
                        op("dve", lambda e, ct=ct, pre=pre, ch=ch, wch=wch: e.tensor_scalar(
                            out=ct.t[:], in0=pre.t[:, ch, 0:512], scalar1=cw_s.t[:, wch, 0:1], scalar2=None,
                            op0=ALU.mult), r=[pre, cw_s], w=[ct])
                        for j in range(1, 4):
                            op("dve", lambda e, ct=ct, pre=pre, ch=ch, wch=wch, j=j: e.scalar_tensor_tensor(
                                out=ct.t[:], in0=pre.t[:, ch, j:j + 512], scalar=cw_s.t[:, wch, j:j + 1], in1=ct.t[:],
                                op0=ALU.mult, op1=ALU.add), r=[pre, cw_s, ct], w=[ct])
                        op("act", lambda e, ct=ct, dst=dst, ch=ch, wch=wch: e.activation(
                            out=dst.t[:, ch, :], in_=ct.t[:], func=AF.Silu, bias=cb_s.t[:, wch:wch + 1], scale=1.0),
                           r=[ct, cb_s], w=[dst])
                op("pool", lambda e: e.tensor_copy(out=kpre.t[:, :, 0:3], in_=kpre.t[:, :, 512:515]),
                   r=[kpre], w=[kpre])
                if qcomp:
                    op("pool", lambda e: e.tensor_copy(out=qpre.t[:, :, 0:3], in_=qpre.t[:, :, 512:515]),
                       r=[qpre], w=[qpre])
                for ci in range(4):
                    gc = 4 * gi + ci
                    cs = slice(ci * 128, (ci + 1) * 128)
                    gt_, a_, eb_, eG_ = gt[gc % 2], a_s[gc % 2], eb[gc % 2], eG[gc % 2]

                    def mmc(e, ci=ci):
                        e.matmul(pG.t[:, 256:260], triu_f.t[:, :], lfn.t[:, ci, :], start=True, stop=True)
                        return e.matmul(pG.t[:, 260:264], ones_f.t[:, :], lfn.t[:, ci, :], start=True, stop=True)
                    op("pe", mmc, r=[triu_f, ones_f, lfn], w=[pG])
                    op("dve", lambda e, gt_=gt_, ci=ci: e.tensor_tensor(out=gt_.t[:], in0=gz.t[:, ci, 0:4],
                                                                       in1=pG.t[:, 256:260], op=ALU.add),
                       r=[gz, pG], w=[gt_])
                    op("act", lambda e, gt_=gt_, a_=a_: e.activation(out=a_.t[:], in_=gt_.t[:], func=AF.Exp),
                       r=[gt_], w=[a_])
                    op("dve", lambda e, a_=a_, gc=gc: e.tensor_scalar(out=a_.t[:], in0=a_.t[:],
                                                                     scalar1=valid_s.t[:, gc:gc + 1], scalar2=0.0625,
                                                                     op0=ALU.mult, op1=ALU.mult),
                       r=[a_, valid_s], w=[a_])
                    op("act", lambda e, eG_=eG_: e.activation(out=eG_.t[:], in_=pG.t[:, 260:264], func=AF.Exp,
                                                              scale=-1.0), r=[pG], w=[eG_])
                    if own:
                        op("act", lambda e, eb_=eb_: e.activation(out=eb_.t[:], in_=pG.t[:, 256:260], func=AF.Exp,
                                                                  scale=-1.0), r=[pG], w=[eb_])
                    for h in range(4):
                        ak_ = ak[ci_ % 2]
                        WT_ = WT[ci_ % 2]
                        dd_ = dd[ci_ % 2]
                        hm_ = hm[ci_ % 2]
                        ss_ = ssm[ci_ % 2]
                        yn_ = ynm[ci_ % 2]
                        ci_ += 1

                        def trk(e, h=h, cs=cs):
                            for cc in range(2):
                                ins = e.transpose(pKb[:, cc * 128:(cc + 1) * 128], kTm.t[:, 2 * h + cc, cs],
                                                  ident_b.t[:, :])
                            return ins
                        op("pe", trk, r=[kTm, ident_b], w=[pK])
                        op("dve", lambda e, ak_=ak_, a_=a_, h=h: e.tensor_scalar(
                            out=ak_.t[:], in0=pKb[:, 0:256], scalar1=a_.t[:, h:h + 1], scalar2=None, op0=ALU.mult),
                           r=[pK, a_], w=[ak_])
                        if own:
                            def mms(e, h=h, cs=cs):
                                for cc in range(2):
                                    ins = e.matmul(pS.t[:, 0:128], kTm.t[:, 2 * h + cc, cs], qTm.t[:, 2 * h + cc, cs],
                                                   start=(cc == 0), stop=(cc == 1))
                                return ins
                            op("pe", mms, r=[kTm, qTm], w=[pS])
                            op("dve", lambda e, WT_=WT_, a_=a_, h=h: e.scalar_tensor_tensor(
                                out=WT_.t[:], in0=pS.t[:, 0:128], scalar=a_.t[:, h:h + 1], in1=triu_f.t[:, :],
                                op0=ALU.mult, op1=ALU.mult), r=[pS, a_, triu_f], w=[WT_])

                            def mmh(e, WT_=WT_, h=h, ci=ci, cs=cs):
                                e.matmul(pH.t[:, 0:257], WT_.t[:, :], v_tm.t[:, ci, h, :], start=True, stop=False)
                                e.matmul(pH.t[:, 0:257], qTm.t[:, 2 * h, cs], Cb.t[:, h, 0, :], start=False, stop=False)
                                return e.matmul(pH.t[:, 0:257], qTm.t[:, 2 * h + 1, cs], Cb.t[:, h, 1, :],
                                                start=False, stop=True)
                            op("pe", mmh, r=[WT_, v_tm, qTm, Cb], w=[pH])
                            op("dve", lambda e, dd_=dd_, eb_=eb_, h=h: e.tensor_scalar(
                                out=dd_.t[:], in0=pH.t[:, 256:257], scalar1=eb_.t[:, h:h + 1], scalar2=None,
                                op0=ALU.mult), r=[pH, eb_], w=[dd_])
                            op("dve", lambda e, dd_=dd_: e.scalar_tensor_tensor(
                                out=dd_.t[:], in0=dd_.t[:], scalar=-1.0, in1=dd_.t[:], op0=ALU.mult, op1=ALU.max),
                               r=[dd_], w=[dd_])
                            op("dve", lambda e, dd_=dd_: e.tensor_scalar(
                                out=dd_.t[:], in0=dd_.t[:], scalar1=1.0, scalar2=None, op0=ALU.max),
                               r=[dd_], w=[dd_])
                            op("dve", lambda e, dd_=dd_: e.reciprocal(out=dd_.t[:], in_=dd_.t[:]), r=[dd_], w=[dd_])
                            op("dve", lambda e, dd_=dd_, eb_=eb_, h=h: e.tensor_tensor(
                                out=dd_.t[:], in0=dd_.t[:], in1=eb_.t[:, h:h + 1], op=ALU.mult),
                               r=[dd_, eb_], w=[dd_])
                            op("dve", lambda e, hm_=hm_, dd_=dd_, ci=ci, h=h: e.scalar_tensor_tensor(
                                out=hm_.t[:], in0=pH.t[:, 0:256], scalar=dd_.t[:, 0:1],
                                in1=og.t[:, ci, h * 256:(h + 1) * 256], op0=ALU.mult, op1=ALU.mult),
                               r=[pH, dd_, og], w=[hm_])
                            op("act", lambda e, hm_=hm_: e.activation(out=sqm.t[:], in_=hm_.t[:], func=AF.Square),
                               r=[hm_], w=[sqm])
                            op("dve", lambda e, ss_=ss_: e.tensor_reduce(out=ss_.t[:], in_=sqm.t[:], axis=AX.X,
                                                                         op=ALU.add), r=[sqm], w=[ss_])
                            op("dve", lambda e, ss_=ss_: e.tensor_scalar(
                                out=ss_.t[:], in0=ss_.t[:], scalar1=1.0 / 256.0, scalar2=HN_EPS, op0=ALU.mult,
                                op1=ALU.add), r=[ss_], w=[ss_])
                            op("act", lambda e, ss_=ss_: e.activation(out=ss_.t[:], in_=ss_.t[:], func=AF.Sqrt),
                               r=[ss_], w=[ss_])
                            op("dve", lambda e, ss_=ss_: e.reciprocal(out=ss_.t[:], in_=ss_.t[:]), r=[ss_], w=[ss_])
                            op("dve", lambda e, yn_=yn_, hm_=hm_, ss_=ss_, h=h: e.scalar_tensor_tensor(
                                out=yn_.t[:], in0=hm_.t[:], scalar=ss_.t[:, 0:1],
                                in1=beta_bc.t[:, 1024 + h * 256:1024 + (h + 1) * 256], op0=ALU.mult, op1=ALU.mult),
                               r=[hm_, ss_, beta_bc], w=[yn_])

                            def try_(e, yn_=yn_):
                                for cc in range(2):
                                    ins = e.transpose(pKb[:, 256 + cc * 128:256 + (cc + 1) * 128],
                                                      yn_.t[:, cc * 128:(cc + 1) * 128], ident_b.t[:, :])
                                return ins
                            op("pe", try_, r=[yn_, ident_b], w=[pK])
                            op("act", lambda e, h=h, cs=cs: e.copy(
                                out=ynTs.t[:, 2 * h:2 * h + 2, cs],
                                in_=pKb[:, 256:512].rearrange("p (c t) -> p c t", c=2)), r=[pK], w=[ynTs])

                        def mmu(e, ak_=ak_, ci=ci, h=h):
                            for cc in range(2):
                                e.matmul(pU.t[:, cc * 256:(cc + 1) * 256], ak_.t[:, cc * 128:(cc + 1) * 128],
                                         v_tm.t[:, ci, h, 0:256], start=True, stop=True)
                            for cc in range(2):
                                ins = e.matmul(pN.t[:, 300 + cc:301 + cc], ak_.t[:, cc * 128:(cc + 1) * 128],
                                               v_tm.t[:, ci, h, 256:257], start=True, stop=True)
                            return ins
                        op("pe", mmu, r=[ak_, v_tm], w=[pU, pN])
                        op("dve", lambda e, eG_=eG_, h=h: e.tensor_scalar(
                            out=Cst.t[:, h].rearrange("p c n -> p (c n)"),
                            in0=Cst.t[:, h].rearrange("p c n -> p (c n)"),
                            scalar1=eG_.t[:, h:h + 1], scalar2=None, op0=ALU.mult), r=[Cst, eG_], w=[Cst])
                        op("dve", lambda e, eG_=eG_, h=h: e.scalar_tensor_tensor(
                            out=Cst.t[:, h, :, 0:256], in0=pU.t[:, :].rearrange("p (c n) -> p c n", c=2),
                            scalar=eG_.t[:, h:h + 1], in1=Cst.t[:, h, :, 0:256], op0=ALU.mult, op1=ALU.add),
                           r=[pU, eG_, Cst], w=[Cst])
                        op("dve", lambda e, eG_=eG_, h=h: e.scalar_tensor_tensor(
                            out=Cst.t[:, h, :, 256], in0=pN.t[:, 300:302], scalar=eG_.t[:, h:h + 1],
                            in1=Cst.t[:, h, :, 256], op0=ALU.mult, op1=ALU.add), r=[pN, eG_, Cst], w=[Cst])
                        if gc >= 47:
                            op("pool", lambda e, h=h: e.tensor_copy(out=Cb.t[:, h], in_=Cst.t[:, h]),
                               r=[Cst], w=[Cb])
                if own:
                    t0 = (gi - 12) * 512
                    op("sp", lambda e, t0=t0: e.dma_start(
                        out=ynT_d.ap()[8:16, :, t0:t0 + 512].rearrange("c p t -> p c t"), in_=ynTs.t[:]),
                       r=[ynTs], w=[ynT_db], dma=True)
            S.flush()
        st_holder[0] = gst

    def layer_norm_ops(y_ap, yB, g_bc, b_bc, stats, mv, rstd):
        for q in range(4):
            op("dve", lambda e, q=q: e.bn_stats(out=stats.t[:, q, :], in_=y_ap[:, q * 512:(q + 1) * 512]),
               r=[yB], w=[stats])
        op("dve", lambda e: e.bn_aggr(out=mv.t[:], in_=stats.t[:].rearrange("p a b -> p (a b)")), r=[stats], w=[mv])
        op("dve", lambda e: e.tensor_scalar(out=rstd.t[:], in0=mv.t[:, 1:2], scalar1=LN_EPS, scalar2=None,
                                            op0=ALU.add), r=[mv], w=[rstd])
        op("act", lambda e: e.activation(out=rstd.t[:], in_=rstd.t[:], func=AF.Sqrt), r=[rstd], w=[rstd])
        op("dve", lambda e: e.reciprocal(out=rstd.t[:], in_=rstd.t[:]), r=[rstd], w=[rstd])
        op("dve", lambda e: e.tensor_scalar(out=y_ap, in0=y_ap, scalar1=mv.t[:, 0:1], scalar2=rstd.t[:, 0:1],
                                            op0=ALU.subtract, op1=ALU.mult), r=[yB, mv, rstd], w=[yB])
        op("pool", lambda e: e.tensor_tensor(out=y_ap, in0=y_ap, in1=g_bc.t[:], op=ALU.mult), r=[yB, g_bc], w=[yB])
        op("pool", lambda e: e.tensor_tensor(out=y_ap, in0=y_ap, in1=b_bc.t[:], op=ALU.add), r=[yB, b_bc], w=[yB])

    def phase3():
        with ExitStack() as st:
            st_holder[0] = st
            ynTg = sb("ynTg", [128, 16, 512], BF16)
            wo = [sb("wo%d" % i, [128, 16, 512], BF16) for i in range(2)]
            xo = sb("xo", [128, 4, D], F32)
            y1 = [sb("y1_%d" % i, [128, D], F32) for i in range(4)]
            g_bc = sb("g1_bc", [128, D], F32)
            b_bc = sb("b1_bc", [128, D], F32)
            h1T = sb("h1T", [128, 16, 128], F32)
            h1b = [sb("h1b%d" % i, [128, D], BF16) for i in range(2)]
            wr = sb("wr", [128, 16, NEXP], F32)
            br_bc = sb("br_bc", [128, NEXP], F32)
            ecap_bc = sb("ecap_bc", [128, NEXP], F32)
            cntb = sb("cntb", [128, NEXP], F32)
            stats = sb("stats", [128, 4, 6], F32)
            mv = sb("mv", [128, 2], F32)
            rstd = sb("rstd", [128, 1], F32)
            lg = sb("lg", [128, NEXP], F32)
            mx8 = sb("mx8", [128, 8], F32)
            nmx = sb("nmx", [128, 1], F32)
            msk = sb("msk", [128, NEXP], F32)
            eml = sb("eml", [128, NEXP], F32)
            esum = sb("esum", [128, 1], F32)
            Gd = sb("Gd", [128, NEXP], F32)
            pos = sb("pos", [128, NEXP], F32)
            ovf = sb("ovf", [128, NEXP], F32)
            oh = sb("oh", [128, NEXP], F32)
            tmp32 = sb("tmp32", [128, NEXP], F32)
            offs_f = sb("offs_f", [128, 4], F32)

            op("sp", lambda e: e.dma_start(out=g_bc.t[:], in_=bcast(ln1_g, D)), w=[g_bc], dma=True)
            op("sp", lambda e: e.dma_start(out=b_bc.t[:], in_=bcast(ln1_b, D)), w=[b_bc], dma=True)
            op("sp", lambda e: e.dma_start(out=br_bc.t[:], in_=bcast(b_router, NEXP)), w=[br_bc], dma=True)
            op("sp", lambda e: e.dma_start(out=ecap_bc.t[:], in_=bcast(ecap, NEXP)), w=[ecap_bc], dma=True)
            op("sp", lambda e: e.dma_start(out=wr.t[:], in_=w_router.ap().rearrange("(kc p) n -> p kc n", p=128)),
               w=[wr], dma=True)
            op("pool", lambda e: e.memset(cntb.t[:], 0.0), w=[cntb])
            w_out_r = w_out.ap().rearrange("(kc p) c -> p kc c", p=128)
            pT = ps[4]
            pL = ps[5]
            pP = ps[6]
            pC = ps[7]
            woi = 0
            for tg in range(4):
                t0 = 512 * tg
                op("sp", lambda e, t0=t0: e.dma_start(
                    out=ynTg.t[:], in_=ynT_d.ap()[:, :, t0:t0 + 512].rearrange("c p t -> p c t")),
                   r=[ynT_db], w=[ynTg], dma=True)
                op("sp", lambda e, t0=t0: e.dma_start(
                    out=xo.t[:], in_=x_own.ap()[t0:t0 + 512, :].rearrange("(t p) d -> p t d", p=128)),
                   w=[xo], dma=True)
                for cg in range(4):
                    w_ = wo[woi % 2]
                    woi += 1
                    op("pool", lambda e, w_=w_, cg=cg: e.dma_start(out=w_.t[:], in_=w_out_r[:, :, cg * 512:(cg + 1) * 512]),
                       w=[w_], dma=True)
                    for tl in range(4):
                        pp = ps[(cg * 4 + tl) % 4]

                        def mmo(e, pp=pp, w_=w_, tl=tl):
                            for kc in range(16):
                                ins = e.matmul(pp.t[:, :], ynTg.t[:, kc, tl * 128:(tl + 1) * 128], w_.t[:, kc, :],
                                               start=(kc == 0), stop=(kc == 15))
                            return ins
                        op("pe", mmo, r=[ynTg, w_], w=[pp])
                        op("dve", lambda e, pp=pp, tl=tl, cg=cg: e.scalar_tensor_tensor(
                            out=y1[tl].t[:, cg * 512:(cg + 1) * 512], in0=xo.t[:, tl, cg * 512:(cg + 1) * 512],
                            scalar=ALPHA, in1=pp.t[:, :], op0=ALU.mult, op1=ALU.add), r=[xo, pp], w=[y1[tl]])
                for tl in range(4):
                    tt = 4 * tg + tl
                    y_ = y1[tl]
                    layer_norm_ops(y_.t[:], y_, g_bc, b_bc, stats, mv, rstd)
                    op("sp", lambda e, y_=y_, tt=tt: e.dma_start(out=h1_d.ap()[tt * 128:(tt + 1) * 128, :], in_=y_.t[:]),
                       r=[y_], w=[h1_db], dma=True, par=True)
                    hb = h1b[tt % 2]
                    op("act", lambda e, y_=y_, hb=hb: e.copy(out=hb.t[:], in_=y_.t[:]), r=[y_], w=[hb])
                    for q in range(4):
                        def trh(e, y_=y_, q=q):
                            for u in range(4):
                                kc = 4 * q + u
                                ins = e.transpose(pT.t[:, u * 128:(u + 1) * 128], y_.t[:, kc * 128:(kc + 1) * 128],
                                                  ident_f.t[:, :])
                            return ins
                        op("pe", trh, r=[y_, ident_f], w=[pT])
                        op("act", lambda e, q=q: e.copy(
                            out=h1T.t[:, 4 * q:4 * q + 4, :], in_=pT.t[:, :].rearrange("p (u t) -> p u t", u=4)),
                           r=[pT], w=[h1T])

                    def mml(e):
                        for kc in range(16):
                            ins = e.matmul(pL.t[:, 0:32], h1T.t[:, kc, :], wr.t[:, kc, :], start=(kc == 0),
                                           stop=(kc == 15))
                        return ins
                    op("pe", mml, r=[h1T, wr], w=[pL])
                    op("dve", lambda e: e.tensor_tensor(out=lg.t[:], in0=pL.t[:, 0:32], in1=br_bc.t[:], op=ALU.add),
                       r=[pL, br_bc], w=[lg])
                    op("dve", lambda e: e.max(out=mx8.t[:], in_=lg.t[:]), r=[lg], w=[mx8])
                    op("dve", lambda e: e.tensor_scalar(out=nmx.t[:], in0=mx8.t[:, 0:1], scalar1=-1.0, scalar2=None,
                                                        op0=ALU.mult), r=[mx8], w=[nmx])
                    op("dve", lambda e: e.tensor_scalar(out=msk.t[:], in0=lg.t[:], scalar1=mx8.t[:, 3:4], scalar2=None,
                                                        op0=ALU.is_ge), r=[lg, mx8], w=[msk])
                    op("act", lambda e: e.activation(out=eml.t[:], in_=lg.t[:], func=AF.Exp, bias=nmx.t[:, 0:1],
                                                     scale=1.0), r=[lg, nmx], w=[eml])
                    op("dve", lambda e: e.tensor_tensor(out=eml.t[:], in0=eml.t[:], in1=msk.t[:], op=ALU.mult),
                       r=[eml, msk], w=[eml])
                    op("dve", lambda e: e.tensor_reduce(out=esum.t[:], in_=eml.t[:], axis=AX.X, op=ALU.add),
                       r=[eml], w=[esum])
                    op("dve", lambda e: e.reciprocal(out=esum.t[:], in_=esum.t[:]), r=[esum], w=[esum])
                    op("dve", lambda e: e.tensor_scalar(out=Gd.t[:], in0=eml.t[:], scalar1=esum.t[:, 0:1], scalar2=None,
                                                        op0=ALU.mult), r=[eml, esum], w=[Gd])

                    def mmp(e):
                        e.matmul(pP.t[:, 64:96], trist_f.t[:, :], msk.t[:, :], start=True, stop=True)
                        return e.matmul(pC.t[:, 128:160], ones_f.t[:, :], msk.t[:, :], start=True, stop=True)
                    op("pe", mmp, r=[trist_f, ones_f, msk], w=[pP, pC])
                    op("dve", lambda e: e.tensor_tensor(out=pos.t[:], in0=pP.t[:, 64:96], in1=cntb.t[:], op=ALU.add),
                       r=[pP, cntb], w=[pos])
                    op("dve", lambda e: e.tensor_tensor(out=cntb.t[:], in0=pC.t[:, 128:160], in1=cntb.t[:], op=ALU.add),
                       r=[pC, cntb], w=[cntb])
                    op("dve", lambda e: e.tensor_scalar(out=ovf.t[:], in0=pos.t[:], scalar1=float(CAP), scalar2=None,
                                                        op0=ALU.is_ge), r=[pos], w=[ovf])
                    op("dve", lambda e: e.tensor_tensor(out=pos.t[:], in0=pos.t[:], in1=ecap_bc.t[:], op=ALU.add),
                       r=[pos, ecap_bc], w=[pos])
                    op("dve", lambda e: e.tensor_scalar(out=tmp32.t[:], in0=pos.t[:], scalar1=-1.0, scalar2=float(NSLOT),
                                                        op0=ALU.mult, op1=ALU.add), r=[pos], w=[tmp32])
                    op("dve", lambda e: e.tensor_tensor(out=tmp32.t[:], in0=tmp32.t[:], in1=ovf.t[:], op=ALU.mult),
                       r=[tmp32, ovf], w=[tmp32])
                    op("dve", lambda e: e.tensor_tensor(out=pos.t[:], in0=pos.t[:], in1=tmp32.t[:], op=ALU.add),
                       r=[pos, tmp32], w=[pos])
                    for k in range(4):
                        op("dve", lambda e, k=k: e.tensor_scalar(out=oh.t[:], in0=lg.t[:], scalar1=mx8.t[:, k:k + 1],
                                                                 scalar2=None, op0=ALU.is_equal), r=[lg, mx8], w=[oh])
                        op("dve", lambda e: e.tensor_tensor(out=tmp32.t[:], in0=oh.t[:], in1=pos.t[:], op=ALU.mult),
                           r=[oh, pos], w=[tmp32])
                        op("dve", lambda e, k=k: e.tensor_reduce(out=offs_f.t[:, k:k + 1], in_=tmp32.t[:], axis=AX.X,
                                                                 op=ALU.add), r=[tmp32], w=[offs_f])
                        op("dve", lambda e: e.tensor_tensor(out=tmp32.t[:], in0=oh.t[:], in1=Gd.t[:], op=ALU.mult),
                           r=[oh, Gd], w=[tmp32])
                        op("dve", lambda e, k=k, tt=tt: e.tensor_reduce(out=gk.t[:, tt, k:k + 1], in_=tmp32.t[:],
                                                                        axis=AX.X, op=ALU.add), r=[tmp32], w=[gk])
                    op("dve", lambda e, tt=tt: e.tensor_copy(out=offs_i.t[:, tt, :], in_=offs_f.t[:]),
                       r=[offs_f], w=[offs_i])
                    if debug:
                        dbs = sb("dbs%d" % tt, [128, 40], F32)
                        op("dve", lambda e, dbs=dbs: e.tensor_copy(out=dbs.t[:, 0:32], in_=lg.t[:]), r=[lg], w=[dbs])
                        op("dve", lambda e, dbs=dbs: e.tensor_copy(out=dbs.t[:, 32:36], in_=offs_f.t[:]),
                           r=[offs_f], w=[dbs])
                        op("dve", lambda e, dbs=dbs, tt=tt: e.tensor_copy(out=dbs.t[:, 36:40], in_=gk.t[:, tt, :]),
                           r=[gk], w=[dbs])
                        op("sp", lambda e, dbs=dbs, tt=tt: e.dma_start(out=dbg_r.ap()[:, tt, :], in_=dbs.t[:]),
                           r=[dbs], w=[dbg_rb], dma=True, par=True)
                    for k in range(4):
                        op("pool", lambda e, hb=hb, tt=tt, k=k: e.indirect_dma_start(
                            out=xs_d.ap()[:, :], out_offset=bass.IndirectOffsetOnAxis(ap=offs_i.t[:, tt, k:k + 1], axis=0),
                            in_=hb.t[:, :], in_offset=None),
                           r=[hb, offs_i], w=[xs_db], dma=True, par=True, key=hb.b)
            S.flush()
        st_holder[0] = gst

    def phase4():
        with ExitStack() as st:
            st_holder[0] = st
            wb = [sb("wb%d" % i, [128, 16, 512], BF16) for i in range(4)]
            xs_tm = [sb("xs_tm%d" % i, [128, NTC, D], BF16) for i in range(1)]
            xsT = sb("xsT", [128, 16, CAP], BF16)
            gaT = sb("gaT", [128, 16, CAP], BF16)
            actT = sb("actT", [128, 16, CAP], BF16)
            g32 = [sb("g32_%d" % i, [128, CAP], F32) for i in range(2)]
            sg32 = [sb("sg32_%d" % i, [128, CAP], F32) for i in range(2)]
            u32 = [sb("u32_%d" % i, [128, CAP], F32) for i in range(2)]
            ys_sb = [sb("ys_sb%d" % i, [128, 512], F32) for i in range(3)]
            bup_s = sb("bup_s", [128, NEXP, 32], F32)
            bd = [sb("bd%d" % i, [1, D], BF16) for i in range(2)]
            op("sp", lambda e: e.dma_start(out=bup_s.t[:], in_=bupT.ap()), w=[bup_s], dma=True)
            zrow = sb("zrow", [1, D], F32)
            op("pool", lambda e: e.memset(zrow.t[:], 0.0), w=[zrow])
            op("sp", lambda e: e.dma_start(out=ys_d.ap()[NSLOT:NSLOT + 1, :], in_=zrow.t[:]), r=[zrow], w=[ys_db],
               dma=True, par=True)
            wbi = 0
            upi = 0
            dni = 0
            for ex_ in range(NEXP):
                xt = xs_tm[0]
                bd_ = bd[ex_ % 2]
                r0 = ex_ * CAP
                op("sp", lambda e, xt=xt, r0=r0: e.dma_start(
                    out=xt.t[:], in_=xs_d.ap()[r0:r0 + CAP, :].rearrange("(t p) d -> p t d", p=128)),
                   r=[xs_db], w=[xt], dma=True)
                op("pool", lambda e, bd_=bd_, ex_=ex_: e.dma_start(out=bd_.t[:], in_=b_down.ap()[ex_:ex_ + 1, :]),
                   w=[bd_], dma=True)
                for tl in range(NTC):
                    for q in range(2):
                        pt = ps[6 + q]
                        ptb = psb16[6 + q]

                        def trx(e, ptb=ptb, xt=xt, tl=tl, q=q):
                            for u in range(8):
                                kc = 8 * q + u
                                ins = e.transpose(ptb[:, u * 128:(u + 1) * 128], xt.t[:, tl, kc * 128:(kc + 1) * 128],
                                                  ident_b.t[:, :])
                            return ins
                        op("pe", trx, r=[xt, ident_b], w=[pt])
                        op("act", lambda e, ptb=ptb, tl=tl, q=q: e.copy(
                            out=xsT.t[:, 8 * q:8 * q + 8, tl * 128:(tl + 1) * 128],
                            in_=ptb[:, 0:1024].rearrange("p (u t) -> p u t", u=8)), r=[pt], w=[xsT])
                w_up_r = w_up.ap()[ex_].rearrange("(kc p) c -> p kc c", p=128)
                for cg in range(8):
                    w_ = wb[wbi % 4]
                    wbi += 1
                    op("pool", lambda e, w_=w_, w_up_r=w_up_r, cg=cg: e.dma_start(
                        out=w_.t[:], in_=w_up_r[:, :, cg * 512:(cg + 1) * 512]), w=[w_], dma=True)
                    for cc in range(4):
                        ch = 4 * cg + cc
                        pp = ps[upi % 3]
                        upi += 1

                        def mmup(e, pp=pp, w_=w_, cc=cc):
                            for kc in range(16):
                                ins = e.matmul(pp.t[:, 0:CAP], w_.t[:, kc, cc * 128:(cc + 1) * 128], xsT.t[:, kc, :],
                                               start=(kc == 0), stop=(kc == 15))
                            return ins
                        op("pe", mmup, r=[w_, xsT], w=[pp])
                        if ch < 16:
                            g_ = g32[ch % 2]
                            s_ = sg32[ch % 2]
                            op("dve", lambda e, pp=pp, g_=g_, ex_=ex_, ch=ch: e.tensor_scalar(
                                out=g_.t[:], in0=pp.t[:, 0:CAP], scalar1=bup_s.t[:, ex_, ch:ch + 1], scalar2=7.0,
                                op0=ALU.add, op1=ALU.min), r=[pp, bup_s], w=[g_])
                            op("act", lambda e, g_=g_, s_=s_: e.activation(out=s_.t[:], in_=g_.t[:], func=AF.Sigmoid,
                                                                          scale=1.702), r=[g_], w=[s_])
                            op("pool", lambda e, g_=g_, s_=s_, ch=ch: e.tensor_tensor(
                                out=gaT.t[:, ch, :], in0=g_.t[:], in1=s_.t[:], op=ALU.mult), r=[g_, s_], w=[gaT])
                        else:
                            c2 = ch - 16
                            u_ = u32[ch % 2]
                            op("dve", lambda e, pp=pp, u_=u_, ex_=ex_, ch=ch: e.tensor_scalar(
                                out=u_.t[:], in0=pp.t[:, 0:CAP], scalar1=bup_s.t[:, ex_, ch:ch + 1], scalar2=7.0,
                                op0=ALU.add, op1=ALU.min), r=[pp, bup_s], w=[u_])
                            op("dve", lambda e, u_=u_: e.tensor_scalar(
                                out=u_.t[:], in0=u_.t[:], scalar1=-7.0, scalar2=1.0, op0=ALU.max, op1=ALU.add),
                               r=[u_], w=[u_])
                            op("pool", lambda e, u_=u_, c2=c2: e.tensor_tensor(
                                out=actT.t[:, c2, :], in0=u_.t[:], in1=gaT.t[:, c2, :], op=ALU.mult),
                               r=[u_, gaT], w=[actT])
                w_dn_r = w_down.ap()[ex_].rearrange("(kc p) c -> p kc c", p=128)
                for cg in range(4):
                    w_ = wb[wbi % 4]
                    wbi += 1
                    op("pool", lambda e, w_=w_, w_dn_r=w_dn_r, cg=cg: e.dma_start(
                        out=w_.t[:], in_=w_dn_r[:, :, cg * 512:(cg + 1) * 512]), w=[w_], dma=True)
                    for tl in range(NTC):
                        pp = ps[3 + dni % 3]
                        ysb = ys_sb[dni % 3]
                        dni += 1

                        def mmdn(e, pp=pp, w_=w_, tl=tl, bd_=bd_, cg=cg):
                            for kc in range(16):
                                e.matmul(pp.t[:, :], actT.t[:, kc, tl * 128:(tl + 1) * 128], w_.t[:, kc, :],
                                         start=(kc == 0), stop=False)
                            return e.matmul(pp.t[:, :], ones_b.t[0:1, :], bd_.t[0:1, cg * 512:(cg + 1) * 512],
                                            start=False, stop=True)
                        op("pe", mmdn, r=[actT, w_, bd_, ones_b], w=[pp])
                        op("act", lambda e, pp=pp, ysb=ysb: e.copy(out=ysb.t[:, :], in_=pp.t[:, :]), r=[pp], w=[ysb])
                        op("sp", lambda e, r0=r0, tl=tl, cg=cg, ysb=ysb: e.dma_start(
                            out=ys_d.ap()[r0 + tl * 128:r0 + (tl + 1) * 128, cg * 512:(cg + 1) * 512], in_=ysb.t[:, :]),
                           r=[ysb], w=[ys_db], dma=True, par=True)
            S.flush()
        st_holder[0] = gst

    def phase5():
        with ExitStack() as st:
            st_holder[0] = st
            h1t = [sb("h1t%d" % i, [128, D], F32) for i in range(2)]
            yk = [sb("yk%d" % i, [128, D], F32) for i in range(4)]
            g_bc = sb("g2_bc", [128, D], F32)
            b_bc = sb("b2_bc", [128, D], F32)
            stats = sb("stats2", [128, 4, 6], F32)
            mv = sb("mv2", [128, 2], F32)
            rstd = sb("rstd2", [128, 1], F32)
            op("sp", lambda e: e.dma_start(out=g_bc.t[:], in_=bcast(ln2_g, D)), w=[g_bc], dma=True)
            op("sp", lambda e: e.dma_start(out=b_bc.t[:], in_=bcast(ln2_b, D)), w=[b_bc], dma=True)
            yi = 0
            for tt in range(16):
                a_ = h1t[tt % 2]
                op("sp", lambda e, a_=a_, tt=tt: e.dma_start(out=a_.t[:], in_=h1_d.ap()[tt * 128:(tt + 1) * 128, :]),
                   r=[h1_db], w=[a_], dma=True)
                op("act", lambda e, a_=a_: e.activation(out=a_.t[:], in_=a_.t[:], func=AF.Copy, scale=ALPHA),
                   r=[a_], w=[a_])
                for k in range(4):
                    y_ = yk[yi % 4]
                    yi += 1
                    op("pool", lambda e, y_=y_, tt=tt, k=k: e.indirect_dma_start(
                        out=y_.t[:, :], out_offset=None, in_=ys_d.ap()[:, :],
                        in_offset=bass.IndirectOffsetOnAxis(ap=offs_i.t[:, tt, k:k + 1], axis=0)),
                       r=[ys_db, offs_i], w=[y_], dma=True)
                    op("dve", lambda e, y_=y_, a_=a_, tt=tt, k=k: e.scalar_tensor_tensor(
                        out=a_.t[:], in0=y_.t[:], scalar=gk.t[:, tt, k:k + 1], in1=a_.t[:], op0=ALU.mult, op1=ALU.add),
                       r=[y_, gk, a_], w=[a_])
                layer_norm_ops(a_.t[:], a_, g_bc, b_bc, stats, mv, rstd)
                op("sp", lambda e, a_=a_, tt=tt: e.dma_start(out=out_d.ap()[tt * 128:(tt + 1) * 128, :], in_=a_.t[:]),
                   r=[a_], w=[out_db[tt]], dma=True)
            S.flush()
        st_holder[0] = gst

    if ph_stop >= 1:
        phase1()
    if ph_stop >= 2:
        phase2()
    if ph_stop >= 3:
        phase3()
    if ph_stop >= 4:
        phase4()
    if ph_stop >= 5:
        phase5()
    if S.ops:
        S.flush()
    gst.close()
    return nc


def _t5_bucket(dist):
    dist = np.asarray(dist, np.int32)
    max_exact = 16
    d_f = np.maximum(dist, 1).astype(np.float32)
    large = max_exact + (np.log(d_f / np.float32(max_exact)) / np.float32(math.log(2048 / max_exact))
                         * np.float32(32 - max_exact)).astype(np.int32)
    large = np.minimum(large, 31)
    return np.where(dist < max_exact, dist, large)


def _attn_bias(rel_bias):
    out = np.full((128, 3, 16, 256), NEGB, np.float32)
    ki = np.arange(128)[:, None]
    qi = np.arange(128)[None, :]
    for g, d in enumerate(DILS):
        jc = qi - ki
        jp = qi + 128 - ki
        bc = _t5_bucket(np.maximum(jc, 0) * d)
        bp = _t5_bucket(np.minimum(np.maximum(jp, 0), 128) * d)
        for h in range(16):
            cur = np.where(jc >= 0, rel_bias[bc, h], np.float32(NEGB))
            prv = np.where(jp <= 128, rel_bias[bp, h], np.float32(NEGB))
            out[:, g, h, 0:128] = cur
            out[:, g, h, 128:256] = prv
    return out.reshape(128, 3 * 16 * 256)


def make_in_maps(inputs, ph_stop=5, cores=range(NCORES)):
    x = np.asarray(inputs["x"], np.float32)
    f = lambda k: np.ascontiguousarray(np.asarray(inputs[k], np.float32)[0])
    w_in = f("w_in")
    common = {
        "w_in": w_in,
        "bgate": np.concatenate([f("b_igate"), f("b_fgate")])[None, :].astype(np.float32),
        "cw": np.ascontiguousarray(f("conv_w").reshape(4, 16, 128).transpose(2, 1, 0)),
        "cb": np.ascontiguousarray(f("conv_b").reshape(16, 128).T),
        "battn": _attn_bias(np.asarray(inputs["rel_bias"], np.float32)),
        "beta": np.concatenate([f("beta_attn"), f("beta_mlstm")])[None, :],
        "ident": np.eye(128, dtype=np.float32),
        "triu": np.triu(np.ones((128, 128), np.float32)),
        "trist": np.triu(np.ones((128, 128), np.float32), 1),
    }
    if ph_stop >= 3:
        common.update({
            "w_out": f("w_out"), "ln1_g": f("ln1_g")[None, :], "ln1_b": f("ln1_b")[None, :],
            "w_router": f("w_router"), "b_router": f("b_router")[None, :],
            "ecap": (np.arange(NEXP, dtype=np.float32) * CAP)[None, :],
        })
    if ph_stop >= 4:
        common.update({
            "w_up": f("w_up"), "w_down": f("w_down"), "b_down": f("b_down"),
            "bupT": np.ascontiguousarray(f("b_up").reshape(NEXP, 32, 128).transpose(2, 0, 1)),
        })
    if ph_stop >= 5:
        common.update({"ln2_g": f("ln2_g")[None, :], "ln2_b": f("ln2_b")[None, :]})
    maps = []
    for c in cores:
        b, j = c // 4, c % 4
        nvalid = 2048 * (j + 1)
        xT = np.zeros((D, NSL), np.float32)
        xT[:, NSL - nvalid:] = x[b, :nvalid, :].T
        vs = np.zeros(NSL, np.float32)
        vs[NSL - nvalid:] = 1.0
        m = dict(common)
        m["xT"] = xT
        m["valid"] = np.ascontiguousarray(vs.reshape(64, 128).T)
        m["hv"] = np.full((128, 1), 1.0 if j > 0 else 0.0, np.float32)
        if ph_stop >= 3:
            m["x_own"] = np.ascontiguousarray(x[b, 2048 * j:2048 * (j + 1), :])
        maps.append(m)
    return maps


_NC_CACHE = {}


def kernel(**inputs):
    if "nc" not in _NC_CACHE:
        _NC_CACHE["nc"] = build(5, False)
    nc = _NC_CACHE["nc"]
    maps = make_in_maps(inputs, 5)
    res = run_bass_kernel_spmd(nc, maps, core_ids=list(range(NCORES)))
    out = np.zeros((2, 8192, D), np.float32)
    for c in range(NCORES):
        b, j = c // 4, c % 4
        out[b, 2048 * j:2048 * (j + 1), :] = res.results[c]["out"]
    return out
```

```python
import math
from contextlib import ExitStack

import numpy as np
import concourse.bass as bass
import concourse.mybir as mybir
from concourse.bass_utils import run_bass_kernel_spmd

F32 = mybir.dt.float32
BF16 = mybir.dt.bfloat16
I32 = mybir.dt.int32
AF = mybir.ActivationFunctionType
ALU = mybir.AluOpType
AX = mybir.AxisListType

NCORES = 8
D = 2048
NSL = 8192
OWN0 = 6144
HAL0 = 4096
CAP = 512
NTC = CAP // 128
NEXP = 32
NSLOT = NEXP * CAP
ALPHA = 2.0 ** 0.25
NEGB = -80.0
LN_EPS = 1e-5
HN_EPS = 1e-6
IN_COLS = 7176
DILS = (1, 4, 16)
SAME_ENGINE_SYNC = True
DBG = {}


class Buf:
    __slots__ = ("name", "lw", "rd", "excl")

    def __init__(self, name):
        self.name = name
        self.lw = None
        self.rd = []
        self.excl = False


class DBuf(Buf):
    pass


class Op:
    __slots__ = ("eng", "fn", "deps", "dma", "sig", "idx", "key", "dsem", "dval", "xw", "done")


class Sched:
    ENGS = ["pe", "act", "dve", "pool", "sp"]
    EP = 20000

    def __init__(self, nc, stack):
        self.nc = nc
        self.stack = stack
        self.ops = []
        self.cnt = {e: 0 for e in self.ENGS}
        self.esems = {e: [] for e in self.ENGS}
        self.dsem_pool = []
        self.dsem_free = []
        self.keymap = {}
        self.waited = {e: {} for e in self.ENGS}

    def _dsem(self, key):
        if key not in self.keymap:
            if not self.dsem_free:
                sem = self.stack.enter_context(self.nc.semaphore("dq%d" % len(self.dsem_pool)))
                self.dsem_pool.append([sem, 0])
                self.dsem_free.append(len(self.dsem_pool) - 1)
            self.keymap[key] = self.dsem_free.pop()
        return self.keymap[key]

    def add(self, eng, fn, reads=(), writes=(), dma=False, key=None, par=False):
        op = Op()
        op.eng, op.fn, op.dma, op.sig, op.deps, op.xw, op.done = eng, fn, dma, False, [], None, False
        for b in reads:
            if b.lw is not None and not b.lw.done:
                op.deps.append((b.lw, "raw"))
            if b.excl:
                for r in b.rd:
                    if r.eng != eng and not r.done:
                        op.deps.append((r, "rar"))
        for b in writes:
            if b.lw is not None and not b.lw.done and not (par and b.lw.dma and dma):
                op.deps.append((b.lw, "waw"))
            for r in b.rd:
                if r is not op and not r.done:
                    op.deps.append((r, "war"))
        for b in reads:
            b.rd.append(op)
        for b in writes:
            b.lw = op
            b.rd = []
        if dma:
            if key is None:
                key = writes[0] if (writes and not isinstance(writes[0], DBuf)) else reads[0]
            si = self._dsem(key)
            self.dsem_pool[si][1] += 16
            op.dsem = self.dsem_pool[si][0]
            op.dval = self.dsem_pool[si][1]
        self.ops.append(op)
        return op

    def flush(self):
        nc = self.nc
        last = {}
        for op in self.ops:
            if not op.dma and op.fn is not None:
                last[op.eng] = op
        dm = [(p[0], p[1]) for p in self.dsem_pool if p[1] > 0]
        for e in self.ENGS:
            op = Op()
            op.eng, op.fn, op.dma, op.sig, op.done = e, None, False, False, False
            op.deps = [(o, "bar") for o in last.values()]
            op.xw = dm
            self.ops.append(op)
        for op in self.ops:
            nd = []
            for d, kind in op.deps:
                if d.dma:
                    nd.append(d)
                    continue
                if d.eng == op.eng and not op.dma:
                    if kind == "bar" or op.eng == "pe" or kind == "war" or not SAME_ENGINE_SYNC:
                        continue
                d.sig = True
                nd.append(d)
            op.deps = nd
        EP = self.EP
        for op in self.ops:
            if op.sig:
                self.cnt[op.eng] += 1
                op.idx = self.cnt[op.eng]
        for e in self.ENGS:
            need = (self.cnt[e] + EP - 1) // EP
            while len(self.esems[e]) < max(need, 1):
                self.esems[e].append(self.stack.enter_context(nc.semaphore("e_%s%d" % (e, len(self.esems[e])))))
        ops = self.ops

        def gen(ename):
            def body(e):
                waited = self.waited[ename]
                for op in ops:
                    if op.eng != ename:
                        continue
                    need = {}
                    for d in op.deps:
                        if d.dma:
                            sem, val = d.dsem, d.dval
                        else:
                            sem = self.esems[d.eng][(d.idx - 1) // EP]
                            val = (d.idx - 1) % EP + 1
                        if need.get(sem, (None, 0))[1] < val:
                            need[sem] = (sem, val)
                    if op.xw:
                        for sem, c in op.xw:
                            if need.get(sem, (None, 0))[1] < c:
                                need[sem] = (sem, c)
                    for sem, val in need.values():
                        if waited.get(sem, 0) >= val:
                            continue
                        e.wait_ge(sem, val)
                        waited[sem] = val
                    if op.fn is None:
                        continue
                    ins = op.fn(e)
                    if op.dma:
                        ins.then_inc(op.dsem, 16)
                    elif op.sig:
                        ins.then_inc(self.esems[ename][(op.idx - 1) // EP], 1)
            return body

        with nc.Block() as block:
            block.tensor(gen("pe"))
            block.scalar(gen("act"))
            block.vector(gen("dve"))
            block.gpsimd(gen("pool"))
            block.sync(gen("sp"))
        for op in self.ops:
            op.done = True
        self.ops = []
        self.keymap = {}
        self.dsem_free = list(range(len(self.dsem_pool)))


class TB:
    def __init__(self, t, name):
        self.t = t
        self.b = Buf(name)


class Ctx:
    pass


def _bs(xs):
    return [x.b if isinstance(x, TB) else x for x in xs]


def build(ph_stop=5, debug=False, cut=99):
    nc = bass.Bass("TRN2", target_bir_lowering=False)
    gst = ExitStack()
    S = Sched(nc, gst)

    def din(name, shape, dt=F32):
        return nc.dram_tensor(name, shape, dt, kind="ExternalInput")

    def dscr(name, shape, dt):
        return nc.dram_tensor(name, shape, dt, kind="ExternalOutput" if debug else "Internal")

    def op(eng, fn, r=(), w=(), **kw):
        return S.add(eng, fn, _bs(r), _bs(w), **kw)

    st_holder = [gst]

    def sb(name, shape, dt):
        return TB(st_holder[0].enter_context(nc.sbuf_tensor(name, shape, dt)), name)

    xT = din("xT", [D, NSL])
    valid = din("valid", [128, 64])
    hv = din("hv", [128, 1])
    w_in = din("w_in", [D, IN_COLS])
    bgate = din("bgate", [1, 8])
    cw = din("cw", [128, 16, 4])
    cbias = din("cb", [128, 16])
    battn = din("battn", [128, 3 * 16 * 256])
    beta = din("beta", [1, D])
    ident = din("ident", [128, 128])
    triu = din("triu", [128, 128])
    trist = din("trist", [128, 128])
    ynT_d = dscr("ynT_d", [16, 128, 2048], BF16)
    v_d = nc.dram_tensor("v_d", [4096, 260], BF16, kind="Internal")
    ynT_db = DBuf("ynT_d")
    v_db = DBuf("v_d")
    if ph_stop >= 3:
        x_own = din("x_own", [2048, D])
        w_out = din("w_out", [D, D])
        ln1_g = din("ln1_g", [1, D])
        ln1_b = din("ln1_b", [1, D])
        w_router = din("w_router", [D, NEXP])
        b_router = din("b_router", [1, NEXP])
        ecap = din("ecap", [1, NEXP])
        h1_d = dscr("h1_d", [2048, D], F32)
        xs_d = nc.dram_tensor("xs_d", [NSLOT + 1, D], BF16, kind="Internal")
        h1_db = DBuf("h1_d")
        xs_db = DBuf("xs_d")
        if debug:
            dbg_r = nc.dram_tensor("dbg_r", [128, 16, 40], F32, kind="ExternalOutput")
            dbg_rb = DBuf("dbg_r")
    if ph_stop >= 4:
        w_up = din("w_up", [NEXP, D, 2 * D])
        bupT = din("bupT", [128, NEXP, 32])
        w_down = din("w_down", [NEXP, D, D])
        b_down = din("b_down", [NEXP, D])
        ys_d = nc.dram_tensor("ys_d", [NSLOT + 1, D], F32, kind="Internal")
        ys_db = DBuf("ys_d")
    if ph_stop >= 5:
        ln2_g = din("ln2_g", [1, D])
        ln2_b = din("ln2_b", [1, D])
        out_d = nc.dram_tensor("out", [2048, D], F32, kind="ExternalOutput")
        out_db = [DBuf("out%d" % i) for i in range(16)]

    xT_r = xT.ap().rearrange("(kc p) s -> p kc s", p=128)
    w_in_r = w_in.ap().rearrange("(kc p) c -> p kc c", p=128)

    def bcast(dt_, n):
        return bass.AP(dt_, 0, [[0, 128], [1, n]])

    ps = []
    for i in range(8):
        t = gst.enter_context(nc.psum_tensor("psb%d" % i, [128, 512], F32))
        ps.append(TB(t, "psb%d" % i))
        ps[-1].b.excl = True
    psb16 = [p.t.bitcast(BF16) for p in ps]

    ident_f = sb("ident_f", [128, 128], F32)
    ident_b = sb("ident_b", [128, 128], BF16)
    triu_f = sb("triu_f", [128, 128], F32)
    trist_f = sb("trist_f", [128, 128], F32)
    ones_f = sb("ones_f", [128, 128], F32)
    ones_b = sb("ones_b", [1, 128], BF16)
    hv_s = sb("hv_s", [128, 1], F32)
    valid_s = sb("valid_s", [128, 64], F32)
    beta_bc = sb("beta_bc", [128, D], F32)
    cw_s = sb("cw_s", [128, 16, 4], F32)
    cb_s = sb("cb_s", [128, 16], F32)
    bg_bc = sb("bg_bc", [128, 8], F32)
    offs_i = sb("offs_i", [128, 16, 4], I32)
    gk = sb("gk", [128, 16, 4], F32)

    op("sp", lambda e: e.dma_start(out=ident_f.t[:], in_=ident.ap()), w=[ident_f], dma=True)
    op("sp", lambda e: e.dma_start(out=triu_f.t[:], in_=triu.ap()), w=[triu_f], dma=True)
    op("sp", lambda e: e.dma_start(out=trist_f.t[:], in_=trist.ap()), w=[trist_f], dma=True)
    op("sp", lambda e: e.dma_start(out=hv_s.t[:], in_=hv.ap()), w=[hv_s], dma=True)
    op("sp", lambda e: e.dma_start(out=valid_s.t[:], in_=valid.ap()), w=[valid_s], dma=True)
    op("sp", lambda e: e.dma_start(out=beta_bc.t[:], in_=bcast(beta, D)), w=[beta_bc], dma=True)
    op("sp", lambda e: e.dma_start(out=cw_s.t[:], in_=cw.ap()), w=[cw_s], dma=True)
    op("sp", lambda e: e.dma_start(out=cb_s.t[:], in_=cbias.ap()), w=[cb_s], dma=True)
    op("sp", lambda e: e.dma_start(out=bg_bc.t[:], in_=bcast(bgate, 8)), w=[bg_bc], dma=True)
    op("dve", lambda e: e.tensor_copy(out=ident_b.t[:], in_=ident_f.t[:]), r=[ident_f], w=[ident_b])
    op("pool", lambda e: e.memset(ones_f.t[:], 1.0), w=[ones_f])
    op("pool", lambda e: e.memset(ones_b.t[:], 1.0), w=[ones_b])

    def phase1():
        with ExitStack() as st:
            st_holder[0] = st
            expB = sb("expB", [128, 3 * 16 * 256], BF16)
            tmpBs = [sb("tmpB%d" % i, [128, 1024], F32) for i in range(2)]
            for g in range(12):
                tmpB = tmpBs[g % 2]
                op("sp", lambda e, g=g, tmpB=tmpB: e.dma_start(out=tmpB.t[:], in_=battn.ap()[:, g * 1024:(g + 1) * 1024]),
                   w=[tmpB], dma=True)
                op("act", lambda e, g=g, tmpB=tmpB: e.activation(out=expB.t[:, g * 1024:(g + 1) * 1024], in_=tmpB.t[:],
                                                      func=AF.Exp), r=[tmpB], w=[expB])
            Wq = sb("Wq", [128, 16, 256], BF16)
            Wk = sb("Wk", [128, 16, 256], BF16)
            Wv = sb("Wv", [128, 16, 256], BF16)
            kT = sb("kT", [128, 2, 4096], BF16)
            qT = sb("qT", [128, 2, 2048], BF16)
            xg = [sb("xg%d" % i, [128, 16, 512], BF16) for i in range(2)]
            vt = [sb("vt%d" % i, [128, 4, 4, 65], BF16) for i in range(2)]
            NBLK = 17 + 20 + 32
            vaug = sb("vaug", [128, NBLK, 4, 65], BF16)
            acc = sb("acc", [128, 4, 2048], F32)
            E = [sb("E%d" % i, [128, 256], BF16) for i in range(3)]
            PT = [sb("PT%d" % i, [128, 256], BF16) for i in range(3)]
            accB = [Buf("accP0"), Buf("accP1")]
            dn = sb("dn", [128, 4], F32)
            d2 = sb("d2", [128, 4], F32)
            sq = sb("sq", [128, 4, 65], F32)
            ssq = sb("ssq", [128, 4], F32)
            sc = sb("sc", [128, 4], F32)
            yn_tm = [sb("yn_tm%d" % i, [128, 256], BF16) for i in range(2)]
            ynTs = sb("ynTs", [128, 2, 1024], BF16)

            for v_ in vt:
                op("pool", lambda e, v_=v_: e.memset(v_.t[:], 1.0), w=[v_])
            blk = {}
            nb = 0
            for g, d in enumerate(DILS):
                first = 16 // d
                for r in range(d):
                    for mb in range(first - 1, 2 * first):
                        blk[(g, r, mb)] = nb
                        nb += 1
            assert nb == NBLK

            for hg in range(4):
                if cut <= 0:
                    break
                cq, ck, cv = 256 * hg, 1024 + 256 * hg, 2048 + 256 * hg
                op("pool", lambda e, c0=cq: e.dma_start(out=Wq.t[:], in_=w_in_r[:, :, c0:c0 + 256]), w=[Wq], dma=True)
                op("pool", lambda e, c0=ck: e.dma_start(out=Wk.t[:], in_=w_in_r[:, :, c0:c0 + 256]), w=[Wk], dma=True)
                op("pool", lambda e, c0=cv: e.dma_start(out=Wv.t[:], in_=w_in_r[:, :, c0:c0 + 256]), w=[Wv], dma=True)
                op("dve", lambda e: e.memset(acc.t[:], 0.0), w=[acc] + accB)
                for sg in range(8):
                    s0 = HAL0 + 512 * sg
                    x_ = xg[sg % 2]
                    op("pool", lambda e, x_=x_, s0=s0: e.dma_start(out=x_.t[:], in_=xT_r[:, :, s0:s0 + 512]),
                       w=[x_], dma=True)
                    for pi in range(2):
                        pk = ps[pi]

                        def mmk(e, pk=pk, x_=x_, pi=pi):
                            for kc in range(16):
                                ins = e.matmul(pk.t[:, :], Wk.t[:, kc, pi * 128:(pi + 1) * 128], x_.t[:, kc, :],
                                               start=(kc == 0), stop=(kc == 15))
                            return ins
                        op("pe", mmk, r=[Wk, x_], w=[pk])
                        op("act", lambda e, pk=pk, pi=pi, sg=sg: e.copy(out=kT.t[:, pi, sg * 512:(sg + 1) * 512],
                                                                       in_=pk.t[:, :]), r=[pk], w=[kT])
                    if sg >= 4:
                        for pi in range(2):
                            pq = ps[2 + pi]

                            def mmq(e, pq=pq, x_=x_, pi=pi):
                                for kc in range(16):
                                    ins = e.matmul(pq.t[:, :], Wq.t[:, kc, pi * 128:(pi + 1) * 128], x_.t[:, kc, :],
                                                   start=(kc == 0), stop=(kc == 15))
                                return ins
                            op("pe", mmq, r=[Wq, x_], w=[pq])
                            op("act", lambda e, pq=pq, pi=pi, sg=sg: e.activation(
                                out=qT.t[:, pi, (sg - 4) * 512:(sg - 3) * 512], in_=pq.t[:, :], func=AF.Copy,
                                scale=0.125), r=[pq], w=[qT])
                    v_ = vt[sg % 2]
                    for tl in range(4):
                        pv = ps[4 + tl % 2]

                        def mmv(e, pv=pv, x_=x_, tl=tl):
                            for kc in range(16):
                                ins = e.matmul(pv.t[:, 0:256], x_.t[:, kc, tl * 128:(tl + 1) * 128], Wv.t[:, kc, :],
                                               start=(kc == 0), stop=(kc == 15))
                            return ins
                        op("pe", mmv, r=[Wv, x_], w=[pv])
                        op("dve", lambda e, pv=pv, v_=v_, tl=tl: e.tensor_copy(
                            out=v_.t[:, tl, :, 0:64], in_=pv.t[:, 0:256].rearrange("p (h c) -> p h c", h=4)),
                           r=[pv], w=[v_])
                    u0 = 512 * sg
                    op("sp", lambda e, v_=v_, u0=u0: e.dma_start(
                        out=v_d.ap()[u0:u0 + 512, :].rearrange("(t p) c -> p t c", p=128),
                        in_=v_.t[:].rearrange("p t h c -> p t (h c)")),
                       r=[v_], w=[v_db], dma=True)
                if cut <= 1:
                    continue
                for g, d in enumerate(DILS):
                    first = 16 // d
                    nblk = first + 1
                    src = v_d.ap().rearrange("(mb p r) c -> r p mb c", p=128, r=d)
                    for r in range(d):
                        b0 = blk[(g, r, first - 1)]
                        op("sp", lambda e, src=src, r=r, b0=b0, nblk=nblk, first=first: e.dma_start(
                            out=vaug.t[:, b0:b0 + nblk].rearrange("p b h c -> p b (h c)"), in_=src[r][:, first - 1:2 * first]),
                           r=[v_db], w=[vaug], dma=True, par=True)
                if cut <= 2:
                    continue
                it = 0
                for hl in range(4):
                    pi, p0 = hl // 2, 64 * (hl % 2)
                    h = 4 * hg + hl
                    for g, d in enumerate(DILS):
                        first = 16 // d
                        for r in range(d):
                            for mb in range(first - 1, 2 * first):
                                halo = (mb == first - 1)
                                last = (mb == 2 * first - 1)
                                if halo:
                                    qb0, nq, bc0 = mb + 1, 1, 128
                                elif last:
                                    qb0, nq, bc0 = mb, 1, 0
                                else:
                                    qb0, nq, bc0 = mb, 2, 0
                                N = 128 * nq
                                ks = r + d * 128 * mb
                                ksl = slice(ks, ks + d * 127 + 1, d)
                                qs = r + d * 128 * qb0 - 2048
                                qsl = slice(qs, qs + d * (N - 1) + 1, d)
                                bcol = (g * 16 + h) * 256 + bc0
                                bi = blk[(g, r, mb)]
                                pS = ps[it % 3]
                                pO = ps[3 + it % 3]
                                E_ = E[it % 3]
                                P_ = PT[it % 3]
                                it += 1
                                op("pe", lambda e, pS=pS, ksl=ksl, qsl=qsl, pi=pi, p0=p0, N=N: e.matmul(
                                    pS.t[:, 0:N], kT.t[p0:p0 + 64, pi, ksl], qT.t[p0:p0 + 64, pi, qsl],
                                    start=True, stop=True), r=[kT, qT], w=[pS])
                                op("act", lambda e, pS=pS, E_=E_, N=N: e.activation(
                                    out=E_.t[:, 0:N], in_=pS.t[:, 0:N], func=AF.Exp), r=[pS], w=[E_])
                                if halo:
                                    op("dve", lambda e, E_=E_, P_=P_, N=N, bcol=bcol: e.scalar_tensor_tensor(
                                        out=P_.t[:, 0:N], in0=E_.t[:, 0:N], scalar=hv_s.t[:, 0:1],
                                        in1=expB.t[:, bcol:bcol + N], op0=ALU.mult, op1=ALU.mult),
                                       r=[E_, expB, hv_s], w=[P_])
                                else:
                                    op("dve", lambda e, E_=E_, P_=P_, N=N, bcol=bcol: e.tensor_tensor(
                                        out=P_.t[:, 0:N], in0=E_.t[:, 0:N], in1=expB.t[:, bcol:bcol + N],
                                        op=ALU.mult), r=[E_, expB], w=[P_])
                                op("pe", lambda e, pO=pO, P_=P_, bi=bi, hl=hl, N=N: e.matmul(
                                    pO.t[0:65, 0:N], vaug.t[:, bi, hl, 0:65], P_.t[:, 0:N], start=True, stop=True),
                                   r=[vaug, P_], w=[pO])
                                op("dve", lambda e, pO=pO, hl=hl, qsl=qsl, N=N: e.tensor_tensor(
                                    out=acc.t[0:65, hl, qsl], in0=acc.t[0:65, hl, qsl], in1=pO.t[0:65, 0:N],
                                    op=ALU.add), r=[pO, acc], w=[acc])
                if cut <= 3:
                    continue
                if cut <= 3:
                    continue
                pF = ps[6]
                pFv = pF.t[:, 0:512].rearrange("p (h c) -> p h c", h=4)
                pY = ps[7]
                pYb = psb16[7]
                for t in range(16):
                    def trF(e, t=t):
                        for hl in range(4):
                            ins = e.transpose(pF.t[:, hl * 128:(hl + 1) * 128], acc.t[:, hl, t * 128:(t + 1) * 128],
                                              ident_f.t[:, :])
                        return ins
                    op("pe", trF, r=[acc, ident_f] + accB, w=[pF])
                    if cut <= 4:
                        continue
                    op("act", lambda e: e.activation(out=sq.t[:], in_=pFv[:, :, 0:65],
                                                     func=AF.Square), r=[pF], w=[sq])
                    op("dve", lambda e: e.tensor_copy(out=dn.t[:], in_=pFv[:, :, 64]), r=[pF], w=[dn])
                    op("dve", lambda e: e.tensor_tensor(out=d2.t[:], in0=dn.t[:], in1=dn.t[:], op=ALU.mult),
                       r=[dn], w=[d2])
                    op("dve", lambda e: e.tensor_reduce(out=ssq.t[:], in_=sq.t[:, :, 0:64], axis=AX.X, op=ALU.add),
                       r=[sq], w=[ssq])
                    op("dve", lambda e: e.scalar_tensor_tensor(out=sc.t[:], in0=d2.t[:], scalar=64.0 * HN_EPS,
                                                               in1=ssq.t[:], op0=ALU.mult, op1=ALU.add),
                       r=[d2, ssq], w=[sc])
                    op("act", lambda e: e.activation(out=sc.t[:], in_=sc.t[:], func=AF.Sqrt, scale=1.0 / 64.0),
                       r=[sc], w=[sc])
                    op("dve", lambda e: e.reciprocal(out=sc.t[:], in_=sc.t[:]), r=[sc], w=[sc])
                    if cut <= 5:
                        continue
                    y_ = yn_tm[t % 2]
                    for hl in range(4):
                        hh = 4 * hg + hl
                        op("dve", lambda e, y_=y_, hl=hl, hh=hh: e.scalar_tensor_tensor(
                            out=y_.t[:, hl * 64:(hl + 1) * 64], in0=pFv[:, hl, 0:64], scalar=sc.t[:, hl:hl + 1],
                            in1=beta_bc.t[:, hh * 64:(hh + 1) * 64], op0=ALU.mult, op1=ALU.mult),
                           r=[pF, sc, beta_bc], w=[y_])

                    if cut <= 6:
                        continue

                    def trY(e, y_=y_):
                        for cc in range(2):
                            ins = e.transpose(pYb[:, cc * 128:(cc + 1) * 128], y_.t[:, cc * 128:(cc + 1) * 128],
                                              ident_b.t[:, :])
                        return ins
                    op("pe", trY, r=[y_, ident_b], w=[pY])
                    op("act", lambda e, t=t: e.copy(out=ynTs.t[:, :, (t % 8) * 128:(t % 8 + 1) * 128],
                                                    in_=pYb[:, 0:256].rearrange("p (c t) -> p c t", c=2)),
                       r=[pY], w=[ynTs])
                    if t % 8 == 7:
                        th = (t // 8) * 1024
                        op("sp", lambda e, hg=hg, th=th: e.dma_start(
                            out=ynT_d.ap()[2 * hg:2 * hg + 2, :, th:th + 1024].rearrange("c p t -> p c t"),
                            in_=ynTs.t[:]), r=[ynTs], w=[ynT_db], dma=True, par=True)
            S.flush()
        st_holder[0] = gst

    def phase2():
        with ExitStack() as st:
            st_holder[0] = st
            xg = [sb("m_xg%d" % i, [128, 16, 512], BF16) for i in range(2)]
            wp = [sb("m_wp%d" % i, [128, 16, 256], BF16) for i in range(3)]
            wg = sb("m_wg", [128, 16, 8], BF16)
            kpre = sb("kpre", [128, 8, 515], BF16)
            qpre = sb("qpre", [128, 8, 515], BF16)
            kTm = sb("kTm", [128, 8, 512], BF16)
            qTm = sb("qTm", [128, 8, 512], BF16)
            ctmp = [sb("ctmp%d" % i, [128, 512], F32) for i in range(2)]
            v_tm = sb("v_tm", [128, 4, 4, 257], BF16)
            og = sb("og", [128, 4, 1024], BF16)
            gz = sb("gz", [128, 4, 8], F32)
            ex = sb("ex", [128, 4, 4], F32)
            lfn = sb("lfn", [128, 4, 4], F32)
            gt = [sb("gt%d" % i, [128, 4], F32) for i in range(2)]
            a_s = [sb("a_s%d" % i, [128, 4], F32) for i in range(2)]
            eb = [sb("eb%d" % i, [128, 4], F32) for i in range(2)]
            eG = [sb("eG%d" % i, [128, 4], F32) for i in range(2)]
            Cst = sb("Cst", [128, 4, 2, 257], F32)
            Cb = sb("Cb", [128, 4, 2, 257], BF16)
            ak = [sb("ak%d" % i, [128, 256], BF16) for i in range(2)]
            WT = [sb("WT%d" % i, [128, 128], BF16) for i in range(2)]
            dd = [sb("dd%d" % i, [128, 1], F32) for i in range(2)]
            hm = [sb("hm%d" % i, [128, 256], F32) for i in range(2)]
            sqm = sb("sqm", [128, 256], F32)
            ssm = [sb("ssm%d" % i, [128, 1], F32) for i in range(2)]
            ynm = [sb("ynm%d" % i, [128, 256], BF16) for i in range(2)]
            ynTs = sb("m_ynTs", [128, 8, 512], BF16)

            op("pool", lambda e: e.memset(kpre.t[:], 0.0), w=[kpre])
            op("pool", lambda e: e.memset(qpre.t[:], 0.0), w=[qpre])
            op("pool", lambda e: e.memset(v_tm.t[:], 1.0), w=[v_tm])
            op("pool", lambda e: e.memset(Cst.t[:], 0.0), w=[Cst])
            op("pool", lambda e: e.memset(Cb.t[:], 0.0), w=[Cb])
            op("pool", lambda e: e.dma_start(out=wg.t[:], in_=w_in_r[:, :, 7168:7176]), w=[wg], dma=True)

            pK = ps[4]
            pKb = psb16[4]
            pS = ps[5]
            pG = ps[2]
            pN = ps[3]
            pH = ps[6]
            pU = ps[7]
            wpi = 0
            fmi = 0
            tmi = 0
            ci_ = 0
            for gi in range(16):
                own = gi >= 12
                qcomp = gi >= 11
                s0 = 512 * gi
                x_ = xg[gi % 2]
                op("pool", lambda e, x_=x_, s0=s0: e.dma_start(out=x_.t[:], in_=xT_r[:, :, s0:s0 + 512]),
                   w=[x_], dma=True)
                pieces = [("k", i, 4096 + 256 * i) for i in range(4)] + [("v", i, 5120 + 256 * i) for i in range(4)]
                if qcomp:
                    pieces += [("q", i, 3072 + 256 * i) for i in range(4)]
                if own:
                    pieces += [("o", i, 6144 + 256 * i) for i in range(4)]
                for kind, i, c0 in pieces:
                    w_ = wp[wpi % 3]
                    wpi += 1
                    op("pool", lambda e, w_=w_, c0=c0: e.dma_start(out=w_.t[:], in_=w_in_r[:, :, c0:c0 + 256]),
                       w=[w_], dma=True)
                    if kind in ("k", "q"):
                        pre = kpre if kind == "k" else qpre
                        for cc in range(2):
                            pp = ps[fmi % 2]
                            fmi += 1

                            def mmf(e, pp=pp, w_=w_, x_=x_, cc=cc):
                                for kc in range(16):
                                    ins = e.matmul(pp.t[:, :], w_.t[:, kc, cc * 128:(cc + 1) * 128], x_.t[:, kc, :],
                                                   start=(kc == 0), stop=(kc == 15))
                                return ins
                            op("pe", mmf, r=[w_, x_], w=[pp])
                            op("act", lambda e, pp=pp, pre=pre, ch=2 * i + cc: e.copy(
                                out=pre.t[:, ch, 3:515], in_=pp.t[:, :]), r=[pp], w=[pre])
                    else:
                        for tl in range(4):
                            pp = ps[tmi % 2]
                            tmi += 1

                            def mmt(e, pp=pp, w_=w_, x_=x_, tl=tl):
                                for kc in range(16):
                                    ins = e.matmul(pp.t[:, 0:256], x_.t[:, kc, tl * 128:(tl + 1) * 128], w_.t[:, kc, :],
                                                   start=(kc == 0), stop=(kc == 15))
                                return ins
                            op("pe", mmt, r=[w_, x_], w=[pp])
                            if kind == "v":
                                op("dve", lambda e, pp=pp, tl=tl, i=i: e.tensor_copy(
                                    out=v_tm.t[:, tl, i, 0:256], in_=pp.t[:, 0:256]), r=[pp], w=[v_tm])
                            else:
                                op("act", lambda e, pp=pp, tl=tl, i=i: e.activation(
                                    out=og.t[:, tl, i * 256:(i + 1) * 256], in_=pp.t[:, 0:256], func=AF.Sigmoid),
                                   r=[pp], w=[og])
                for tl in range(4):
                    pp = ps[tmi % 2]
                    tmi += 1

                    def mmg(e, pp=pp, x_=x_, tl=tl):
                        for kc in range(16):
                            ins = e.matmul(pp.t[:, 0:8], x_.t[:, kc, tl * 128:(tl + 1) * 128], wg.t[:, kc, :],
                                           start=(kc == 0), stop=(kc == 15))
                        return ins
                    op("pe", mmg, r=[wg, x_], w=[pp])
                    op("dve", lambda e, pp=pp, tl=tl: e.tensor_tensor(out=gz.t[:, tl, :], in0=pp.t[:, 0:8],
                                                                     in1=bg_bc.t[:, :], op=ALU.add),
                       r=[pp, bg_bc], w=[gz])
                op("act", lambda e: e.activation(out=ex.t[:], in_=gz.t[:, :, 4:8], func=AF.Exp, scale=-1.0),
                   r=[gz], w=[ex])
                op("act", lambda e: e.activation(out=lfn.t[:], in_=ex.t[:], func=AF.Ln, bias=1.0, scale=1.0),
                   r=[ex], w=[lfn])
                convs = [("k", kpre, kTm, 8)]
                if own:
                    convs.append(("q", qpre, qTm, 0))
                for kind, pre, dst, cof in convs:
                    for ch in range(8):
                        ct = ctmp[ch % 2]
                        wch = cof + ch
                        op("dve", lambda e, ct=ct, pre=pre, ch=ch, wch=wch: e.tensor_scalar(
                            out=ct.t[:], in0=pre.t[:, ch, 0:512], scalar1=cw_s.t[:, wch, 0:1], scalar2=None,
                            op0=ALU.mult), r=[pre, cw_s], w=[ct])
                        for j in range(1, 4):
                            op("dve", lambda e, ct=ct, pre=pre, ch=ch, wch=wch, j=j: e.scalar_tensor_tensor(
                                out=ct.t[:], in0=pre.t[:, ch, j:j + 512], scalar=cw_s.t[:, wch, j:j + 1], in1=ct.t[:],
                                op0=ALU.mult, op1=ALU.add), r=[pre, cw_s, ct], w=[ct])
                        op("act", lambda e, ct=ct, dst=dst, ch=ch, wch=wch: e.activation(
                            out=dst.t[:, ch, :], in_=ct.t[:], func=AF.Silu, bias=cb_s.t[:, wch:wch + 1], scale=1.0),
                           r=[ct, cb_s], w=[dst])
                op("act", lambda e: e.copy(out=kpre.t[:, :, 0:3], in_=kpre.t[:, :, 512:515]),
                   r=[kpre], w=[kpre])
                if qcomp:
                    op("act", lambda e: e.copy(out=qpre.t[:, :, 0:3], in_=qpre.t[:, :, 512:515]),
                       r=[qpre], w=[qpre])
                for ci in range(4):
                    gc = 4 * gi + ci
                    cs = slice(ci * 128, (ci + 1) * 128)
                    gt_, a_, eb_, eG_ = gt[gc % 2], a_s[gc % 2], eb[gc % 2], eG[gc % 2]

                    def mmc(e, ci=ci):
                        e.matmul(pG.t[:, 256:260], triu_f.t[:, :], lfn.t[:, ci, :], start=True, stop=True)
                        return e.matmul(pG.t[:, 260:264], ones_f.t[:, :], lfn.t[:, ci, :], start=True, stop=True)
                    op("pe", mmc, r=[triu_f, ones_f, lfn], w=[pG])
                    op("dve", lambda e, gt_=gt_, ci=ci: e.tensor_tensor(out=gt_.t[:], in0=gz.t[:, ci, 0:4],
                                                                       in1=pG.t[:, 256:260], op=ALU.add),
                       r=[gz, pG], w=[gt_])
                    op("act", lambda e, gt_=gt_, a_=a_: e.activation(out=a_.t[:], in_=gt_.t[:], func=AF.Exp),
                       r=[gt_], w=[a_])
                    op("dve", lambda e, a_=a_, gc=gc: e.tensor_scalar(out=a_.t[:], in0=a_.t[:],
                                                                     scalar1=valid_s.t[:, gc:gc + 1], scalar2=0.0625,
                                                                     op0=ALU.mult, op1=ALU.mult),
                       r=[a_, valid_s], w=[a_])
                    op("act", lambda e, eG_=eG_: e.activation(out=eG_.t[:], in_=pG.t[:, 260:264], func=AF.Exp,
                                                              scale=-1.0), r=[pG], w=[eG_])
                    if own:
                        op("act", lambda e, eb_=eb_: e.activation(out=eb_.t[:], in_=pG.t[:, 256:260], func=AF.Exp,
                                                                  scale=-1.0), r=[pG], w=[eb_])
                    for h in range(4):
                        ak_ = ak[ci_ % 2]
                        WT_ = WT[ci_ % 2]
                        dd_ = dd[ci_ % 2]
                        hm_ = hm[ci_ % 2]
                        ss_ = ssm[ci_ % 2]
                        yn_ = ynm[ci_ % 2]
                        ci_ += 1

                        def trk(e, h=h, cs=cs):
                            for cc in range(2):
                                ins = e.transpose(pKb[:, cc * 128:(cc + 1) * 128], kTm.t[:, 2 * h + cc, cs],
                                                  ident_b.t[:, :])
                            return ins
                        op("pe", trk, r=[kTm, ident_b], w=[pK])
                        op("dve", lambda e, ak_=ak_, a_=a_, h=h: e.tensor_scalar(
                            out=ak_.t[:], in0=pKb[:, 0:256], scalar1=a_.t[:, h:h + 1], scalar2=None, op0=ALU.mult),
                           r=[pK, a_], w=[ak_])
                        if own:
                            def mms(e, h=h, cs=cs):
                                for cc in range(2):
                                    ins = e.matmul(pS.t[:, 0:128], kTm.t[:, 2 * h + cc, cs], qTm.t[:, 2 * h + cc, cs],
                                                   start=(cc == 0), stop=(cc == 1))
                                return ins
                            op("pe", mms, r=[kTm, qTm], w=[pS])
                            op("dve", lambda e, WT_=WT_, a_=a_, h=h: e.scalar_tensor_tensor(
                                out=WT_.t[:], in0=pS.t[:, 0:128], scalar=a_.t[:, h:h + 1], in1=triu_f.t[:, :],
                                op0=ALU.mult, op1=ALU.mult), r=[pS, a_, triu_f], w=[WT_])

                            def mmh(e, WT_=WT_, h=h, ci=ci, cs=cs):
                                e.matmul(pH.t[:, 0:257], WT_.t[:, :], v_tm.t[:, ci, h, :], start=True, stop=False)
                                e.matmul(pH.t[:, 0:257], qTm.t[:, 2 * h, cs], Cb.t[:, h, 0, :], start=False, stop=False)
                                return e.matmul(pH.t[:, 0:257], qTm.t[:, 2 * h + 1, cs], Cb.t[:, h, 1, :],
                                                start=False, stop=True)
                            op("pe", mmh, r=[WT_, v_tm, qTm, Cb], w=[pH])
                            op("dve", lambda e, dd_=dd_, eb_=eb_, h=h: e.tensor_scalar(
                                out=dd_.t[:], in0=pH.t[:, 256:257], scalar1=eb_.t[:, h:h + 1], scalar2=None,
                                op0=ALU.mult), r=[pH, eb_], w=[dd_])
                            op("dve", lambda e, dd_=dd_: e.scalar_tensor_tensor(
                                out=dd_.t[:], in0=dd_.t[:], scalar=-1.0, in1=dd_.t[:], op0=ALU.mult, op1=ALU.max),
                               r=[dd_], w=[dd_])
                            op("dve", lambda e, dd_=dd_: e.tensor_scalar(
                                out=dd_.t[:], in0=dd_.t[:], scalar1=1.0, scalar2=None, op0=ALU.max),
                               r=[dd_], w=[dd_])
                            op("dve", lambda e, dd_=dd_: e.reciprocal(out=dd_.t[:], in_=dd_.t[:]), r=[dd_], w=[dd_])
                            op("dve", lambda e, dd_=dd_, eb_=eb_, h=h: e.tensor_tensor(
                                out=dd_.t[:], in0=dd_.t[:], in1=eb_.t[:, h:h + 1], op=ALU.mult),
                               r=[dd_, eb_], w=[dd_])
                            op("dve", lambda e, hm_=hm_, dd_=dd_, ci=ci, h=h: e.scalar_tensor_tensor(
                                out=hm_.t[:], in0=pH.t[:, 0:256], scalar=dd_.t[:, 0:1],
                                in1=og.t[:, ci, h * 256:(h + 1) * 256], op0=ALU.mult, op1=ALU.mult),
                               r=[pH, dd_, og], w=[hm_])
                            op("act", lambda e, hm_=hm_: e.activation(out=sqm.t[:], in_=hm_.t[:], func=AF.Square),
                               r=[hm_], w=[sqm])
                            op("dve", lambda e, ss_=ss_: e.tensor_reduce(out=ss_.t[:], in_=sqm.t[:], axis=AX.X,
                                                                         op=ALU.add), r=[sqm], w=[ss_])
                            op("dve", lambda e, ss_=ss_: e.tensor_scalar(
                                out=ss_.t[:], in0=ss_.t[:], scalar1=1.0 / 256.0, scalar2=HN_EPS, op0=ALU.mult,
                                op1=ALU.add), r=[ss_], w=[ss_])
                            op("act", lambda e, ss_=ss_: e.activation(out=ss_.t[:], in_=ss_.t[:], func=AF.Sqrt),
                               r=[ss_], w=[ss_])
                            op("dve", lambda e, ss_=ss_: e.reciprocal(out=ss_.t[:], in_=ss_.t[:]), r=[ss_], w=[ss_])
                            op("dve", lambda e, yn_=yn_, hm_=hm_, ss_=ss_, h=h: e.scalar_tensor_tensor(
                                out=yn_.t[:], in0=hm_.t[:], scalar=ss_.t[:, 0:1],
                                in1=beta_bc.t[:, 1024 + h * 256:1024 + (h + 1) * 256], op0=ALU.mult, op1=ALU.mult),
                               r=[hm_, ss_, beta_bc], w=[yn_])

                            def try_(e, yn_=yn_):
                                for cc in range(2):
                                    ins = e.transpose(pKb[:, 256 + cc * 128:256 + (cc + 1) * 128],
                                                      yn_.t[:, cc * 128:(cc + 1) * 128], ident_b.t[:, :])
                                return ins
                            op("pe", try_, r=[yn_, ident_b], w=[pK])
                            op("act", lambda e, h=h, cs=cs: e.copy(
                                out=ynTs.t[:, 2 * h:2 * h + 2, cs],
                                in_=pKb[:, 256:512].rearrange("p (c t) -> p c t", c=2)), r=[pK], w=[ynTs])

                        def mmu(e, ak_=ak_, ci=ci, h=h):
                            for cc in range(2):
                                e.matmul(pU.t[:, cc * 256:(cc + 1) * 256], ak_.t[:, cc * 128:(cc + 1) * 128],
                                         v_tm.t[:, ci, h, 0:256], start=True, stop=True)
                            for cc in range(2):
                                ins = e.matmul(pN.t[:, 300 + cc:301 + cc], ak_.t[:, cc * 128:(cc + 1) * 128],
                                               v_tm.t[:, ci, h, 256:257], start=True, stop=True)
                            return ins
                        op("pe", mmu, r=[ak_, v_tm], w=[pU, pN])
                        op("dve", lambda e, eG_=eG_, h=h: e.tensor_scalar(
                            out=Cst.t[:, h].rearrange("p c n -> p (c n)"),
                            in0=Cst.t[:, h].rearrange("p c n -> p (c n)"),
                            scalar1=eG_.t[:, h:h + 1], scalar2=None, op0=ALU.mult), r=[Cst, eG_], w=[Cst])
                        op("dve", lambda e, eG_=eG_, h=h: e.scalar_tensor_tensor(
                            out=Cst.t[:, h, :, 0:256], in0=pU.t[:, :].rearrange("p (c n) -> p c n", c=2),
                            scalar=eG_.t[:, h:h + 1], in1=Cst.t[:, h, :, 0:256], op0=ALU.mult, op1=ALU.add),
                           r=[pU, eG_, Cst], w=[Cst])
                        op("dve", lambda e, eG_=eG_, h=h: e.scalar_tensor_tensor(
                            out=Cst.t[:, h, :, 256], in0=pN.t[:, 300:302], scalar=eG_.t[:, h:h + 1],
                            in1=Cst.t[:, h, :, 256], op0=ALU.mult, op1=ALU.add), r=[pN, eG_, Cst], w=[Cst])
                        if gc >= 47:
                            op("act", lambda e, h=h: e.copy(out=Cb.t[:, h].rearrange("p c n -> p (c n)"),
                                                            in_=Cst.t[:, h].rearrange("p c n -> p (c n)")),
                               r=[Cst], w=[Cb])
                if own:
                    t0 = (gi - 12) * 512
                    op("sp", lambda e, t0=t0: e.dma_start(
                        out=ynT_d.ap()[8:16, :, t0:t0 + 512].rearrange("c p t -> p c t"), in_=ynTs.t[:]),
                       r=[ynTs], w=[ynT_db], dma=True)
            S.flush()
        st_holder[0] = gst

    def layer_norm_ops(y_ap, yB, g_bc, b_bc, stats, mv, rstd):
        for q in range(4):
            op("dve", lambda e, q=q: e.bn_stats(out=stats.t[:, q, :], in_=y_ap[:, q * 512:(q + 1) * 512]),
               r=[yB], w=[stats])
        op("dve", lambda e: e.bn_aggr(out=mv.t[:], in_=stats.t[:].rearrange("p a b -> p (a b)")), r=[stats], w=[mv])
        op("dve", lambda e: e.tensor_scalar(out=rstd.t[:], in0=mv.t[:, 1:2], scalar1=LN_EPS, scalar2=None,
                                            op0=ALU.add), r=[mv], w=[rstd])
        op("act", lambda e: e.activation(out=rstd.t[:], in_=rstd.t[:], func=AF.Sqrt), r=[rstd], w=[rstd])
        op("dve", lambda e: e.reciprocal(out=rstd.t[:], in_=rstd.t[:]), r=[rstd], w=[rstd])
        op("dve", lambda e: e.tensor_scalar(out=y_ap, in0=y_ap, scalar1=mv.t[:, 0:1], scalar2=rstd.t[:, 0:1],
                                            op0=ALU.subtract, op1=ALU.mult), r=[yB, mv, rstd], w=[yB])
        op("dve", lambda e: e.tensor_tensor(out=y_ap, in0=y_ap, in1=g_bc.t[:], op=ALU.mult), r=[yB, g_bc], w=[yB])
        op("dve", lambda e: e.tensor_tensor(out=y_ap, in0=y_ap, in1=b_bc.t[:], op=ALU.add), r=[yB, b_bc], w=[yB])

    def phase3():
        with ExitStack() as st:
            st_holder[0] = st
            ynTg = sb("ynTg", [128, 16, 512], BF16)
            wo = [sb("wo%d" % i, [128, 16, 512], BF16) for i in range(2)]
            xo = sb("xo", [128, 4, D], F32)
            y1 = [sb("y1_%d" % i, [128, D], F32) for i in range(4)]
            g_bc = sb("g1_bc", [128, D], F32)
            b_bc = sb("b1_bc", [128, D], F32)
            h1T = sb("h1T", [128, 16, 128], F32)
            h1b = [sb("h1b%d" % i, [128, D], BF16) for i in range(2)]
            wr = sb("wr", [128, 16, NEXP], F32)
            br_bc = sb("br_bc", [128, NEXP], F32)
            ecap_bc = sb("ecap_bc", [128, NEXP], F32)
            cntb = sb("cntb", [128, NEXP], F32)
            stats = sb("stats", [128, 4, 6], F32)
            mv = sb("mv", [128, 2], F32)
            rstd = sb("rstd", [128, 1], F32)
            lg = sb("lg", [128, NEXP], F32)
            mx8 = sb("mx8", [128, 8], F32)
            nmx = sb("nmx", [128, 1], F32)
            msk = sb("msk", [128, NEXP], F32)
            eml = sb("eml", [128, NEXP], F32)
            esum = sb("esum", [128, 1], F32)
            Gd = sb("Gd", [128, NEXP], F32)
            pos = sb("pos", [128, NEXP], F32)
            ovf = sb("ovf", [128, NEXP], F32)
            oh = sb("oh", [128, NEXP], F32)
            tmp32 = sb("tmp32", [128, NEXP], F32)
            offs_f = sb("offs_f", [128, 4], F32)

            op("sp", lambda e: e.dma_start(out=g_bc.t[:], in_=bcast(ln1_g, D)), w=[g_bc], dma=True)
            op("sp", lambda e: e.dma_start(out=b_bc.t[:], in_=bcast(ln1_b, D)), w=[b_bc], dma=True)
            op("sp", lambda e: e.dma_start(out=br_bc.t[:], in_=bcast(b_router, NEXP)), w=[br_bc], dma=True)
            op("sp", lambda e: e.dma_start(out=ecap_bc.t[:], in_=bcast(ecap, NEXP)), w=[ecap_bc], dma=True)
            op("sp", lambda e: e.dma_start(out=wr.t[:], in_=w_router.ap().rearrange("(kc p) n -> p kc n", p=128)),
               w=[wr], dma=True)
            op("pool", lambda e: e.memset(cntb.t[:], 0.0), w=[cntb])
            w_out_r = w_out.ap().rearrange("(kc p) c -> p kc c", p=128)
            pT = ps[4]
            pL = ps[5]
            pP = ps[6]
            pC = ps[7]
            woi = 0
            for tg in range(4):
                t0 = 512 * tg
                op("sp", lambda e, t0=t0: e.dma_start(
                    out=ynTg.t[:], in_=ynT_d.ap()[:, :, t0:t0 + 512].rearrange("c p t -> p c t")),
                   r=[ynT_db], w=[ynTg], dma=True)
                op("sp", lambda e, t0=t0: e.dma_start(
                    out=xo.t[:], in_=x_own.ap()[t0:t0 + 512, :].rearrange("(t p) d -> p t d", p=128)),
                   w=[xo], dma=True)
                for cg in range(4):
                    w_ = wo[woi % 2]
                    woi += 1
                    op("pool", lambda e, w_=w_, cg=cg: e.dma_start(out=w_.t[:], in_=w_out_r[:, :, cg * 512:(cg + 1) * 512]),
                       w=[w_], dma=True)
                    for tl in range(4):
                        pp = ps[(cg * 4 + tl) % 4]

                        def mmo(e, pp=pp, w_=w_, tl=tl):
                            for kc in range(16):
                                ins = e.matmul(pp.t[:, :], ynTg.t[:, kc, tl * 128:(tl + 1) * 128], w_.t[:, kc, :],
                                               start=(kc == 0), stop=(kc == 15))
                            return ins
                        op("pe", mmo, r=[ynTg, w_], w=[pp])
                        op("dve", lambda e, pp=pp, tl=tl, cg=cg: e.scalar_tensor_tensor(
                            out=y1[tl].t[:, cg * 512:(cg + 1) * 512], in0=xo.t[:, tl, cg * 512:(cg + 1) * 512],
                            scalar=ALPHA, in1=pp.t[:, :], op0=ALU.mult, op1=ALU.add), r=[xo, pp], w=[y1[tl]])
                for tl in range(4):
                    tt = 4 * tg + tl
                    y_ = y1[tl]
                    layer_norm_ops(y_.t[:], y_, g_bc, b_bc, stats, mv, rstd)
                    op("sp", lambda e, y_=y_, tt=tt: e.dma_start(out=h1_d.ap()[tt * 128:(tt + 1) * 128, :], in_=y_.t[:]),
                       r=[y_], w=[h1_db], dma=True, par=True)
                    hb = h1b[tt % 2]
                    op("act", lambda e, y_=y_, hb=hb: e.copy(out=hb.t[:], in_=y_.t[:]), r=[y_], w=[hb])
                    for q in range(4):
                        def trh(e, y_=y_, q=q):
                            for u in range(4):
                                kc = 4 * q + u
                                ins = e.transpose(pT.t[:, u * 128:(u + 1) * 128], y_.t[:, kc * 128:(kc + 1) * 128],
                                                  ident_f.t[:, :])
                            return ins
                        op("pe", trh, r=[y_, ident_f], w=[pT])
                        op("act", lambda e, q=q: e.copy(
                            out=h1T.t[:, 4 * q:4 * q + 4, :], in_=pT.t[:, :].rearrange("p (u t) -> p u t", u=4)),
                           r=[pT], w=[h1T])

                    def mml(e):
                        for kc in range(16):
                            ins = e.matmul(pL.t[:, 0:32], h1T.t[:, kc, :], wr.t[:, kc, :], start=(kc == 0),
                                           stop=(kc == 15))
                        return ins
                    op("pe", mml, r=[h1T, wr], w=[pL])
                    op("dve", lambda e: e.tensor_tensor(out=lg.t[:], in0=pL.t[:, 0:32], in1=br_bc.t[:], op=ALU.add),
                       r=[pL, br_bc], w=[lg])
                    op("dve", lambda e: e.max(out=mx8.t[:], in_=lg.t[:]), r=[lg], w=[mx8])
                    op("dve", lambda e: e.tensor_scalar(out=nmx.t[:], in0=mx8.t[:, 0:1], scalar1=-1.0, scalar2=None,
                                                        op0=ALU.mult), r=[mx8], w=[nmx])
                    op("dve", lambda e: e.tensor_scalar(out=msk.t[:], in0=lg.t[:], scalar1=mx8.t[:, 3:4], scalar2=None,
                                                        op0=ALU.is_ge), r=[lg, mx8], w=[msk])
                    op("act", lambda e: e.activation(out=eml.t[:], in_=lg.t[:], func=AF.Exp, bias=nmx.t[:, 0:1],
                                                     scale=1.0), r=[lg, nmx], w=[eml])
                    op("dve", lambda e: e.tensor_tensor(out=eml.t[:], in0=eml.t[:], in1=msk.t[:], op=ALU.mult),
                       r=[eml, msk], w=[eml])
                    op("dve", lambda e: e.tensor_reduce(out=esum.t[:], in_=eml.t[:], axis=AX.X, op=ALU.add),
                       r=[eml], w=[esum])
                    op("dve", lambda e: e.reciprocal(out=esum.t[:], in_=esum.t[:]), r=[esum], w=[esum])
                    op("dve", lambda e: e.tensor_scalar(out=Gd.t[:], in0=eml.t[:], scalar1=esum.t[:, 0:1], scalar2=None,
                                                        op0=ALU.mult), r=[eml, esum], w=[Gd])

                    def mmp(e):
                        e.matmul(pP.t[:, 64:96], trist_f.t[:, :], msk.t[:, :], start=True, stop=True)
                        return e.matmul(pC.t[:, 128:160], ones_f.t[:, :], msk.t[:, :], start=True, stop=True)
                    op("pe", mmp, r=[trist_f, ones_f, msk], w=[pP, pC])
                    op("dve", lambda e: e.tensor_tensor(out=pos.t[:], in0=pP.t[:, 64:96], in1=cntb.t[:], op=ALU.add),
                       r=[pP, cntb], w=[pos])
                    op("dve", lambda e: e.tensor_tensor(out=cntb.t[:], in0=pC.t[:, 128:160], in1=cntb.t[:], op=ALU.add),
                       r=[pC, cntb], w=[cntb])
                    op("dve", lambda e: e.tensor_scalar(out=ovf.t[:], in0=pos.t[:], scalar1=float(CAP), scalar2=None,
                                                        op0=ALU.is_ge), r=[pos], w=[ovf])
                    op("dve", lambda e: e.tensor_tensor(out=pos.t[:], in0=pos.t[:], in1=ecap_bc.t[:], op=ALU.add),
                       r=[pos, ecap_bc], w=[pos])
                    op("dve", lambda e: e.tensor_scalar(out=tmp32.t[:], in0=pos.t[:], scalar1=-1.0, scalar2=float(NSLOT),
                                                        op0=ALU.mult, op1=ALU.add), r=[pos], w=[tmp32])
                    op("dve", lambda e: e.tensor_tensor(out=tmp32.t[:], in0=tmp32.t[:], in1=ovf.t[:], op=ALU.mult),
                       r=[tmp32, ovf], w=[tmp32])
                    op("dve", lambda e: e.tensor_tensor(out=pos.t[:], in0=pos.t[:], in1=tmp32.t[:], op=ALU.add),
                       r=[pos, tmp32], w=[pos])
                    for k in range(4):
                        op("dve", lambda e, k=k: e.tensor_scalar(out=oh.t[:], in0=lg.t[:], scalar1=mx8.t[:, k:k + 1],
                                                                 scalar2=None, op0=ALU.is_equal), r=[lg, mx8], w=[oh])
                        op("dve", lambda e: e.tensor_tensor(out=tmp32.t[:], in0=oh.t[:], in1=pos.t[:], op=ALU.mult),
                           r=[oh, pos], w=[tmp32])
                        op("dve", lambda e, k=k: e.tensor_reduce(out=offs_f.t[:, k:k + 1], in_=tmp32.t[:], axis=AX.X,
                                                                 op=ALU.add), r=[tmp32], w=[offs_f])
                        op("dve", lambda e: e.tensor_tensor(out=tmp32.t[:], in0=oh.t[:], in1=Gd.t[:], op=ALU.mult),
                           r=[oh, Gd], w=[tmp32])
                        op("dve", lambda e, k=k, tt=tt: e.tensor_reduce(out=gk.t[:, tt, k:k + 1], in_=tmp32.t[:],
                                                                        axis=AX.X, op=ALU.add), r=[tmp32], w=[gk])
                    op("dve", lambda e, tt=tt: e.tensor_copy(out=offs_i.t[:, tt, :], in_=offs_f.t[:]),
                       r=[offs_f], w=[offs_i])
                    if debug:
                        dbs = sb("dbs%d" % tt, [128, 40], F32)
                        op("dve", lambda e, dbs=dbs: e.tensor_copy(out=dbs.t[:, 0:32], in_=lg.t[:]), r=[lg], w=[dbs])
                        op("dve", lambda e, dbs=dbs: e.tensor_copy(out=dbs.t[:, 32:36], in_=offs_f.t[:]),
                           r=[offs_f], w=[dbs])
                        op("dve", lambda e, dbs=dbs, tt=tt: e.tensor_copy(out=dbs.t[:, 36:40], in_=gk.t[:, tt, :]),
                           r=[gk], w=[dbs])
                        op("sp", lambda e, dbs=dbs, tt=tt: e.dma_start(out=dbg_r.ap()[:, tt, :], in_=dbs.t[:]),
                           r=[dbs], w=[dbg_rb], dma=True, par=True)
                    for k in range(4):
                        op("pool", lambda e, hb=hb, tt=tt, k=k: e.indirect_dma_start(
                            out=xs_d.ap()[:, :], out_offset=bass.IndirectOffsetOnAxis(ap=offs_i.t[:, tt, k:k + 1], axis=0),
                            in_=hb.t[:, :], in_offset=None),
                           r=[hb, offs_i], w=[xs_db], dma=True, par=True, key=hb.b)
            S.flush()
        st_holder[0] = gst

    def phase4():
        with ExitStack() as st:
            st_holder[0] = st
            wb = [sb("wb%d" % i, [128, 16, 512], BF16) for i in range(4)]
            xs_tm = [sb("xs_tm%d" % i, [128, NTC, D], BF16) for i in range(2)]
            xsT = sb("xsT", [128, 16, CAP], BF16)
            gaT = sb("gaT", [128, 16, CAP], BF16)
            actT = sb("actT", [128, 16, CAP], BF16)
            g32 = [sb("g32_%d" % i, [128, CAP], F32) for i in range(2)]
            sg32 = [sb("sg32_%d" % i, [128, CAP], F32) for i in range(2)]
            u32 = [sb("u32_%d" % i, [128, CAP], F32) for i in range(2)]
            ys_sb = [sb("ys_sb%d" % i, [128, 512], F32) for i in range(3)]
            bup_s = sb("bup_s", [128, NEXP, 32], F32)
            bd = [sb("bd%d" % i, [1, D], BF16) for i in range(2)]
            op("sp", lambda e: e.dma_start(out=bup_s.t[:], in_=bupT.ap()), w=[bup_s], dma=True)
            zrow = sb("zrow", [1, 512], F32)
            op("pool", lambda e: e.memset(zrow.t[:], 0.0), w=[zrow])
            for q in range(4):
                op("sp", lambda e, q=q: e.dma_start(out=ys_d.ap()[NSLOT:NSLOT + 1, q * 512:(q + 1) * 512], in_=zrow.t[:]),
                   r=[zrow], w=[ys_db], dma=True, par=True)
            wbi = 0
            upi = 0
            dni = 0

            def load_xs(ex_):
                xt = xs_tm[ex_ % 2]
                r0 = ex_ * CAP
                op("sp", lambda e, xt=xt, r0=r0: e.dma_start(
                    out=xt.t[:], in_=xs_d.ap()[r0:r0 + CAP, :].rearrange("(t p) d -> p t d", p=128)),
                   r=[xs_db], w=[xt], dma=True)

            def transposes(ex_):
                xt = xs_tm[ex_ % 2]
                for tl in range(NTC):
                    for q in range(2):
                        pt = ps[6 + q]
                        ptb = psb16[6 + q]

                        def trx(e, ptb=ptb, xt=xt, tl=tl, q=q):
                            for u in range(8):
                                kc = 8 * q + u
                                ins = e.transpose(ptb[:, u * 128:(u + 1) * 128], xt.t[:, tl, kc * 128:(kc + 1) * 128],
                                                  ident_b.t[:, :])
                            return ins
                        op("pe", trx, r=[xt, ident_b], w=[pt])
                        op("act", lambda e, ptb=ptb, tl=tl, q=q: e.copy(
                            out=xsT.t[:, 8 * q:8 * q + 8, tl * 128:(tl + 1) * 128],
                            in_=ptb[:, 0:1024].rearrange("p (u t) -> p u t", u=8)), r=[pt], w=[xsT])

            load_xs(0)
            transposes(0)
            for ex_ in range(NEXP):
                bd_ = bd[ex_ % 2]
                r0 = ex_ * CAP
                if ex_ + 1 < NEXP:
                    load_xs(ex_ + 1)
                op("pool", lambda e, bd_=bd_, ex_=ex_: e.dma_start(out=bd_.t[:], in_=b_down.ap()[ex_:ex_ + 1, :]),
                   w=[bd_], dma=True)
                w_up_r = w_up.ap()[ex_].rearrange("(kc p) c -> p kc c", p=128)
                for cg in range(8):
                    w_ = wb[wbi % 4]
                    wbi += 1
                    op("pool", lambda e, w_=w_, w_up_r=w_up_r, cg=cg: e.dma_start(
                        out=w_.t[:], in_=w_up_r[:, :, cg * 512:(cg + 1) * 512]), w=[w_], dma=True)
                    for cc in range(4):
                        ch = 4 * cg + cc
                        pp = ps[upi % 3]
                        upi += 1

                        def mmup(e, pp=pp, w_=w_, cc=cc):
                            for kc in range(16):
                                ins = e.matmul(pp.t[:, 0:CAP], w_.t[:, kc, cc * 128:(cc + 1) * 128], xsT.t[:, kc, :],
                                               start=(kc == 0), stop=(kc == 15))
                            return ins
                        op("pe", mmup, r=[w_, xsT], w=[pp])
                        if ch < 16:
                            g_ = g32[ch % 2]
                            s_ = sg32[ch % 2]
                            op("dve", lambda e, pp=pp, g_=g_, ex_=ex_, ch=ch: e.tensor_scalar(
                                out=g_.t[:], in0=pp.t[:, 0:CAP], scalar1=bup_s.t[:, ex_, ch:ch + 1], scalar2=7.0,
                                op0=ALU.add, op1=ALU.min), r=[pp, bup_s], w=[g_])
                            op("act", lambda e, g_=g_, s_=s_: e.activation(out=s_.t[:], in_=g_.t[:], func=AF.Sigmoid,
                                                                          scale=1.702), r=[g_], w=[s_])
                            op("dve", lambda e, g_=g_, s_=s_, ch=ch: e.tensor_tensor(
                                out=gaT.t[:, ch, :], in0=g_.t[:], in1=s_.t[:], op=ALU.mult), r=[g_, s_], w=[gaT])
                        else:
                            c2 = ch - 16
                            u_ = u32[ch % 2]
                            op("dve", lambda e, pp=pp, u_=u_, ex_=ex_, ch=ch: e.tensor_scalar(
                                out=u_.t[:], in0=pp.t[:, 0:CAP], scalar1=bup_s.t[:, ex_, ch:ch + 1], scalar2=7.0,
                                op0=ALU.add, op1=ALU.min), r=[pp, bup_s], w=[u_])
                            op("dve", lambda e, u_=u_: e.tensor_scalar(
                                out=u_.t[:], in0=u_.t[:], scalar1=-7.0, scalar2=1.0, op0=ALU.max, op1=ALU.add),
                               r=[u_], w=[u_])
                            op("dve", lambda e, u_=u_, c2=c2: e.tensor_tensor(
                                out=actT.t[:, c2, :], in0=u_.t[:], in1=gaT.t[:, c2, :], op=ALU.mult),
                               r=[u_, gaT], w=[actT])
                if ex_ + 1 < NEXP:
                    transposes(ex_ + 1)
                w_dn_r = w_down.ap()[ex_].rearrange("(kc p) c -> p kc c", p=128)
                for cg in range(4):
                    w_ = wb[wbi % 4]
                    wbi += 1
                    op("pool", lambda e, w_=w_, w_dn_r=w_dn_r, cg=cg: e.dma_start(
                        out=w_.t[:], in_=w_dn_r[:, :, cg * 512:(cg + 1) * 512]), w=[w_], dma=True)
                    for tl in range(NTC):
                        pp = ps[3 + dni % 3]
                        ysb = ys_sb[dni % 3]
                        dni += 1

                        def mmdn(e, pp=pp, w_=w_, tl=tl, bd_=bd_, cg=cg):
                            for kc in range(16):
                                e.matmul(pp.t[:, :], actT.t[:, kc, tl * 128:(tl + 1) * 128], w_.t[:, kc, :],
                                         start=(kc == 0), stop=False)
                            return e.matmul(pp.t[:, :], ones_b.t[0:1, :], bd_.t[0:1, cg * 512:(cg + 1) * 512],
                                            start=False, stop=True)
                        op("pe", mmdn, r=[actT, w_, bd_, ones_b], w=[pp])
                        op("act", lambda e, pp=pp, ysb=ysb: e.copy(out=ysb.t[:, :], in_=pp.t[:, :]), r=[pp], w=[ysb])
                        op("sp", lambda e, r0=r0, tl=tl, cg=cg, ysb=ysb: e.dma_start(
                            out=ys_d.ap()[r0 + tl * 128:r0 + (tl + 1) * 128, cg * 512:(cg + 1) * 512], in_=ysb.t[:, :]),
                           r=[ysb], w=[ys_db], dma=True, par=True)
            S.flush()
        st_holder[0] = gst

    def phase5():
        with ExitStack() as st:
            st_holder[0] = st
            h1t = [sb("h1t%d" % i, [128, D], F32) for i in range(2)]
            yk = [sb("yk%d" % i, [128, D], F32) for i in range(4)]
            g_bc = sb("g2_bc", [128, D], F32)
            b_bc = sb("b2_bc", [128, D], F32)
            stats = sb("stats2", [128, 4, 6], F32)
            mv = sb("mv2", [128, 2], F32)
            rstd = sb("rstd2", [128, 1], F32)
            op("sp", lambda e: e.dma_start(out=g_bc.t[:], in_=bcast(ln2_g, D)), w=[g_bc], dma=True)
            op("sp", lambda e: e.dma_start(out=b_bc.t[:], in_=bcast(ln2_b, D)), w=[b_bc], dma=True)
            yi = 0
            for tt in range(16):
                a_ = h1t[tt % 2]
                op("sp", lambda e, a_=a_, tt=tt: e.dma_start(out=a_.t[:], in_=h1_d.ap()[tt * 128:(tt + 1) * 128, :]),
                   r=[h1_db], w=[a_], dma=True)
                op("act", lambda e, a_=a_: e.activation(out=a_.t[:], in_=a_.t[:], func=AF.Copy, scale=ALPHA),
                   r=[a_], w=[a_])
                for k in range(4):
                    y_ = yk[yi % 4]
                    yi += 1
                    op("pool", lambda e, y_=y_, tt=tt, k=k: e.indirect_dma_start(
                        out=y_.t[:, :], out_offset=None, in_=ys_d.ap()[:, :],
                        in_offset=bass.IndirectOffsetOnAxis(ap=offs_i.t[:, tt, k:k + 1], axis=0)),
                       r=[ys_db, offs_i], w=[y_], dma=True)
                    op("dve", lambda e, y_=y_, a_=a_, tt=tt, k=k: e.scalar_tensor_tensor(
                        out=a_.t[:], in0=y_.t[:], scalar=gk.t[:, tt, k:k + 1], in1=a_.t[:], op0=ALU.mult, op1=ALU.add),
                       r=[y_, gk, a_], w=[a_])
                layer_norm_ops(a_.t[:], a_, g_bc, b_bc, stats, mv, rstd)
                op("sp", lambda e, a_=a_, tt=tt: e.dma_start(out=out_d.ap()[tt * 128:(tt + 1) * 128, :], in_=a_.t[:]),
                   r=[a_], w=[out_db[tt]], dma=True)
            S.flush()
        st_holder[0] = gst

    if ph_stop >= 1:
        phase1()
    if ph_stop >= 2:
        phase2()
    if ph_stop >= 3:
        phase3()
    if ph_stop >= 4:
        phase4()
    if ph_stop >= 5:
        phase5()
    if S.ops:
        S.flush()
    gst.close()
    return nc


def _t5_bucket(dist):
    dist = np.asarray(dist, np.int32)
    max_exact = 16
    d_f = np.maximum(dist, 1).astype(np.float32)
    large = max_exact + (np.log(d_f / np.float32(max_exact)) / np.float32(math.log(2048 / max_exact))
                         * np.float32(32 - max_exact)).astype(np.int32)
    large = np.minimum(large, 31)
    return np.where(dist < max_exact, dist, large)


def _attn_bias(rel_bias):
    out = np.full((128, 3, 16, 256), NEGB, np.float32)
    ki = np.arange(128)[:, None]
    qi = np.arange(128)[None, :]
    for g, d in enumerate(DILS):
        jc = qi - ki
        jp = qi + 128 - ki
        bc = _t5_bucket(np.maximum(jc, 0) * d)
        bp = _t5_bucket(np.minimum(np.maximum(jp, 0), 128) * d)
        for h in range(16):
            cur = np.where(jc >= 0, rel_bias[bc, h], np.float32(NEGB))
            prv = np.where(jp <= 128, rel_bias[bp, h], np.float32(NEGB))
            out[:, g, h, 0:128] = cur
            out[:, g, h, 128:256] = prv
    return out.reshape(128, 3 * 16 * 256)


def make_in_maps(inputs, ph_stop=5, cores=range(NCORES)):
    x = np.asarray(inputs["x"], np.float32)
    f = lambda k: np.ascontiguousarray(np.asarray(inputs[k], np.float32)[0])
    w_in = f("w_in")
    common = {
        "w_in": w_in,
        "bgate": np.concatenate([f("b_igate"), f("b_fgate")])[None, :].astype(np.float32),
        "cw": np.ascontiguousarray(f("conv_w").reshape(4, 16, 128).transpose(2, 1, 0)),
        "cb": np.ascontiguousarray(f("conv_b").reshape(16, 128).T),
        "battn": _attn_bias(np.asarray(inputs["rel_bias"], np.float32)),
        "beta": np.concatenate([f("beta_attn"), f("beta_mlstm")])[None, :],
        "ident": np.eye(128, dtype=np.float32),
        "triu": np.triu(np.ones((128, 128), np.float32)),
        "trist": np.triu(np.ones((128, 128), np.float32), 1),
    }
    if ph_stop >= 3:
        common.update({
            "w_out": f("w_out"), "ln1_g": f("ln1_g")[None, :], "ln1_b": f("ln1_b")[None, :],
            "w_router": f("w_router"), "b_router": f("b_router")[None, :],
            "ecap": (np.arange(NEXP, dtype=np.float32) * CAP)[None, :],
        })
    if ph_stop >= 4:
        common.update({
            "w_up": f("w_up"), "w_down": f("w_down"), "b_down": f("b_down"),
            "bupT": np.ascontiguousarray(f("b_up").reshape(NEXP, 32, 128).transpose(2, 0, 1)),
        })
    if ph_stop >= 5:
        common.update({"ln2_g": f("ln2_g")[None, :], "ln2_b": f("ln2_b")[None, :]})
    maps = []
    for c in cores:
        b, j = c // 4, c % 4
        nvalid = 2048 * (j + 1)
        xT = np.zeros((D, NSL), np.float32)
        xT[:, NSL - nvalid:] = x[b, :nvalid, :].T
        vs = np.zeros(NSL, np.float32)
        vs[NSL - nvalid:] = 1.0
        m = dict(common)
        m["xT"] = xT
        m["valid"] = np.ascontiguousarray(vs.reshape(64, 128).T)
        m["hv"] = np.full((128, 1), 1.0 if j > 0 else 0.0, np.float32)
        if ph_stop >= 3:
            m["x_own"] = np.ascontiguousarray(x[b, 2048 * j:2048 * (j + 1), :])
        maps.append(m)
    return maps


_NC_CACHE = {}


def kernel(**inputs):
    if "nc" not in _NC_CACHE:
        _NC_CACHE["nc"] = build(5, False)
    nc = _NC_CACHE["nc"]
    maps = make_in_maps(inputs, 5)
    res = run_bass_kernel_spmd(nc, maps, core_ids=list(range(NCORES)))
    out = np.zeros((2, 8192, D), np.float32)
    for c in range(NCORES):
        b, j = c // 4, c % 4
        out[b, 2048 * j:2048 * (j + 1), :] = res.results[c]["out"]
    return out
```

```python
import math
from contextlib import ExitStack

import numpy as np
import concourse.bass as bass
import concourse.mybir as mybir
from concourse.bass_utils import run_bass_kernel_spmd

F32 = mybir.dt.float32
BF16 = mybir.dt.bfloat16
I32 = mybir.dt.int32
AF = mybir.ActivationFunctionType
ALU = mybir.AluOpType
AX = mybir.AxisListType

NCORES = 8
D = 2048
NSL = 8192
OWN0 = 6144
HAL0 = 4096
CAP = 512
NTC = CAP // 128
NEXP = 32
NSLOT = NEXP * CAP
ALPHA = 2.0 ** 0.25
NEGB = -80.0
LN_EPS = 1e-5
HN_EPS = 1e-6
IN_COLS = 7176
DILS = (1, 4, 16)
SAME_ENGINE_SYNC = True
DBG = {}


class Buf:
    __slots__ = ("name", "lw", "rd", "excl")

    def __init__(self, name):
        self.name = name
        self.lw = None
        self.rd = []
        self.excl = False


class DBuf(Buf):
    pass


class Op:
    __slots__ = ("eng", "fn", "deps", "dma", "sig", "idx", "key", "dsem", "dval", "xw", "done")


class Sched:
    ENGS = ["pe", "act", "dve", "pool", "sp"]
    EP = 20000

    def __init__(self, nc, stack):
        self.nc = nc
        self.stack = stack
        self.ops = []
        self.cnt = {e: 0 for e in self.ENGS}
        self.esems = {e: [] for e in self.ENGS}
        self.dsem_pool = []
        self.dsem_free = []
        self.keymap = {}
        self.waited = {e: {} for e in self.ENGS}

    def _dsem(self, key):
        if key not in self.keymap:
            if not self.dsem_free:
                sem = self.stack.enter_context(self.nc.semaphore("dq%d" % len(self.dsem_pool)))
                self.dsem_pool.append([sem, 0])
                self.dsem_free.append(len(self.dsem_pool) - 1)
            self.keymap[key] = self.dsem_free.pop()
        return self.keymap[key]

    def add(self, eng, fn, reads=(), writes=(), dma=False, key=None, par=False):
        op = Op()
        op.eng, op.fn, op.dma, op.sig, op.deps, op.xw, op.done = eng, fn, dma, False, [], None, False
        for b in reads:
            if b.lw is not None and not b.lw.done:
                op.deps.append((b.lw, "raw"))
            if b.excl:
                for r in b.rd:
                    if r.eng != eng and not r.done:
                        op.deps.append((r, "rar"))
        for b in writes:
            if b.lw is not None and not b.lw.done and not (par and b.lw.dma and dma):
                op.deps.append((b.lw, "waw"))
            for r in b.rd:
                if r is not op and not r.done:
                    op.deps.append((r, "war"))
        for b in reads:
            b.rd.append(op)
        for b in writes:
            b.lw = op
            b.rd = []
        if dma:
            if key is None:
                key = writes[0] if (writes and not isinstance(writes[0], DBuf)) else reads[0]
            si = self._dsem(key)
            self.dsem_pool[si][1] += 16
            op.dsem = self.dsem_pool[si][0]
            op.dval = self.dsem_pool[si][1]
        self.ops.append(op)
        return op

    def flush(self):
        nc = self.nc
        last = {}
        for op in self.ops:
            if not op.dma and op.fn is not None:
                last[op.eng] = op
        dm = [(p[0], p[1]) for p in self.dsem_pool if p[1] > 0]
        for e in self.ENGS:
            op = Op()
            op.eng, op.fn, op.dma, op.sig, op.done = e, None, False, False, False
            op.deps = [(o, "bar") for o in last.values()]
            op.xw = dm
            self.ops.append(op)
        for op in self.ops:
            nd = []
            for d, kind in op.deps:
                if d.dma:
                    nd.append(d)
                    continue
                if d.eng == op.eng and not op.dma:
                    if kind == "bar" or op.eng == "pe" or kind == "war" or not SAME_ENGINE_SYNC:
                        continue
                d.sig = True
                nd.append(d)
            op.deps = nd
        EP = self.EP
        for op in self.ops:
            if op.sig:
                self.cnt[op.eng] += 1
                op.idx = self.cnt[op.eng]
        for e in self.ENGS:
            need = (self.cnt[e] + EP - 1) // EP
            while len(self.esems[e]) < max(need, 1):
                self.esems[e].append(self.stack.enter_context(nc.semaphore("e_%s%d" % (e, len(self.esems[e])))))
        ops = self.ops

        def gen(ename):
            def body(e):
                waited = self.waited[ename]
                for op in ops:
                    if op.eng != ename:
                        continue
                    need = {}
                    for d in op.deps:
                        if d.dma:
                            sem, val = d.dsem, d.dval
                        else:
                            sem = self.esems[d.eng][(d.idx - 1) // EP]
                            val = (d.idx - 1) % EP + 1
                        if need.get(sem, (None, 0))[1] < val:
                            need[sem] = (sem, val)
                    if op.xw:
                        for sem, c in op.xw:
                            if need.get(sem, (None, 0))[1] < c:
                                need[sem] = (sem, c)
                    for sem, val in need.values():
                        if waited.get(sem, 0) >= val:
                            continue
                        e.wait_ge(sem, val)
                        waited[sem] = val
                    if op.fn is None:
                        continue
                    ins = op.fn(e)
                    if op.dma:
                        ins.then_inc(op.dsem, 16)
                    elif op.sig:
                        ins.then_inc(self.esems[ename][(op.idx - 1) // EP], 1)
            return body

        with nc.Block() as block:
            block.tensor(gen("pe"))
            block.scalar(gen("act"))
            block.vector(gen("dve"))
            block.gpsimd(gen("pool"))
            block.sync(gen("sp"))
        for op in self.ops:
            op.done = True
        self.ops = []
        self.keymap = {}
        self.dsem_free = list(range(len(self.dsem_pool)))


class TB:
    def __init__(self, t, name):
        self.t = t
        self.b = Buf(name)


class Ctx:
    pass


def _bs(xs):
    return [x.b if isinstance(x, TB) else x for x in xs]


def build(ph_stop=5, debug=False, cut=99):
    nc = bass.Bass("TRN2", target_bir_lowering=False)
    gst = ExitStack()
    S = Sched(nc, gst)

    def din(name, shape, dt=F32):
        return nc.dram_tensor(name, shape, dt, kind="ExternalInput")

    def dscr(name, shape, dt):
        return nc.dram_tensor(name, shape, dt, kind="ExternalOutput" if debug else "Internal")

    def op(eng, fn, r=(), w=(), **kw):
        return S.add(eng, fn, _bs(r), _bs(w), **kw)

    st_holder = [gst]

    def sb(name, shape, dt):
        return TB(st_holder[0].enter_context(nc.sbuf_tensor(name, shape, dt)), name)

    xT = din("xT", [D, NSL])
    valid = din("valid", [128, 64])
    hv = din("hv", [128, 1])
    w_in = din("w_in", [D, IN_COLS])
    bgate = din("bgate", [1, 8])
    cw = din("cw", [128, 16, 4])
    cbias = din("cb", [128, 16])
    battn = din("battn", [128, 3 * 16 * 256])
    beta = din("beta", [1, D])
    ident = din("ident", [128, 128])
    triu = din("triu", [128, 128])
    trist = din("trist", [128, 128])
    ynT_d = dscr("ynT_d", [16, 128, 2048], BF16)
    v_d = nc.dram_tensor("v_d", [4096, 260], BF16, kind="Internal")
    ynT_db = DBuf("ynT_d")
    v_db = DBuf("v_d")
    if ph_stop >= 3:
        x_own = din("x_own", [2048, D])
        w_out = din("w_out", [D, D])
        ln1_g = din("ln1_g", [1, D])
        ln1_b = din("ln1_b", [1, D])
        w_router = din("w_router", [D, NEXP])
        b_router = din("b_router", [1, NEXP])
        ecap = din("ecap", [1, NEXP])
        h1_d = dscr("h1_d", [2048, D], F32)
        xs_d = nc.dram_tensor("xs_d", [NSLOT + 1, D], BF16, kind="Internal")
        h1_db = DBuf("h1_d")
        xs_db = DBuf("xs_d")
        if debug:
            dbg_r = nc.dram_tensor("dbg_r", [128, 16, 40], F32, kind="ExternalOutput")
            dbg_rb = DBuf("dbg_r")
    if ph_stop >= 4:
        w_up = din("w_up", [NEXP, D, 2 * D])
        bupT = din("bupT", [128, NEXP, 32])
        w_down = din("w_down", [NEXP, D, D])
        b_down = din("b_down", [NEXP, D])
        ys_d = nc.dram_tensor("ys_d", [NSLOT + 1, D], F32, kind="Internal")
        ys_db = DBuf("ys_d")
    if ph_stop >= 5:
        ln2_g = din("ln2_g", [1, D])
        ln2_b = din("ln2_b", [1, D])
        out_d = nc.dram_tensor("out", [2048, D], F32, kind="ExternalOutput")
        out_db = [DBuf("out%d" % i) for i in range(16)]

    xT_r = xT.ap().rearrange("(kc p) s -> p kc s", p=128)
    w_in_r = w_in.ap().rearrange("(kc p) c -> p kc c", p=128)

    def bcast(dt_, n):
        return bass.AP(dt_, 0, [[0, 128], [1, n]])

    ps = []
    for i in range(8):
        t = gst.enter_context(nc.psum_tensor("psb%d" % i, [128, 512], F32))
        ps.append(TB(t, "psb%d" % i))
        ps[-1].b.excl = True
    psb16 = [p.t.bitcast(BF16) for p in ps]

    ident_f = sb("ident_f", [128, 128], F32)
    ident_b = sb("ident_b", [128, 128], BF16)
    triu_f = sb("triu_f", [128, 128], F32)
    trist_f = sb("trist_f", [128, 128], F32)
    ones_f = sb("ones_f", [128, 128], F32)
    ones_b = sb("ones_b", [1, 128], BF16)
    hv_s = sb("hv_s", [128, 1], F32)
    valid_s = sb("valid_s", [128, 64], F32)
    beta_bc = sb("beta_bc", [128, D], F32)
    cw_s = sb("cw_s", [128, 16, 4], F32)
    cb_s = sb("cb_s", [128, 16], F32)
    bg_bc = sb("bg_bc", [128, 8], F32)
    offs_i = sb("offs_i", [128, 16, 4], I32)
    gk = sb("gk", [128, 16, 4], F32)

    op("sp", lambda e: e.dma_start(out=ident_f.t[:], in_=ident.ap()), w=[ident_f], dma=True)
    op("sp", lambda e: e.dma_start(out=triu_f.t[:], in_=triu.ap()), w=[triu_f], dma=True)
    op("sp", lambda e: e.dma_start(out=trist_f.t[:], in_=trist.ap()), w=[trist_f], dma=True)
    op("sp", lambda e: e.dma_start(out=hv_s.t[:], in_=hv.ap()), w=[hv_s], dma=True)
    op("sp", lambda e: e.dma_start(out=valid_s.t[:], in_=valid.ap()), w=[valid_s], dma=True)
    op("sp", lambda e: e.dma_start(out=beta_bc.t[:], in_=bcast(beta, D)), w=[beta_bc], dma=True)
    op("sp", lambda e: e.dma_start(out=cw_s.t[:], in_=cw.ap()), w=[cw_s], dma=True)
    op("sp", lambda e: e.dma_start(out=cb_s.t[:], in_=cbias.ap()), w=[cb_s], dma=True)
    op("sp", lambda e: e.dma_start(out=bg_bc.t[:], in_=bcast(bgate, 8)), w=[bg_bc], dma=True)
    op("dve", lambda e: e.tensor_copy(out=ident_b.t[:], in_=ident_f.t[:]), r=[ident_f], w=[ident_b])
    op("pool", lambda e: e.memset(ones_f.t[:], 1.0), w=[ones_f])
    op("pool", lambda e: e.memset(ones_b.t[:], 1.0), w=[ones_b])

    def phase1():
        with ExitStack() as st:
            st_holder[0] = st
            expB = sb("expB", [128, 3 * 16 * 256], BF16)
            tmpBs = [sb("tmpB%d" % i, [128, 1024], F32) for i in range(2)]
            for g in range(12):
                tmpB = tmpBs[g % 2]
                op("sp", lambda e, g=g, tmpB=tmpB: e.dma_start(out=tmpB.t[:], in_=battn.ap()[:, g * 1024:(g + 1) * 1024]),
                   w=[tmpB], dma=True)
                op("act", lambda e, g=g, tmpB=tmpB: e.activation(out=expB.t[:, g * 1024:(g + 1) * 1024], in_=tmpB.t[:],
                                                      func=AF.Exp), r=[tmpB], w=[expB])
            Wq = sb("Wq", [128, 16, 256], BF16)
            Wk = sb("Wk", [128, 16, 256], BF16)
            Wv = sb("Wv", [128, 16, 256], BF16)
            kT = sb("kT", [128, 2, 4096], BF16)
            qT = sb("qT", [128, 2, 2048], BF16)
            xg = [sb("xg%d" % i, [128, 16, 512], BF16) for i in range(2)]
            vt = [sb("vt%d" % i, [128, 4, 4, 65], BF16) for i in range(2)]
            NBLK = 17 + 20 + 32
            vaug = sb("vaug", [128, NBLK, 4, 65], BF16)
            acc = sb("acc", [128, 4, 2048], F32)
            E = [sb("E%d" % i, [128, 256], BF16) for i in range(3)]
            PT = [sb("PT%d" % i, [128, 256], BF16) for i in range(3)]
            accB = [Buf("accP0"), Buf("accP1")]
            dn = sb("dn", [128, 4], F32)
            d2 = sb("d2", [128, 4], F32)
            sq = sb("sq", [128, 4, 65], F32)
            ssq = sb("ssq", [128, 4], F32)
            sc = sb("sc", [128, 4], F32)
            yn_tm = [sb("yn_tm%d" % i, [128, 256], BF16) for i in range(2)]
            ynTs = sb("ynTs", [128, 2, 1024], BF16)

            for v_ in vt:
                op("pool", lambda e, v_=v_: e.memset(v_.t[:], 1.0), w=[v_])
            blk = {}
            nb = 0
            for g, d in enumerate(DILS):
                first = 16 // d
                for r in range(d):
                    for mb in range(first - 1, 2 * first):
                        blk[(g, r, mb)] = nb
                        nb += 1
            assert nb == NBLK

            for hg in range(4):
                if cut <= 0:
                    break
                cq, ck, cv = 256 * hg, 1024 + 256 * hg, 2048 + 256 * hg
                op("pool", lambda e, c0=cq: e.dma_start(out=Wq.t[:], in_=w_in_r[:, :, c0:c0 + 256]), w=[Wq], dma=True)
                op("pool", lambda e, c0=ck: e.dma_start(out=Wk.t[:], in_=w_in_r[:, :, c0:c0 + 256]), w=[Wk], dma=True)
                op("pool", lambda e, c0=cv: e.dma_start(out=Wv.t[:], in_=w_in_r[:, :, c0:c0 + 256]), w=[Wv], dma=True)
                op("dve", lambda e: e.memset(acc.t[:], 0.0), w=[acc] + accB)
                for sg in range(8):
                    s0 = HAL0 + 512 * sg
                    x_ = xg[sg % 2]
                    op("pool", lambda e, x_=x_, s0=s0: e.dma_start(out=x_.t[:], in_=xT_r[:, :, s0:s0 + 512]),
                       w=[x_], dma=True)
                    for pi in range(2):
                        pk = ps[pi]

                        def mmk(e, pk=pk, x_=x_, pi=pi):
                            for kc in range(16):
                                ins = e.matmul(pk.t[:, :], Wk.t[:, kc, pi * 128:(pi + 1) * 128], x_.t[:, kc, :],
                                               start=(kc == 0), stop=(kc == 15))
                            return ins
                        op("pe", mmk, r=[Wk, x_], w=[pk])
                        op("act", lambda e, pk=pk, pi=pi, sg=sg: e.copy(out=kT.t[:, pi, sg * 512:(sg + 1) * 512],
                                                                       in_=pk.t[:, :]), r=[pk], w=[kT])
                    if sg >= 4:
                        for pi in range(2):
                            pq = ps[2 + pi]

                            def mmq(e, pq=pq, x_=x_, pi=pi):
                                for kc in range(16):
                                    ins = e.matmul(pq.t[:, :], Wq.t[:, kc, pi * 128:(pi + 1) * 128], x_.t[:, kc, :],
                                                   start=(kc == 0), stop=(kc == 15))
                                return ins
                            op("pe", mmq, r=[Wq, x_], w=[pq])
                            op("act", lambda e, pq=pq, pi=pi, sg=sg: e.activation(
                                out=qT.t[:, pi, (sg - 4) * 512:(sg - 3) * 512], in_=pq.t[:, :], func=AF.Copy,
                                scale=0.125), r=[pq], w=[qT])
                    v_ = vt[sg % 2]
                    for tl in range(4):
                        pv = ps[4 + tl % 2]

                        def mmv(e, pv=pv, x_=x_, tl=tl):
                            for kc in range(16):
                                ins = e.matmul(pv.t[:, 0:256], x_.t[:, kc, tl * 128:(tl + 1) * 128], Wv.t[:, kc, :],
                                               start=(kc == 0), stop=(kc == 15))
                            return ins
                        op("pe", mmv, r=[Wv, x_], w=[pv])
                        op("dve", lambda e, pv=pv, v_=v_, tl=tl: e.tensor_copy(
                            out=v_.t[:, tl, :, 0:64], in_=pv.t[:, 0:256].rearrange("p (h c) -> p h c", h=4)),
                           r=[pv], w=[v_])
                    u0 = 512 * sg
                    op("sp", lambda e, v_=v_, u0=u0: e.dma_start(
                        out=v_d.ap()[u0:u0 + 512, :].rearrange("(t p) c -> p t c", p=128),
                        in_=v_.t[:].rearrange("p t h c -> p t (h c)")),
                       r=[v_], w=[v_db], dma=True)
                if cut <= 1:
                    continue
                for g, d in enumerate(DILS):
                    first = 16 // d
                    nblk = first + 1
                    src = v_d.ap().rearrange("(mb p r) c -> r p mb c", p=128, r=d)
                    for r in range(d):
                        b0 = blk[(g, r, first - 1)]
                        op("sp", lambda e, src=src, r=r, b0=b0, nblk=nblk, first=first: e.dma_start(
                            out=vaug.t[:, b0:b0 + nblk].rearrange("p b h c -> p b (h c)"), in_=src[r][:, first - 1:2 * first]),
                           r=[v_db], w=[vaug], dma=True, par=True)
                if cut <= 2:
                    continue
                iters = []
                for hl in range(4):
                    pi, p0 = hl // 2, 64 * (hl % 2)
                    h = 4 * hg + hl
                    for g, d in enumerate(DILS):
                        first = 16 // d
                        for r in range(d):
                            for mb in range(first - 1, 2 * first):
                                halo = (mb == first - 1)
                                last = (mb == 2 * first - 1)
                                if halo:
                                    qb0, nq, bc0 = mb + 1, 1, 128
                                elif last:
                                    qb0, nq, bc0 = mb, 1, 0
                                else:
                                    qb0, nq, bc0 = mb, 2, 0
                                N = 128 * nq
                                ks = r + d * 128 * mb
                                qs = r + d * 128 * qb0 - 2048
                                iters.append(dict(
                                    hl=hl, pi=pi, p0=p0, halo=halo, N=N,
                                    ksl=slice(ks, ks + d * 127 + 1, d), qsl=slice(qs, qs + d * (N - 1) + 1, d),
                                    bcol=(g * 16 + h) * 256 + bc0, bi=blk[(g, r, mb)]))

                def stageA(i):
                    c = iters[i]
                    pS, E_, P_ = ps[i % 3], E[i % 3], PT[i % 3]
                    N, p0, pi = c["N"], c["p0"], c["pi"]
                    op("pe", lambda e: e.matmul(pS.t[:, 0:N], kT.t[p0:p0 + 64, pi, c["ksl"]],
                                                qT.t[p0:p0 + 64, pi, c["qsl"]], start=True, stop=True),
                       r=[kT, qT], w=[pS])
                    op("act", lambda e: e.activation(out=E_.t[:, 0:N], in_=pS.t[:, 0:N], func=AF.Exp),
                       r=[pS], w=[E_])
                    bcol = c["bcol"]
                    if c["halo"]:
                        op("dve", lambda e: e.scalar_tensor_tensor(
                            out=P_.t[:, 0:N], in0=E_.t[:, 0:N], scalar=hv_s.t[:, 0:1],
                            in1=expB.t[:, bcol:bcol + N], op0=ALU.mult, op1=ALU.mult), r=[E_, expB, hv_s], w=[P_])
                    else:
                        op("dve", lambda e: e.tensor_tensor(out=P_.t[:, 0:N], in0=E_.t[:, 0:N],
                                                            in1=expB.t[:, bcol:bcol + N], op=ALU.mult),
                           r=[E_, expB], w=[P_])

                def stageB(i):
                    c = iters[i]
                    pO, P_ = ps[3 + i % 3], PT[i % 3]
                    N, hl = c["N"], c["hl"]
                    op("pe", lambda e: e.matmul(pO.t[0:65, 0:N], vaug.t[:, c["bi"], hl, 0:65], P_.t[:, 0:N],
                                                start=True, stop=True), r=[vaug, P_], w=[pO])
                    op("dve", lambda e: e.tensor_tensor(out=acc.t[0:65, hl, c["qsl"]], in0=acc.t[0:65, hl, c["qsl"]],
                                                        in1=pO.t[0:65, 0:N], op=ALU.add), r=[pO, acc], w=[acc])
                LOOK = 2
                for i in range(len(iters) + LOOK):
                    if i < len(iters):
                        stageA(i)
                    if i - LOOK >= 0:
                        stageB(i - LOOK)
                if cut <= 3:
                    continue
                pF = ps[6]
                pFv = pF.t[:, 0:512].rearrange("p (h c) -> p h c", h=4)
                pY = ps[7]
                pYb = psb16[7]
                for t in range(16):
                    def trF(e, t=t):
                        for hl in range(4):
                            ins = e.transpose(pF.t[:, hl * 128:(hl + 1) * 128], acc.t[:, hl, t * 128:(t + 1) * 128],
                                              ident_f.t[:, :])
                        return ins
                    op("pe", trF, r=[acc, ident_f] + accB, w=[pF])
                    if cut <= 4:
                        continue
                    op("act", lambda e: e.activation(out=sq.t[:], in_=pFv[:, :, 0:65],
                                                     func=AF.Square), r=[pF], w=[sq])
                    op("dve", lambda e: e.tensor_copy(out=dn.t[:], in_=pFv[:, :, 64]), r=[pF], w=[dn])
                    op("dve", lambda e: e.tensor_tensor(out=d2.t[:], in0=dn.t[:], in1=dn.t[:], op=ALU.mult),
                       r=[dn], w=[d2])
                    op("dve", lambda e: e.tensor_reduce(out=ssq.t[:], in_=sq.t[:, :, 0:64], axis=AX.X, op=ALU.add),
                       r=[sq], w=[ssq])
                    op("dve", lambda e: e.scalar_tensor_tensor(out=sc.t[:], in0=d2.t[:], scalar=64.0 * HN_EPS,
                                                               in1=ssq.t[:], op0=ALU.mult, op1=ALU.add),
                       r=[d2, ssq], w=[sc])
                    op("act", lambda e: e.activation(out=sc.t[:], in_=sc.t[:], func=AF.Sqrt, scale=1.0 / 64.0),
                       r=[sc], w=[sc])
                    op("dve", lambda e: e.reciprocal(out=sc.t[:], in_=sc.t[:]), r=[sc], w=[sc])
                    if cut <= 5:
                        continue
                    y_ = yn_tm[t % 2]
                    for hl in range(4):
                        hh = 4 * hg + hl
                        op("dve", lambda e, y_=y_, hl=hl, hh=hh: e.scalar_tensor_tensor(
                            out=y_.t[:, hl * 64:(hl + 1) * 64], in0=pFv[:, hl, 0:64], scalar=sc.t[:, hl:hl + 1],
                            in1=beta_bc.t[:, hh * 64:(hh + 1) * 64], op0=ALU.mult, op1=ALU.mult),
                           r=[pF, sc, beta_bc], w=[y_])

                    if cut <= 6:
                        continue

                    def trY(e, y_=y_):
                        for cc in range(2):
                            ins = e.transpose(pYb[:, cc * 128:(cc + 1) * 128], y_.t[:, cc * 128:(cc + 1) * 128],
                                              ident_b.t[:, :])
                        return ins
                    op("pe", trY, r=[y_, ident_b], w=[pY])
                    op("act", lambda e, t=t: e.copy(out=ynTs.t[:, :, (t % 8) * 128:(t % 8 + 1) * 128],
                                                    in_=pYb[:, 0:256].rearrange("p (c t) -> p c t", c=2)),
                       r=[pY], w=[ynTs])
                    if t % 8 == 7:
                        th = (t // 8) * 1024
                        op("sp", lambda e, hg=hg, th=th: e.dma_start(
                            out=ynT_d.ap()[2 * hg:2 * hg + 2, :, th:th + 1024].rearrange("c p t -> p c t"),
                            in_=ynTs.t[:]), r=[ynTs], w=[ynT_db], dma=True, par=True)
            S.flush()
        st_holder[0] = gst

    def phase2():
        with ExitStack() as st:
            st_holder[0] = st
            xg = [sb("m_xg%d" % i, [128, 16, 512], BF16) for i in range(2)]
            wp = [sb("m_wp%d" % i, [128, 16, 256], BF16) for i in range(3)]
            wg = sb("m_wg", [128, 16, 8], BF16)
            kpre = sb("kpre", [128, 8, 515], BF16)
            qpre = sb("qpre", [128, 8, 515], BF16)
            kTm = sb("kTm", [128, 8, 512], BF16)
            qTm = sb("qTm", [128, 8, 512], BF16)
            ctmp = [sb("ctmp%d" % i, [128, 512], F32) for i in range(2)]
            v_tm2 = [sb("v_tm%d" % i, [128, 4, 4, 257], BF16) for i in range(2)]
            og2 = [sb("og%d" % i, [128, 4, 1024], BF16) for i in range(2)]
            gz2 = [sb("gz%d" % i, [128, 4, 8], F32) for i in range(2)]
            ex = sb("ex", [128, 4, 4], F32)
            lfn = sb("lfn", [128, 4, 4], F32)
            gt = [sb("gt%d" % i, [128, 4], F32) for i in range(2)]
            a_s = [sb("a_s%d" % i, [128, 4], F32) for i in range(2)]
            eb = [sb("eb%d" % i, [128, 4], F32) for i in range(2)]
            eG = [sb("eG%d" % i, [128, 4], F32) for i in range(2)]
            Cst = sb("Cst", [128, 4, 2, 257], F32)
            Cb = sb("Cb", [128, 4, 2, 257], BF16)
            ak = [sb("ak%d" % i, [128, 256], BF16) for i in range(2)]
            WT = [sb("WT%d" % i, [128, 128], BF16) for i in range(2)]
            dd = [sb("dd%d" % i, [128, 1], F32) for i in range(2)]
            hm = [sb("hm%d" % i, [128, 256], F32) for i in range(2)]
            sqm = sb("sqm", [128, 256], F32)
            ssm = [sb("ssm%d" % i, [128, 1], F32) for i in range(2)]
            ynm = [sb("ynm%d" % i, [128, 256], BF16) for i in range(2)]
            ynTs = sb("m_ynTs", [128, 8, 512], BF16)

            op("pool", lambda e: e.memset(kpre.t[:], 0.0), w=[kpre])
            op("pool", lambda e: e.memset(qpre.t[:], 0.0), w=[qpre])
            for v_ in v_tm2:
                op("pool", lambda e, v_=v_: e.memset(v_.t[:], 1.0), w=[v_])
            op("pool", lambda e: e.memset(Cst.t[:], 0.0), w=[Cst])
            op("pool", lambda e: e.memset(Cb.t[:], 0.0), w=[Cb])
            op("pool", lambda e: e.dma_start(out=wg.t[:], in_=w_in_r[:, :, 7168:7176]), w=[wg], dma=True)

            pK = ps[4]
            pKb = psb16[4]
            pS = ps[5]
            pG = ps[2]
            pN = ps[3]
            pH = ps[6]
            pU = ps[7]
            cnt = {"wp": 0, "fm": 0, "tm": 0, "ci": 0}

            def proj_units(gi):
                units = []
                own = gi >= 12
                qcomp = gi >= 11
                s0 = 512 * gi
                x_ = xg[gi % 2]
                v_tm, og, gz = v_tm2[gi % 2], og2[gi % 2], gz2[gi % 2]

                def u_x():
                    op("pool", lambda e: e.dma_start(out=x_.t[:], in_=xT_r[:, :, s0:s0 + 512]), w=[x_], dma=True)
                units.append(u_x)
                pieces = [("k", i, 4096 + 256 * i) for i in range(4)] + [("v", i, 5120 + 256 * i) for i in range(4)]
                if qcomp:
                    pieces += [("q", i, 3072 + 256 * i) for i in range(4)]
                if own:
                    pieces += [("o", i, 6144 + 256 * i) for i in range(4)]
                for kind, i, c0 in pieces:
                    hold = {}

                    def u_w(hold=hold, c0=c0):
                        w_ = wp[cnt["wp"] % 3]
                        cnt["wp"] += 1
                        hold["w"] = w_
                        op("pool", lambda e: e.dma_start(out=w_.t[:], in_=w_in_r[:, :, c0:c0 + 256]), w=[w_], dma=True)
                    units.append(u_w)
                    if kind in ("k", "q"):
                        pre = kpre if kind == "k" else qpre
                        for cc in range(2):
                            def u_fm(hold=hold, pre=pre, cc=cc, i=i):
                                w_ = hold["w"]
                                pp = ps[cnt["fm"] % 2]
                                cnt["fm"] += 1

                                def mmf(e):
                                    for kc in range(16):
                                        ins = e.matmul(pp.t[:, :], w_.t[:, kc, cc * 128:(cc + 1) * 128], x_.t[:, kc, :],
                                                       start=(kc == 0), stop=(kc == 15))
                                    return ins
                                op("pe", mmf, r=[w_, x_], w=[pp])
                                ch = 2 * i + cc
                                op("act", lambda e: e.copy(out=pre.t[:, ch, 3:515], in_=pp.t[:, :]), r=[pp], w=[pre])
                            units.append(u_fm)
                    else:
                        for tl in range(4):
                            def u_tm(hold=hold, kind=kind, tl=tl, i=i):
                                w_ = hold["w"]
                                pp = ps[cnt["tm"] % 2]
                                cnt["tm"] += 1

                                def mmt(e):
                                    for kc in range(16):
                                        ins = e.matmul(pp.t[:, 0:256], x_.t[:, kc, tl * 128:(tl + 1) * 128], w_.t[:, kc, :],
                                                       start=(kc == 0), stop=(kc == 15))
                                    return ins
                                op("pe", mmt, r=[w_, x_], w=[pp])
                                if kind == "v":
                                    op("dve", lambda e: e.tensor_copy(out=v_tm.t[:, tl, i, 0:256], in_=pp.t[:, 0:256]),
                                       r=[pp], w=[v_tm])
                                else:
                                    op("act", lambda e: e.activation(out=og.t[:, tl, i * 256:(i + 1) * 256],
                                                                     in_=pp.t[:, 0:256], func=AF.Sigmoid),
                                       r=[pp], w=[og])
                            units.append(u_tm)
                for tl in range(4):
                    def u_g(tl=tl):
                        pp = ps[cnt["tm"] % 2]
                        cnt["tm"] += 1

                        def mmg(e):
                            for kc in range(16):
                                ins = e.matmul(pp.t[:, 0:8], x_.t[:, kc, tl * 128:(tl + 1) * 128], wg.t[:, kc, :],
                                               start=(kc == 0), stop=(kc == 15))
                            return ins
                        op("pe", mmg, r=[wg, x_], w=[pp])
                        op("dve", lambda e: e.tensor_tensor(out=gz.t[:, tl, :], in0=pp.t[:, 0:8], in1=bg_bc.t[:, :],
                                                            op=ALU.add), r=[pp, bg_bc], w=[gz])
                    units.append(u_g)
                return units

            def group_prep(gi):
                own = gi >= 12
                qcomp = gi >= 11
                gz = gz2[gi % 2]
                op("act", lambda e: e.activation(out=ex.t[:], in_=gz.t[:, :, 4:8], func=AF.Exp, scale=-1.0),
                   r=[gz], w=[ex])
                op("act", lambda e: e.activation(out=lfn.t[:], in_=ex.t[:], func=AF.Ln, bias=1.0, scale=1.0),
                   r=[ex], w=[lfn])
                convs = [("k", kpre, kTm, 8)]
                if own:
                    convs.append(("q", qpre, qTm, 0))
                for kind, pre, dst, cof in convs:
                    for ch in range(8):
                        ct = ctmp[ch % 2]
                        wch = cof + ch
                        op("dve", lambda e, ct=ct, pre=pre, ch=ch, wch=wch: e.tensor_scalar(
                            out=ct.t[:], in0=pre.t[:, ch, 0:512], scalar1=cw_s.t[:, wch, 0:1], scalar2=None,
                            op0=ALU.mult), r=[pre, cw_s], w=[ct])
                        for j in range(1, 4):
                            op("dve", lambda e, ct=ct, pre=pre, ch=ch, wch=wch, j=j: e.scalar_tensor_tensor(
                                out=ct.t[:], in0=pre.t[:, ch, j:j + 512], scalar=cw_s.t[:, wch, j:j + 1], in1=ct.t[:],
                                op0=ALU.mult, op1=ALU.add), r=[pre, cw_s, ct], w=[ct])
                        op("act", lambda e, ct=ct, dst=dst, ch=ch, wch=wch: e.activation(
                            out=dst.t[:, ch, :], in_=ct.t[:], func=AF.Silu, bias=cb_s.t[:, wch:wch + 1], scale=1.0),
                           r=[ct, cb_s], w=[dst])
                op("act", lambda e: e.copy(out=kpre.t[:, :, 0:3], in_=kpre.t[:, :, 512:515]), r=[kpre], w=[kpre])
                if qcomp:
                    op("act", lambda e: e.copy(out=qpre.t[:, :, 0:3], in_=qpre.t[:, :, 512:515]), r=[qpre], w=[qpre])

            def chunk_units(gi):
                units = []
                own = gi >= 12
                v_tm, og, gz = v_tm2[gi % 2], og2[gi % 2], gz2[gi % 2]
                for ci in range(4):
                    gc = 4 * gi + ci
                    cs = slice(ci * 128, (ci + 1) * 128)
                    gt_, a_, eb_, eG_ = gt[gc % 2], a_s[gc % 2], eb[gc % 2], eG[gc % 2]

                    def u_gate(ci=ci, gc=gc, gt_=gt_, a_=a_, eb_=eb_, eG_=eG_):
                        def mmc(e):
                            e.matmul(pG.t[:, 256:260], triu_f.t[:, :], lfn.t[:, ci, :], start=True, stop=True)
                            return e.matmul(pG.t[:, 260:264], ones_f.t[:, :], lfn.t[:, ci, :], start=True, stop=True)
                        op("pe", mmc, r=[triu_f, ones_f, lfn], w=[pG])
                        op("dve", lambda e: e.tensor_tensor(out=gt_.t[:], in0=gz.t[:, ci, 0:4], in1=pG.t[:, 256:260],
                                                            op=ALU.add), r=[gz, pG], w=[gt_])
                        op("act", lambda e: e.activation(out=a_.t[:], in_=gt_.t[:], func=AF.Exp), r=[gt_], w=[a_])
                        op("dve", lambda e: e.tensor_scalar(out=a_.t[:], in0=a_.t[:], scalar1=valid_s.t[:, gc:gc + 1],
                                                            scalar2=0.0625, op0=ALU.mult, op1=ALU.mult),
                           r=[a_, valid_s], w=[a_])
                        op("act", lambda e: e.activation(out=eG_.t[:], in_=pG.t[:, 260:264], func=AF.Exp, scale=-1.0),
                           r=[pG], w=[eG_])
                        if own:
                            op("act", lambda e: e.activation(out=eb_.t[:], in_=pG.t[:, 256:260], func=AF.Exp,
                                                             scale=-1.0), r=[pG], w=[eb_])
                    for h in range(4):
                        def u_ch(ci=ci, gc=gc, cs=cs, h=h, a_=a_, eb_=eb_, eG_=eG_, first=(h == 0), u_gate=u_gate):
                            if first:
                                u_gate()
                            k_ = cnt["ci"] % 2
                            cnt["ci"] += 1
                            ak_, WT_, dd_, hm_, ss_, yn_ = ak[k_], WT[k_], dd[k_], hm[k_], ssm[k_], ynm[k_]

                            def trk(e):
                                for cc in range(2):
                                    ins = e.transpose(pKb[:, cc * 128:(cc + 1) * 128], kTm.t[:, 2 * h + cc, cs],
                                                      ident_b.t[:, :])
                                return ins
                            op("pe", trk, r=[kTm, ident_b], w=[pK])
                            op("dve", lambda e: e.tensor_scalar(out=ak_.t[:], in0=pKb[:, 0:256], scalar1=a_.t[:, h:h + 1],
                                                                scalar2=None, op0=ALU.mult), r=[pK, a_], w=[ak_])
                            yield
                            if own:
                                def mms(e):
                                    for cc in range(2):
                                        ins = e.matmul(pS.t[:, 0:128], kTm.t[:, 2 * h + cc, cs], qTm.t[:, 2 * h + cc, cs],
                                                       start=(cc == 0), stop=(cc == 1))
                                    return ins
                                op("pe", mms, r=[kTm, qTm], w=[pS])
                                op("dve", lambda e: e.scalar_tensor_tensor(
                                    out=WT_.t[:], in0=pS.t[:, 0:128], scalar=a_.t[:, h:h + 1], in1=triu_f.t[:, :],
                                    op0=ALU.mult, op1=ALU.mult), r=[pS, a_, triu_f], w=[WT_])
                                yield

                                def mmh(e):
                                    e.matmul(pH.t[:, 0:257], WT_.t[:, :], v_tm.t[:, ci, h, :], start=True, stop=False)
                                    e.matmul(pH.t[:, 0:257], qTm.t[:, 2 * h, cs], Cb.t[:, h, 0, :], start=False, stop=False)
                                    return e.matmul(pH.t[:, 0:257], qTm.t[:, 2 * h + 1, cs], Cb.t[:, h, 1, :],
                                                    start=False, stop=True)
                                op("pe", mmh, r=[WT_, v_tm, qTm, Cb], w=[pH])
                                op("dve", lambda e: e.tensor_scalar(out=dd_.t[:], in0=pH.t[:, 256:257],
                                                                    scalar1=eb_.t[:, h:h + 1], scalar2=None, op0=ALU.mult),
                                   r=[pH, eb_], w=[dd_])
                                op("dve", lambda e: e.scalar_tensor_tensor(out=dd_.t[:], in0=dd_.t[:], scalar=-1.0,
                                                                           in1=dd_.t[:], op0=ALU.mult, op1=ALU.max),
                                   r=[dd_], w=[dd_])
                                op("dve", lambda e: e.tensor_scalar(out=dd_.t[:], in0=dd_.t[:], scalar1=1.0, scalar2=None,
                                                                    op0=ALU.max), r=[dd_], w=[dd_])
                                op("dve", lambda e: e.reciprocal(out=dd_.t[:], in_=dd_.t[:]), r=[dd_], w=[dd_])
                                op("dve", lambda e: e.tensor_tensor(out=dd_.t[:], in0=dd_.t[:], in1=eb_.t[:, h:h + 1],
                                                                    op=ALU.mult), r=[dd_, eb_], w=[dd_])
                                op("dve", lambda e: e.scalar_tensor_tensor(
                                    out=hm_.t[:], in0=pH.t[:, 0:256], scalar=dd_.t[:, 0:1],
                                    in1=og.t[:, ci, h * 256:(h + 1) * 256], op0=ALU.mult, op1=ALU.mult),
                                   r=[pH, dd_, og], w=[hm_])
                                op("act", lambda e: e.activation(out=sqm.t[:], in_=hm_.t[:], func=AF.Square),
                                   r=[hm_], w=[sqm])
                                op("dve", lambda e: e.tensor_reduce(out=ss_.t[:], in_=sqm.t[:], axis=AX.X, op=ALU.add),
                                   r=[sqm], w=[ss_])
                                op("dve", lambda e: e.tensor_scalar(out=ss_.t[:], in0=ss_.t[:], scalar1=1.0 / 256.0,
                                                                    scalar2=HN_EPS, op0=ALU.mult, op1=ALU.add),
                                   r=[ss_], w=[ss_])
                                op("act", lambda e: e.activation(out=ss_.t[:], in_=ss_.t[:], func=AF.Sqrt),
                                   r=[ss_], w=[ss_])
                                op("dve", lambda e: e.reciprocal(out=ss_.t[:], in_=ss_.t[:]), r=[ss_], w=[ss_])
                                op("dve", lambda e: e.scalar_tensor_tensor(
                                    out=yn_.t[:], in0=hm_.t[:], scalar=ss_.t[:, 0:1],
                                    in1=beta_bc.t[:, 1024 + h * 256:1024 + (h + 1) * 256], op0=ALU.mult, op1=ALU.mult),
                                   r=[hm_, ss_, beta_bc], w=[yn_])
                                yield

                                def try_(e):
                                    for cc in range(2):
                                        ins = e.transpose(pKb[:, 256 + cc * 128:256 + (cc + 1) * 128],
                                                          yn_.t[:, cc * 128:(cc + 1) * 128], ident_b.t[:, :])
                                    return ins
                                op("pe", try_, r=[yn_, ident_b], w=[pK])
                                op("act", lambda e: e.copy(out=ynTs.t[:, 2 * h:2 * h + 2, cs],
                                                           in_=pKb[:, 256:512].rearrange("p (c t) -> p c t", c=2)),
                                   r=[pK], w=[ynTs])

                            def mmu(e):
                                for cc in range(2):
                                    e.matmul(pU.t[:, cc * 256:(cc + 1) * 256], ak_.t[:, cc * 128:(cc + 1) * 128],
                                             v_tm.t[:, ci, h, 0:256], start=True, stop=True)
                                for cc in range(2):
                                    ins = e.matmul(pN.t[:, 300 + cc:301 + cc], ak_.t[:, cc * 128:(cc + 1) * 128],
                                                   v_tm.t[:, ci, h, 256:257], start=True, stop=True)
                                return ins
                            op("pe", mmu, r=[ak_, v_tm], w=[pU, pN])
                            op("dve", lambda e: e.tensor_scalar(
                                out=Cst.t[:, h].rearrange("p c n -> p (c n)"), in0=Cst.t[:, h].rearrange("p c n -> p (c n)"),
                                scalar1=eG_.t[:, h:h + 1], scalar2=None, op0=ALU.mult), r=[Cst, eG_], w=[Cst])
                            op("dve", lambda e: e.scalar_tensor_tensor(
                                out=Cst.t[:, h, :, 0:256], in0=pU.t[:, :].rearrange("p (c n) -> p c n", c=2),
                                scalar=eG_.t[:, h:h + 1], in1=Cst.t[:, h, :, 0:256], op0=ALU.mult, op1=ALU.add),
                               r=[pU, eG_, Cst], w=[Cst])
                            op("dve", lambda e: e.scalar_tensor_tensor(
                                out=Cst.t[:, h, :, 256], in0=pN.t[:, 300:302], scalar=eG_.t[:, h:h + 1],
                                in1=Cst.t[:, h, :, 256], op0=ALU.mult, op1=ALU.add), r=[pN, eG_, Cst], w=[Cst])
                            if gc >= 47:
                                op("act", lambda e: e.copy(out=Cb.t[:, h].rearrange("p c n -> p (c n)"),
                                                           in_=Cst.t[:, h].rearrange("p c n -> p (c n)")),
                                   r=[Cst], w=[Cb])
                        units.append(u_ch)
                return units

            for u in proj_units(0):
                u()
            for gi in range(16):
                group_prep(gi)
                cu = chunk_units(gi)
                pu = proj_units(gi + 1) if gi + 1 < 16 else []
                nslots = len(cu) * (4 if gi >= 12 else 2)
                for u in cu:
                    for _ in u():
                        k_emit = -(-len(pu) // max(nslots, 1)) if pu else 0
                        nslots -= 1
                        for _i in range(k_emit):
                            if pu:
                                pu.pop(0)()
                    k_emit = -(-len(pu) // max(nslots, 1)) if pu else 0
                    nslots -= 1
                    for _i in range(k_emit):
                        if pu:
                            pu.pop(0)()
                while pu:
                    pu.pop(0)()
                if gi >= 12:
                    t0 = (gi - 12) * 512
                    op("sp", lambda e, t0=t0: e.dma_start(
                        out=ynT_d.ap()[8:16, :, t0:t0 + 512].rearrange("c p t -> p c t"), in_=ynTs.t[:]),
                       r=[ynTs], w=[ynT_db], dma=True)
            S.flush()
        st_holder[0] = gst

    def layer_norm_ops(y_ap, yB, g_bc, b_bc, stats, mv, rstd):
        for q in range(4):
            op("dve", lambda e, q=q: e.bn_stats(out=stats.t[:, q, :], in_=y_ap[:, q * 512:(q + 1) * 512]),
               r=[yB], w=[stats])
        op("dve", lambda e: e.bn_aggr(out=mv.t[:], in_=stats.t[:].rearrange("p a b -> p (a b)")), r=[stats], w=[mv])
        op("dve", lambda e: e.tensor_scalar(out=rstd.t[:], in0=mv.t[:, 1:2], scalar1=LN_EPS, scalar2=None,
                                            op0=ALU.add), r=[mv], w=[rstd])
        op("act", lambda e: e.activation(out=rstd.t[:], in_=rstd.t[:], func=AF.Sqrt), r=[rstd], w=[rstd])
        op("dve", lambda e: e.reciprocal(out=rstd.t[:], in_=rstd.t[:]), r=[rstd], w=[rstd])
        op("dve", lambda e: e.tensor_scalar(out=y_ap, in0=y_ap, scalar1=mv.t[:, 0:1], scalar2=rstd.t[:, 0:1],
                                            op0=ALU.subtract, op1=ALU.mult), r=[yB, mv, rstd], w=[yB])
        op("dve", lambda e: e.tensor_tensor(out=y_ap, in0=y_ap, in1=g_bc.t[:], op=ALU.mult), r=[yB, g_bc], w=[yB])
        op("dve", lambda e: e.tensor_tensor(out=y_ap, in0=y_ap, in1=b_bc.t[:], op=ALU.add), r=[yB, b_bc], w=[yB])

    def phase3():
        with ExitStack() as st:
            st_holder[0] = st
            ynTg = sb("ynTg", [128, 16, 512], BF16)
            wo = [sb("wo%d" % i, [128, 16, 512], BF16) for i in range(2)]
            xo = sb("xo", [128, 4, D], F32)
            y1 = [sb("y1_%d" % i, [128, D], F32) for i in range(4)]
            g_bc = sb("g1_bc", [128, D], F32)
            b_bc = sb("b1_bc", [128, D], F32)
            h1T = sb("h1T", [128, 16, 128], F32)
            h1b = [sb("h1b%d" % i, [128, D], BF16) for i in range(2)]
            wr = sb("wr", [128, 16, NEXP], F32)
            br_bc = sb("br_bc", [128, NEXP], F32)
            ecap_bc = sb("ecap_bc", [128, NEXP], F32)
            cntb = sb("cntb", [128, NEXP], F32)
            stats = sb("stats", [128, 4, 6], F32)
            mv = sb("mv", [128, 2], F32)
            rstd = sb("rstd", [128, 1], F32)
            lg = sb("lg", [128, NEXP], F32)
            mx8 = sb("mx8", [128, 8], F32)
            nmx = sb("nmx", [128, 1], F32)
            msk = sb("msk", [128, NEXP], F32)
            eml = sb("eml", [128, NEXP], F32)
            esum = sb("esum", [128, 1], F32)
            Gd = sb("Gd", [128, NEXP], F32)
            pos = sb("pos", [128, NEXP], F32)
            ovf = sb("ovf", [128, NEXP], F32)
            oh = sb("oh", [128, NEXP], F32)
            tmp32 = sb("tmp32", [128, NEXP], F32)
            offs_f = sb("offs_f", [128, 4], F32)

            op("sp", lambda e: e.dma_start(out=g_bc.t[:], in_=bcast(ln1_g, D)), w=[g_bc], dma=True)
            op("sp", lambda e: e.dma_start(out=b_bc.t[:], in_=bcast(ln1_b, D)), w=[b_bc], dma=True)
            op("sp", lambda e: e.dma_start(out=br_bc.t[:], in_=bcast(b_router, NEXP)), w=[br_bc], dma=True)
            op("sp", lambda e: e.dma_start(out=ecap_bc.t[:], in_=bcast(ecap, NEXP)), w=[ecap_bc], dma=True)
            op("sp", lambda e: e.dma_start(out=wr.t[:], in_=w_router.ap().rearrange("(kc p) n -> p kc n", p=128)),
               w=[wr], dma=True)
            op("pool", lambda e: e.memset(cntb.t[:], 0.0), w=[cntb])
            w_out_r = w_out.ap().rearrange("(kc p) c -> p kc c", p=128)
            pT = ps[4]
            pL = ps[5]
            pP = ps[6]
            pC = ps[7]
            woi = 0
            for tg in range(4):
                t0 = 512 * tg
                op("sp", lambda e, t0=t0: e.dma_start(
                    out=ynTg.t[:], in_=ynT_d.ap()[:, :, t0:t0 + 512].rearrange("c p t -> p c t")),
                   r=[ynT_db], w=[ynTg], dma=True)
                op("sp", lambda e, t0=t0: e.dma_start(
                    out=xo.t[:], in_=x_own.ap()[t0:t0 + 512, :].rearrange("(t p) d -> p t d", p=128)),
                   w=[xo], dma=True)
                for cg in range(4):
                    w_ = wo[woi % 2]
                    woi += 1
                    op("pool", lambda e, w_=w_, cg=cg: e.dma_start(out=w_.t[:], in_=w_out_r[:, :, cg * 512:(cg + 1) * 512]),
                       w=[w_], dma=True)
                    for tl in range(4):
                        pp = ps[(cg * 4 + tl) % 4]

                        def mmo(e, pp=pp, w_=w_, tl=tl):
                            for kc in range(16):
                                ins = e.matmul(pp.t[:, :], ynTg.t[:, kc, tl * 128:(tl + 1) * 128], w_.t[:, kc, :],
                                               start=(kc == 0), stop=(kc == 15))
                            return ins
                        op("pe", mmo, r=[ynTg, w_], w=[pp])
                        op("dve", lambda e, pp=pp, tl=tl, cg=cg: e.scalar_tensor_tensor(
                            out=y1[tl].t[:, cg * 512:(cg + 1) * 512], in0=xo.t[:, tl, cg * 512:(cg + 1) * 512],
                            scalar=ALPHA, in1=pp.t[:, :], op0=ALU.mult, op1=ALU.add), r=[xo, pp], w=[y1[tl]])
                for tl in range(4):
                    tt = 4 * tg + tl
                    y_ = y1[tl]
                    layer_norm_ops(y_.t[:], y_, g_bc, b_bc, stats, mv, rstd)
                    op("sp", lambda e, y_=y_, tt=tt: e.dma_start(out=h1_d.ap()[tt * 128:(tt + 1) * 128, :], in_=y_.t[:]),
                       r=[y_], w=[h1_db], dma=True, par=True)
                    hb = h1b[tt % 2]
                    op("act", lambda e, y_=y_, hb=hb: e.copy(out=hb.t[:], in_=y_.t[:]), r=[y_], w=[hb])
                    for q in range(4):
                        def trh(e, y_=y_, q=q):
                            for u in range(4):
                                kc = 4 * q + u
                                ins = e.transpose(pT.t[:, u * 128:(u + 1) * 128], y_.t[:, kc * 128:(kc + 1) * 128],
                                                  ident_f.t[:, :])
                            return ins
                        op("pe", trh, r=[y_, ident_f], w=[pT])
                        op("act", lambda e, q=q: e.copy(
                            out=h1T.t[:, 4 * q:4 * q + 4, :], in_=pT.t[:, :].rearrange("p (u t) -> p u t", u=4)),
                           r=[pT], w=[h1T])

                    def mml(e):
                        for kc in range(16):
                            ins = e.matmul(pL.t[:, 0:32], h1T.t[:, kc, :], wr.t[:, kc, :], start=(kc == 0),
                                           stop=(kc == 15))
                        return ins
                    op("pe", mml, r=[h1T, wr], w=[pL])
                    op("dve", lambda e: e.tensor_tensor(out=lg.t[:], in0=pL.t[:, 0:32], in1=br_bc.t[:], op=ALU.add),
                       r=[pL, br_bc], w=[lg])
                    op("dve", lambda e: e.max(out=mx8.t[:], in_=lg.t[:]), r=[lg], w=[mx8])
                    op("dve", lambda e: e.tensor_scalar(out=nmx.t[:], in0=mx8.t[:, 0:1], scalar1=-1.0, scalar2=None,
                                                        op0=ALU.mult), r=[mx8], w=[nmx])
                    op("dve", lambda e: e.tensor_scalar(out=msk.t[:], in0=lg.t[:], scalar1=mx8.t[:, 3:4], scalar2=None,
                                                        op0=ALU.is_ge), r=[lg, mx8], w=[msk])
                    op("act", lambda e: e.activation(out=eml.t[:], in_=lg.t[:], func=AF.Exp, bias=nmx.t[:, 0:1],
                                                     scale=1.0), r=[lg, nmx], w=[eml])
                    op("dve", lambda e: e.tensor_tensor(out=eml.t[:], in0=eml.t[:], in1=msk.t[:], op=ALU.mult),
                       r=[eml, msk], w=[eml])
                    op("dve", lambda e: e.tensor_reduce(out=esum.t[:], in_=eml.t[:], axis=AX.X, op=ALU.add),
                       r=[eml], w=[esum])
                    op("dve", lambda e: e.reciprocal(out=esum.t[:], in_=esum.t[:]), r=[esum], w=[esum])
                    op("dve", lambda e: e.tensor_scalar(out=Gd.t[:], in0=eml.t[:], scalar1=esum.t[:, 0:1], scalar2=None,
                                                        op0=ALU.mult), r=[eml, esum], w=[Gd])

                    def mmp(e):
                        e.matmul(pP.t[:, 64:96], trist_f.t[:, :], msk.t[:, :], start=True, stop=True)
                        return e.matmul(pC.t[:, 128:160], ones_f.t[:, :], msk.t[:, :], start=True, stop=True)
                    op("pe", mmp, r=[trist_f, ones_f, msk], w=[pP, pC])
                    op("dve", lambda e: e.tensor_tensor(out=pos.t[:], in0=pP.t[:, 64:96], in1=cntb.t[:], op=ALU.add),
                       r=[pP, cntb], w=[pos])
                    op("dve", lambda e: e.tensor_tensor(out=cntb.t[:], in0=pC.t[:, 128:160], in1=cntb.t[:], op=ALU.add),
                       r=[pC, cntb], w=[cntb])
                    op("dve", lambda e: e.tensor_scalar(out=ovf.t[:], in0=pos.t[:], scalar1=float(CAP), scalar2=None,
                                                        op0=ALU.is_ge), r=[pos], w=[ovf])
                    op("dve", lambda e: e.tensor_tensor(out=pos.t[:], in0=pos.t[:], in1=ecap_bc.t[:], op=ALU.add),
                       r=[pos, ecap_bc], w=[pos])
                    op("dve", lambda e: e.tensor_scalar(out=tmp32.t[:], in0=pos.t[:], scalar1=-1.0, scalar2=float(NSLOT),
                                                        op0=ALU.mult, op1=ALU.add), r=[pos], w=[tmp32])
                    op("dve", lambda e: e.tensor_tensor(out=tmp32.t[:], in0=tmp32.t[:], in1=ovf.t[:], op=ALU.mult),
                       r=[tmp32, ovf], w=[tmp32])
                    op("dve", lambda e: e.tensor_tensor(out=pos.t[:], in0=pos.t[:], in1=tmp32.t[:], op=ALU.add),
                       r=[pos, tmp32], w=[pos])
                    for k in range(4):
                        op("dve", lambda e, k=k: e.tensor_scalar(out=oh.t[:], in0=lg.t[:], scalar1=mx8.t[:, k:k + 1],
                                                                 scalar2=None, op0=ALU.is_equal), r=[lg, mx8], w=[oh])
                        op("dve", lambda e: e.tensor_tensor(out=tmp32.t[:], in0=oh.t[:], in1=pos.t[:], op=ALU.mult),
                           r=[oh, pos], w=[tmp32])
                        op("dve", lambda e, k=k: e.tensor_reduce(out=offs_f.t[:, k:k + 1], in_=tmp32.t[:], axis=AX.X,
                                                                 op=ALU.add), r=[tmp32], w=[offs_f])
                        op("dve", lambda e: e.tensor_tensor(out=tmp32.t[:], in0=oh.t[:], in1=Gd.t[:], op=ALU.mult),
                           r=[oh, Gd], w=[tmp32])
                        op("dve", lambda e, k=k, tt=tt: e.tensor_reduce(out=gk.t[:, tt, k:k + 1], in_=tmp32.t[:],
                                                                        axis=AX.X, op=ALU.add), r=[tmp32], w=[gk])
                    op("dve", lambda e, tt=tt: e.tensor_copy(out=offs_i.t[:, tt, :], in_=offs_f.t[:]),
                       r=[offs_f], w=[offs_i])
                    if debug:
                        dbs = sb("dbs%d" % tt, [128, 40], F32)
                        op("dve", lambda e, dbs=dbs: e.tensor_copy(out=dbs.t[:, 0:32], in_=lg.t[:]), r=[lg], w=[dbs])
                        op("dve", lambda e, dbs=dbs: e.tensor_copy(out=dbs.t[:, 32:36], in_=offs_f.t[:]),
                           r=[offs_f], w=[dbs])
                        op("dve", lambda e, dbs=dbs, tt=tt: e.tensor_copy(out=dbs.t[:, 36:40], in_=gk.t[:, tt, :]),
                           r=[gk], w=[dbs])
                        op("sp", lambda e, dbs=dbs, tt=tt: e.dma_start(out=dbg_r.ap()[:, tt, :], in_=dbs.t[:]),
                           r=[dbs], w=[dbg_rb], dma=True, par=True)
                    for k in range(4):
                        op("pool", lambda e, hb=hb, tt=tt, k=k: e.indirect_dma_start(
                            out=xs_d.ap()[:, :], out_offset=bass.IndirectOffsetOnAxis(ap=offs_i.t[:, tt, k:k + 1], axis=0),
                            in_=hb.t[:, :], in_offset=None),
                           r=[hb, offs_i], w=[xs_db], dma=True, par=True, key=hb.b)
            S.flush()
        st_holder[0] = gst

    def phase4():
        with ExitStack() as st:
            st_holder[0] = st
            wb = [sb("wb%d" % i, [128, 16, 512], BF16) for i in range(4)]
            xs_tm = [sb("xs_tm%d" % i, [128, NTC, D], BF16) for i in range(2)]
            xsT = sb("xsT", [128, 16, CAP], BF16)
            gaT = sb("gaT", [128, 16, CAP], BF16)
            actT = sb("actT", [128, 16, CAP], BF16)
            g32 = [sb("g32_%d" % i, [128, CAP], F32) for i in range(2)]
            sg32 = [sb("sg32_%d" % i, [128, CAP], F32) for i in range(2)]
            u32 = [sb("u32_%d" % i, [128, CAP], F32) for i in range(2)]
            ys_sb = [sb("ys_sb%d" % i, [128, 512], F32) for i in range(3)]
            bup_s = sb("bup_s", [128, NEXP, 32], F32)
            bd = [sb("bd%d" % i, [1, D], BF16) for i in range(2)]
            op("sp", lambda e: e.dma_start(out=bup_s.t[:], in_=bupT.ap()), w=[bup_s], dma=True)
            zrow = sb("zrow", [1, 512], F32)
            op("pool", lambda e: e.memset(zrow.t[:], 0.0), w=[zrow])
            for q in range(4):
                op("sp", lambda e, q=q: e.dma_start(out=ys_d.ap()[NSLOT:NSLOT + 1, q * 512:(q + 1) * 512], in_=zrow.t[:]),
                   r=[zrow], w=[ys_db], dma=True, par=True)
            wbi = 0
            upi = 0
            dni = 0

            def load_xs(ex_):
                xt = xs_tm[ex_ % 2]
                r0 = ex_ * CAP
                op("sp", lambda e, xt=xt, r0=r0: e.dma_start(
                    out=xt.t[:], in_=xs_d.ap()[r0:r0 + CAP, :].rearrange("(t p) d -> p t d", p=128)),
                   r=[xs_db], w=[xt], dma=True)

            def transposes(ex_):
                xt = xs_tm[ex_ % 2]
                for tl in range(NTC):
                    for q in range(2):
                        pt = ps[6 + q]
                        ptb = psb16[6 + q]

                        def trx(e, ptb=ptb, xt=xt, tl=tl, q=q):
                            for u in range(8):
                                kc = 8 * q + u
                                ins = e.transpose(ptb[:, u * 128:(u + 1) * 128], xt.t[:, tl, kc * 128:(kc + 1) * 128],
                                                  ident_b.t[:, :])
                            return ins
                        op("pe", trx, r=[xt, ident_b], w=[pt])
                        op("act", lambda e, ptb=ptb, tl=tl, q=q: e.copy(
                            out=xsT.t[:, 8 * q:8 * q + 8, tl * 128:(tl + 1) * 128],
                            in_=ptb[:, 0:1024].rearrange("p (u t) -> p u t", u=8)), r=[pt], w=[xsT])

            load_xs(0)
            transposes(0)
            for ex_ in range(NEXP):
                bd_ = bd[ex_ % 2]
                r0 = ex_ * CAP
                if ex_ + 1 < NEXP:
                    load_xs(ex_ + 1)
                op("pool", lambda e, bd_=bd_, ex_=ex_: e.dma_start(out=bd_.t[:], in_=b_down.ap()[ex_:ex_ + 1, :]),
                   w=[bd_], dma=True)
                w_up_r = w_up.ap()[ex_].rearrange("(kc p) c -> p kc c", p=128)
                for cg in range(8):
                    w_ = wb[wbi % 4]
                    wbi += 1
                    op("pool", lambda e, w_=w_, w_up_r=w_up_r, cg=cg: e.dma_start(
                        out=w_.t[:], in_=w_up_r[:, :, cg * 512:(cg + 1) * 512]), w=[w_], dma=True)
                    for cc in range(4):
                        ch = 4 * cg + cc
                        pp = ps[upi % 3]
                        upi += 1

                        def mmup(e, pp=pp, w_=w_, cc=cc):
                            for kc in range(16):
                                ins = e.matmul(pp.t[:, 0:CAP], w_.t[:, kc, cc * 128:(cc + 1) * 128], xsT.t[:, kc, :],
                                               start=(kc == 0), stop=(kc == 15))
                            return ins
                        op("pe", mmup, r=[w_, xsT], w=[pp])
                        if ch < 16:
                            g_ = g32[ch % 2]
                            s_ = sg32[ch % 2]
                            op("dve", lambda e, pp=pp, g_=g_, ex_=ex_, ch=ch: e.tensor_scalar(
                                out=g_.t[:], in0=pp.t[:, 0:CAP], scalar1=bup_s.t[:, ex_, ch:ch + 1], scalar2=7.0,
                                op0=ALU.add, op1=ALU.min), r=[pp, bup_s], w=[g_])
                            op("act", lambda e, g_=g_, s_=s_: e.activation(out=s_.t[:], in_=g_.t[:], func=AF.Sigmoid,
                                                                          scale=1.702), r=[g_], w=[s_])
                            op("dve", lambda e, g_=g_, s_=s_, ch=ch: e.tensor_tensor(
                                out=gaT.t[:, ch, :], in0=g_.t[:], in1=s_.t[:], op=ALU.mult), r=[g_, s_], w=[gaT])
                        else:
                            c2 = ch - 16
                            u_ = u32[ch % 2]
                            op("dve", lambda e, pp=pp, u_=u_, ex_=ex_, ch=ch: e.tensor_scalar(
                                out=u_.t[:], in0=pp.t[:, 0:CAP], scalar1=bup_s.t[:, ex_, ch:ch + 1], scalar2=7.0,
                                op0=ALU.add, op1=ALU.min), r=[pp, bup_s], w=[u_])
                            op("dve", lambda e, u_=u_: e.tensor_scalar(
                                out=u_.t[:], in0=u_.t[:], scalar1=-7.0, scalar2=1.0, op0=ALU.max, op1=ALU.add),
                               r=[u_], w=[u_])
                            op("dve", lambda e, u_=u_, c2=c2: e.tensor_tensor(
                                out=actT.t[:, c2, :], in0=u_.t[:], in1=gaT.t[:, c2, :], op=ALU.mult),
                               r=[u_, gaT], w=[actT])
                if ex_ + 1 < NEXP:
                    transposes(ex_ + 1)
                w_dn_r = w_down.ap()[ex_].rearrange("(kc p) c -> p kc c", p=128)
                for cg in range(4):
                    w_ = wb[wbi % 4]
                    wbi += 1
                    op("pool", lambda e, w_=w_, w_dn_r=w_dn_r, cg=cg: e.dma_start(
                        out=w_.t[:], in_=w_dn_r[:, :, cg * 512:(cg + 1) * 512]), w=[w_], dma=True)
                    for tl in range(NTC):
                        pp = ps[3 + dni % 3]
                        ysb = ys_sb[dni % 3]
                        dni += 1

                        def mmdn(e, pp=pp, w_=w_, tl=tl, bd_=bd_, cg=cg):
                            for kc in range(16):
                                e.matmul(pp.t[:, :], actT.t[:, kc, tl * 128:(tl + 1) * 128], w_.t[:, kc, :],
                                         start=(kc == 0), stop=False)
                            return e.matmul(pp.t[:, :], ones_b.t[0:1, :], bd_.t[0:1, cg * 512:(cg + 1) * 512],
                                            start=False, stop=True)
                        op("pe", mmdn, r=[actT, w_, bd_, ones_b], w=[pp])
                        op("act", lambda e, pp=pp, ysb=ysb: e.copy(out=ysb.t[:, :], in_=pp.t[:, :]), r=[pp], w=[ysb])
                        op("sp", lambda e, r0=r0, tl=tl, cg=cg, ysb=ysb: e.dma_start(
                            out=ys_d.ap()[r0 + tl * 128:r0 + (tl + 1) * 128, cg * 512:(cg + 1) * 512], in_=ysb.t[:, :]),
                           r=[ysb], w=[ys_db], dma=True, par=True)
            S.flush()
        st_holder[0] = gst

    def phase5():
        with ExitStack() as st:
            st_holder[0] = st
            h1t = [sb("h1t%d" % i, [128, D], F32) for i in range(2)]
            yk = [sb("yk%d" % i, [128, D], F32) for i in range(4)]
            g_bc = sb("g2_bc", [128, D], F32)
            b_bc = sb("b2_bc", [128, D], F32)
            stats = sb("stats2", [128, 4, 6], F32)
            mv = sb("mv2", [128, 2], F32)
            rstd = sb("rstd2", [128, 1], F32)
            op("sp", lambda e: e.dma_start(out=g_bc.t[:], in_=bcast(ln2_g, D)), w=[g_bc], dma=True)
            op("sp", lambda e: e.dma_start(out=b_bc.t[:], in_=bcast(ln2_b, D)), w=[b_bc], dma=True)
            yi = 0
            for tt in range(16):
                a_ = h1t[tt % 2]
                op("sp", lambda e, a_=a_, tt=tt: e.dma_start(out=a_.t[:], in_=h1_d.ap()[tt * 128:(tt + 1) * 128, :]),
                   r=[h1_db], w=[a_], dma=True)
                op("act", lambda e, a_=a_: e.activation(out=a_.t[:], in_=a_.t[:], func=AF.Copy, scale=ALPHA),
                   r=[a_], w=[a_])
                for k in range(4):
                    y_ = yk[yi % 4]
                    yi += 1
                    op("pool", lambda e, y_=y_, tt=tt, k=k: e.indirect_dma_start(
                        out=y_.t[:, :], out_offset=None, in_=ys_d.ap()[:, :],
                        in_offset=bass.IndirectOffsetOnAxis(ap=offs_i.t[:, tt, k:k + 1], axis=0)),
                       r=[ys_db, offs_i], w=[y_], dma=True)
                    op("dve", lambda e, y_=y_, a_=a_, tt=tt, k=k: e.scalar_tensor_tensor(
                        out=a_.t[:], in0=y_.t[:], scalar=gk.t[:, tt, k:k + 1], in1=a_.t[:], op0=ALU.mult, op1=ALU.add),
                       r=[y_, gk, a_], w=[a_])
                layer_norm_ops(a_.t[:], a_, g_bc, b_bc, stats, mv, rstd)
                op("sp", lambda e, a_=a_, tt=tt: e.dma_start(out=out_d.ap()[tt * 128:(tt + 1) * 128, :], in_=a_.t[:]),
                   r=[a_], w=[out_db[tt]], dma=True)
            S.flush()
        st_holder[0] = gst

    if ph_stop >= 1:
        phase1()
    if ph_stop >= 2:
        phase2()
    if ph_stop >= 3:
        phase3()
    if ph_stop >= 4:
        phase4()
    if ph_stop >= 5:
        phase5()
    if S.ops:
        S.flush()
    gst.close()
    return nc


def _t5_bucket(dist):
    dist = np.asarray(dist, np.int32)
    max_exact = 16
    d_f = np.maximum(dist, 1).astype(np.float32)
    large = max_exact + (np.log(d_f / np.float32(max_exact)) / np.float32(math.log(2048 / max_exact))
                         * np.float32(32 - max_exact)).astype(np.int32)
    large = np.minimum(large, 31)
    return np.where(dist < max_exact, dist, large)


def _attn_bias(rel_bias):
    out = np.full((128, 3, 16, 256), NEGB, np.float32)
    ki = np.arange(128)[:, None]
    qi = np.arange(128)[None, :]
    for g, d in enumerate(DILS):
        jc = qi - ki
        jp = qi + 128 - ki
        bc = _t5_bucket(np.maximum(jc, 0) * d)
        bp = _t5_bucket(np.minimum(np.maximum(jp, 0), 128) * d)
        for h in range(16):
            cur = np.where(jc >= 0, rel_bias[bc, h], np.float32(NEGB))
            prv = np.where(jp <= 128, rel_bias[bp, h], np.float32(NEGB))
            out[:, g, h, 0:128] = cur
            out[:, g, h, 128:256] = prv
    return out.reshape(128, 3 * 16 * 256)


def make_in_maps(inputs, ph_stop=5, cores=range(NCORES)):
    x = np.asarray(inputs["x"], np.float32)
    f = lambda k: np.ascontiguousarray(np.asarray(inputs[k], np.float32)[0])
    w_in = f("w_in")
    common = {
        "w_in": w_in,
        "bgate": np.concatenate([f("b_igate"), f("b_fgate")])[None, :].astype(np.float32),
        "cw": np.ascontiguousarray(f("conv_w").reshape(4, 16, 128).transpose(2, 1, 0)),
        "cb": np.ascontiguousarray(f("conv_b").reshape(16, 128).T),
        "battn": _attn_bias(np.asarray(inputs["rel_bias"], np.float32)),
        "beta": np.concatenate([f("beta_attn"), f("beta_mlstm")])[None, :],
        "ident": np.eye(128, dtype=np.float32),
        "triu": np.triu(np.ones((128, 128), np.float32)),
        "trist": np.triu(np.ones((128, 128), np.float32), 1),
    }
    if ph_stop >= 3:
        common.update({
            "w_out": f("w_out"), "ln1_g": f("ln1_g")[None, :], "ln1_b": f("ln1_b")[None, :],
            "w_router": f("w_router"), "b_router": f("b_router")[None, :],
            "ecap": (np.arange(NEXP, dtype=np.float32) * CAP)[None, :],
        })
    if ph_stop >= 4:
        common.update({
            "w_up": f("w_up"), "w_down": f("w_down"), "b_down": f("b_down"),
            "bupT": np.ascontiguousarray(f("b_up").reshape(NEXP, 32, 128).transpose(2, 0, 1)),
        })
    if ph_stop >= 5:
        common.update({"ln2_g": f("ln2_g")[None, :], "ln2_b": f("ln2_b")[None, :]})
    maps = []
    for c in cores:
        b, j = c // 4, c % 4
        nvalid = 2048 * (j + 1)
        xT = np.zeros((D, NSL), np.float32)
        xT[:, NSL - nvalid:] = x[b, :nvalid, :].T
        vs = np.zeros(NSL, np.float32)
        vs[NSL - nvalid:] = 1.0
        m = dict(common)
        m["xT"] = xT
        m["valid"] = np.ascontiguousarray(vs.reshape(64, 128).T)
        m["hv"] = np.full((128, 1), 1.0 if j > 0 else 0.0, np.float32)
        if ph_stop >= 3:
            m["x_own"] = np.ascontiguousarray(x[b, 2048 * j:2048 * (j + 1), :])
        maps.append(m)
    return maps


_NC_CACHE = {}


def kernel(**inputs):
    if "nc" not in _NC_CACHE:
        _NC_CACHE["nc"] = build(5, False)
    nc = _NC_CACHE["nc"]
    maps = make_in_maps(inputs, 5)
    res = run_bass_kernel_spmd(nc, maps, core_ids=list(range(NCORES)))
    out = np.zeros((2, 8192, D), np.float32)
    for c in range(NCORES):
        b, j = c // 4, c % 4
        out[b, 2048 * j:2048 * (j + 1), :] = res.results[c]["out"]
    return out
```
